# Optimizing a Trainium2 kernel written in Bass

```python
import math
import jax, jax.numpy as jnp
from jax import lax
import numpy as np


D_MODEL = 1024
BATCH = 8
SEQ = 4096
DEPTH = 2

N_META = 16
ROPE_THETA = 10000.0
RMS_EPS = 1e-6
LN_EPS = 1e-5
CONV_W = 5
Q_BLOCK = 128
N_BRANCH = 4
BRANCH_W = 512
GDN_HEADS = 4
GDN_DK = 128
GDN_DV = 128
GDN_CHUNK = 64
MLA_HEADS = 4
MLA_NOPE = 128
MLA_ROPE = 64
MLA_V = 128
MLA_Q_LORA = 256
MLA_KV_LORA = 128
LRU_WIDTH = 512
LRU_BLOCKS = 8
LRU_C = 8.0
DIFF_HEADS = 4
DIFF_DH = 64
N_EXPERTS = 32
TOP_K = 4
D_FF = D_MODEL
SWIGLU_LIMIT = 7.0
SWIGLU_ALPHA = 1.702
EXPERT_BLOCK = 256
DEEPNORM_ALPHA = (2 * DEPTH) ** 0.25
DEEPNORM_BETA = (8 * DEPTH) ** -0.25
GDN_QKV_W = GDN_HEADS * (2 * GDN_DK + GDN_DV)
IN_SPLITS = (GDN_QKV_W, GDN_HEADS * GDN_DV, 2 * GDN_HEADS, 2 * GDN_HEADS,
             MLA_Q_LORA, MLA_KV_LORA, MLA_ROPE, LRU_WIDTH,
             DIFF_HEADS * 2 * DIFF_DH, DIFF_HEADS * 2 * DIFF_DH, DIFF_HEADS * 2 * DIFF_DH)
IN_WIDTH = sum(IN_SPLITS)

kernel_name = 'hybrid_gdn_mla_rglru_diffattn_moe_encoder'


def rms_norm(u, g):
    uf = u.astype(jnp.float32)
    y = uf * lax.rsqrt(jnp.mean(uf * uf, axis=-1, keepdims=True) + RMS_EPS)
    return (y * g.astype(jnp.float32)).astype(u.dtype)


def layer_norm(u, g, b):
    uf = u.astype(jnp.float32)
    mu = jnp.mean(uf, axis=-1, keepdims=True)
    var = jnp.mean(jnp.square(uf - mu), axis=-1, keepdims=True)
    y = (uf - mu) * lax.rsqrt(var + LN_EPS) * g.astype(jnp.float32) + b.astype(jnp.float32)
    return y.astype(u.dtype)


def l2_norm(u):
    return u * lax.rsqrt(jnp.sum(u * u, axis=-1, keepdims=True) + 1e-6)


def rope(u, pos):
    half = u.shape[-1] // 2
    inv_freq = ROPE_THETA ** (-jnp.arange(half, dtype=jnp.float32) / half)
    ang = pos.astype(jnp.float32)[..., None] * inv_freq
    ang = ang.reshape(ang.shape[:2] + (1,) * (u.ndim - 3) + (half,))
    cos, sin = jnp.cos(ang), jnp.sin(ang)
    uf = u.astype(jnp.float32)
    u1, u2 = uf[..., :half], uf[..., half:]
    return jnp.concatenate([u1 * cos - u2 * sin, u2 * cos + u1 * sin], axis=-1).astype(u.dtype)


def dwconv(u, w):
    return lax.conv_general_dilated(u, w[:, None, :].astype(u.dtype), window_strides=(1,),
                                    padding=[(CONV_W // 2, CONV_W // 2)],
                                    dimension_numbers=('NWC', 'WIO', 'NWC'),
                                    feature_group_count=u.shape[-1])


def split_cols(u, widths):
    cuts = [int(c) for c in np.cumsum(widths)[:-1]]
    return jnp.split(u, cuts, axis=-1)


def to_query_blocks(u):
    t = u.shape[1]
    nb = -(-t // Q_BLOCK)
    widths = [(0, 0)] * u.ndim
    widths[1] = (0, nb * Q_BLOCK - t)
    u = jnp.pad(u, widths)
    return jnp.moveaxis(u.reshape((u.shape[0], nb, Q_BLOCK) + u.shape[2:]), 1, 0)


def from_query_blocks(o, t):
    o = jnp.moveaxis(o, 0, 1)
    return o.reshape((o.shape[0], -1) + o.shape[3:])[:, :t]


def gdn_chunked(q, k, v, g, beta):
    bsz, nh, length, dk = q.shape
    dv = v.shape[-1]
    nc = length // GDN_CHUNK
    chunks = lambda u: u.reshape((bsz, nh, nc, GDN_CHUNK) + u.shape[3:])
    q, k, v, g, beta = chunks(q), chunks(k), chunks(v), chunks(g), chunks(beta)
    gc = jnp.cumsum(g, axis=-1)
    incl = jnp.tril(jnp.ones((GDN_CHUNK, GDN_CHUNK), dtype=bool))
    strict = jnp.tril(jnp.ones((GDN_CHUNK, GDN_CHUNK), dtype=bool), -1)
    decay = jnp.where(incl, jnp.exp(jnp.where(incl, gc[..., :, None] - gc[..., None, :], 0.0)), 0.0)
    kb = k * beta[..., None]
    l_mat = jnp.where(strict, jnp.einsum('bhnid,bhnjd->bhnij', kb, k) * decay, 0.0)
    a_mat = l_mat + jnp.eye(GDN_CHUNK, dtype=l_mat.dtype)
    rhs = jnp.concatenate([v * beta[..., None], kb * jnp.exp(gc)[..., None]], axis=-1)
    sol = lax.linalg.triangular_solve(a_mat, rhs, left_side=True, lower=True, unit_diagonal=True)
    u_c, w_c = sol[..., :dv], sol[..., dv:]
    attn = jnp.where(incl, jnp.einsum('bhnid,bhnjd->bhnij', q, k) * decay, 0.0)
    q_dec = q * jnp.exp(gc)[..., None]
    k_dec = k * jnp.exp(gc[..., -1:] - gc)[..., None]
    chunk_decay = jnp.exp(gc[..., -1])

    def step(state, inp):
        u_i, w_i, q_i, k_i, a_i, d_i = inp
        v_new = u_i - jnp.einsum('bhik,bhkv->bhiv', w_i, state)
        o_i = jnp.einsum('bhik,bhkv->bhiv', q_i, state) + jnp.einsum('bhij,bhjv->bhiv', a_i, v_new)
        state = state * d_i[..., None, None] + jnp.einsum('bhik,bhiv->bhkv', k_i, v_new)
        return state, o_i

    xs = tuple(jnp.moveaxis(t_, 2, 0) for t_ in (u_c, w_c, q_dec, k_dec, attn, chunk_decay))
    state0 = jnp.zeros((bsz, nh, dk, dv), q.dtype)
    _, o = lax.scan(step, state0, xs)
    return jnp.moveaxis(o, 0, 2).reshape(bsz, nh, length, dv)


def gdn_bidirectional(q, k, v, g, beta):
    t = q.shape[2]
    pad = (-N_META) % GDN_CHUNK

    def padt(u, front):
        widths = [(0, 0)] * u.ndim
        widths[2] = (pad, 0) if front else (0, pad)
        return jnp.pad(u, widths)

    fl = lambda u: jnp.flip(u, axis=2)
    fwd = gdn_chunked(padt(q, True), padt(k, True), padt(v, True),
                      padt(g[0], True), padt(beta[0], True))[:, :, pad:]
    bwd = gdn_chunked(padt(fl(q), False), padt(fl(k), False), padt(fl(v), False),
                      padt(fl(g[1]), False), padt(fl(beta[1]), False))[:, :, :t]
    return fwd + fl(bwd)


def gdn_mixer(qkv, z, a_in, b_in, conv_w, a_log, dt_bias, norm_g):
    bsz, t, _ = qkv.shape
    qkv = jax.nn.silu(dwconv(qkv, conv_w)).astype(jnp.float32)
    q, k, v = jnp.split(qkv, [GDN_HEADS * GDN_DK, 2 * GDN_HEADS * GDN_DK], axis=-1)
    heads = lambda u, d: u.reshape(bsz, t, GDN_HEADS, d).transpose(0, 2, 1, 3)
    q = l2_norm(heads(q, GDN_DK)) * (GDN_DK ** -0.5)
    k = l2_norm(heads(k, GDN_DK))
    v = heads(v, GDN_DV)
    a_in = a_in.astype(jnp.float32).reshape(bsz, t, 2, GDN_HEADS)
    g = -jnp.exp(a_log.astype(jnp.float32)) * jax.nn.softplus(a_in + dt_bias.astype(jnp.float32))
    beta = jax.nn.sigmoid(b_in.astype(jnp.float32).reshape(bsz, t, 2, GDN_HEADS))
    g, beta = g.transpose(2, 0, 3, 1), beta.transpose(2, 0, 3, 1)
    o = gdn_bidirectional(q, k, v, g, beta).transpose(0, 2, 1, 3)
    gate = jax.nn.silu(z.astype(jnp.float32)).reshape(bsz, t, GDN_HEADS, GDN_DV)
    o = rms_norm(o, norm_g) * gate
    return o.reshape(bsz, t, GDN_HEADS * GDN_DV).astype(z.dtype)


def mla_mixer(c_q, c_kv, k_r, pos, q_norm_g, kv_norm_g, w_uq, w_ukv):
    bsz, t, _ = c_q.shape
    q = (rms_norm(c_q, q_norm_g) @ w_uq).reshape(bsz, t, MLA_HEADS, MLA_NOPE + MLA_ROPE)
    q_nope, q_rope = q[..., :MLA_NOPE], rope(q[..., MLA_NOPE:], pos)
    kv = (rms_norm(c_kv, kv_norm_g) @ w_ukv).reshape(bsz, t, MLA_HEADS, MLA_NOPE + MLA_V)
    k_nope, v = kv[..., :MLA_NOPE], kv[..., MLA_NOPE:]
    k_rope = rope(k_r, pos)
    scale = (MLA_NOPE + MLA_ROPE) ** -0.5

    def attend(blk):
        qn, qr = blk
        s = (jnp.einsum('bqhd,bkhd->bhqk', qn, k_nope)
             + jnp.einsum('bqhr,bkr->bhqk', qr, k_rope)).astype(jnp.float32) * scale
        p = jax.nn.softmax(s, axis=-1).astype(v.dtype)
        return jnp.einsum('bhqk,bkhd->bqhd', p, v)

    o = lax.map(attend, (to_query_blocks(q_nope), to_query_blocks(q_rope)))
    return from_query_blocks(o, t).reshape(bsz, t, MLA_HEADS * MLA_V)


def rglru_mixer(u, conv_w, conv_b, w_a, b_a, w_x, b_x, lam):
    bsz, t, _ = u.shape
    u = dwconv(u, conv_w) + conv_b
    ub = u.reshape(bsz, t, LRU_BLOCKS, LRU_WIDTH // LRU_BLOCKS)
    blockdiag = lambda w, b: (jnp.einsum('btnc,rncd->rbtnd', ub, w).reshape(2, bsz, t, LRU_WIDTH)
                              + b[:, None, None, :]).astype(jnp.float32)
    r = jax.nn.sigmoid(blockdiag(w_a, b_a))
    i = jax.nn.sigmoid(blockdiag(w_x, b_x))
    log_a = -LRU_C * r * jax.nn.softplus(-lam.astype(jnp.float32))[:, None, None, :]
    a = jnp.exp(log_a)
    inp = jnp.sqrt(1.0 - jnp.exp(2.0 * log_a)) * i * u.astype(jnp.float32)[None]

    def combine(c1, c2):
        return (c1[0] * c2[0], c2[0] * c1[1] + c2[1])

    _, h_f = lax.associative_scan(combine, (a[0], inp[0]), axis=1)
    _, h_b = lax.associative_scan(combine, (a[1], inp[1]), reverse=True, axis=1)
    return (h_f + h_b).astype(u.dtype)


def diff_mixer(q, k, v, pos, lam_vec, norm_g, lam_init):
    bsz, t, _ = q.shape
    q = rope(q.reshape(bsz, t, DIFF_HEADS, 2, DIFF_DH), pos)
    k = rope(k.reshape(bsz, t, DIFF_HEADS, 2, DIFF_DH), pos)
    v = v.reshape(bsz, t, DIFF_HEADS, 2 * DIFF_DH)
    lf = lam_vec.astype(jnp.float32)
    lam = jnp.exp(jnp.sum(lf[0] * lf[1])) - jnp.exp(jnp.sum(lf[2] * lf[3])) + lam_init
    scale = DIFF_DH ** -0.5

    def attend(qb):
        s = jnp.einsum('bqhcd,bkhcd->bchqk', qb, k).astype(jnp.float32) * scale
        p = jax.nn.softmax(s, axis=-1)
        w = (p[:, 0] - lam * p[:, 1]).astype(v.dtype)
        return jnp.einsum('bhqk,bkhd->bqhd', w, v)

    o = from_query_blocks(lax.map(attend, to_query_blocks(q)), t)
    o = rms_norm(o, norm_g) * (1.0 - lam_init)
    return o.reshape(bsz, t, DIFF_HEADS * 2 * DIFF_DH)


def moe_ffn(x2, router_w, router_b, w_gu, b_gu, w_dn, b_dn):
    n = x2.shape[0]
    logits = (x2 @ router_w + router_b).astype(jnp.float32)
    top_val, top_idx = lax.top_k(logits, TOP_K)
    gate = jax.nn.softmax(top_val, axis=-1)
    nk = n * TOP_K
    flat_e = top_idx.reshape(nk).astype(jnp.int32)
    order = jnp.argsort(flat_e)
    sorted_e = flat_e[order]
    counts = jnp.zeros((N_EXPERTS,), jnp.int32).at[flat_e].add(1)
    padded = (counts + EXPERT_BLOCK - 1) // EXPERT_BLOCK * EXPERT_BLOCK
    pad_end = jnp.cumsum(padded)
    pad_start = pad_end - padded
    start = jnp.cumsum(counts) - counts
    slot_sorted = pad_start[sorted_e] + jnp.arange(nk, dtype=jnp.int32) - start[sorted_e]
    slot = jnp.zeros((nk,), jnp.int32).at[order].set(slot_sorted)
    n_blocks = -(-nk // EXPERT_BLOCK) + N_EXPERTS
    n_slots = n_blocks * EXPERT_BLOCK
    tok = jnp.full((n_slots,), n, jnp.int32).at[slot].set(jnp.arange(nk, dtype=jnp.int32) // TOP_K)
    xs = jnp.concatenate([x2, jnp.zeros((1, x2.shape[1]), x2.dtype)], axis=0)[tok]
    xs = xs.reshape(n_blocks, EXPERT_BLOCK, x2.shape[1])
    block_e = jnp.minimum(jnp.searchsorted(pad_end, jnp.arange(n_blocks, dtype=jnp.int32) * EXPERT_BLOCK,
                                           side='right'), N_EXPERTS - 1)

    def expert_block(args):
        xb, e = args
        h = xb @ w_gu[e] + b_gu[e]
        glu = jnp.minimum(h[..., ::2], SWIGLU_LIMIT)
        lin = jnp.clip(h[..., 1::2], -SWIGLU_LIMIT, SWIGLU_LIMIT)
        return ((lin + 1.0) * glu * jax.nn.sigmoid(SWIGLU_ALPHA * glu)) @ w_dn[e] + b_dn[e]

    ys = lax.map(expert_block, (xs, block_e)).reshape(n_slots, x2.shape[1])
    return jnp.einsum('nk,nkd->nd', gate.astype(x2.dtype), ys[slot].reshape(n, TOP_K, x2.shape[1]))


def setup_inputs(seed: int = 0) -> dict:
    key = jax.random.key(seed)
    ks = iter(jax.random.split(key, 48))
    nrm = lambda shape, scale: jax.random.normal(next(ks), shape, jnp.float32) * scale
    gain = lambda shape: 1.0 + nrm(shape, 0.02)
    uni = lambda shape, lo, hi: jax.random.uniform(next(ks), shape, jnp.float32, minval=lo, maxval=hi)
    L = DEPTH
    bw = LRU_WIDTH // LRU_BLOCKS
    dt = jnp.exp(uni((L, 2, GDN_HEADS), math.log(1e-3), math.log(1e-1)))
    s_lam = uni((L, 2, LRU_WIDTH), 0.9, 0.999) ** (1.0 / LRU_C)
    return {
        'x': nrm((BATCH, SEQ, D_MODEL), 1.0),
        'positions': jnp.broadcast_to(jnp.arange(SEQ, dtype=jnp.int32), (BATCH, SEQ)),
        'meta': nrm((N_META, D_MODEL), 1.0),
        'ln_in_g': gain((D_MODEL,)),
        'ln_in_b': nrm((D_MODEL,), 0.02),
        'w_in': nrm((L, D_MODEL, IN_WIDTH), D_MODEL ** -0.5),
        'w_gate': nrm((L, N_BRANCH, D_MODEL, D_MODEL), D_MODEL ** -0.5),
        'gdn_conv_w': nrm((L, CONV_W, GDN_QKV_W), CONV_W ** -0.5),
        'gdn_a_log': jnp.log(uni((L, 2, GDN_HEADS), 1.0, 16.0)),
        'gdn_dt_bias': dt + jnp.log(-jnp.expm1(-dt)),
        'gdn_norm_g': gain((L, GDN_DV)),
        'mla_q_norm_g': gain((L, MLA_Q_LORA)),
        'mla_kv_norm_g': gain((L, MLA_KV_LORA)),
        'mla_w_uq': nrm((L, MLA_Q_LORA, MLA_HEADS * (MLA_NOPE + MLA_ROPE)), MLA_Q_LORA ** -0.5),
        'mla_w_ukv': nrm((L, MLA_KV_LORA, MLA_HEADS * (MLA_NOPE + MLA_V)), MLA_KV_LORA ** -0.5),
        'lru_conv_w': nrm((L, CONV_W, LRU_WIDTH), CONV_W ** -0.5),
        'lru_conv_b': nrm((L, LRU_WIDTH), 0.02),
        'lru_w_a': nrm((L, 2, LRU_BLOCKS, bw, bw), bw ** -0.5),
        'lru_b_a': nrm((L, 2, LRU_WIDTH), 0.02),
        'lru_w_x': nrm((L, 2, LRU_BLOCKS, bw, bw), bw ** -0.5),
        'lru_b_x': nrm((L, 2, LRU_WIDTH), 0.02),
        'lru_lambda': jnp.log(s_lam) - jnp.log1p(-s_lam),
        'diff_lambda': nrm((L, 4, DIFF_DH), 0.1),
        'diff_norm_g': gain((L, 2 * DIFF_DH)),
        'w_branch': nrm((L, N_BRANCH, BRANCH_W, D_MODEL), BRANCH_W ** -0.5 * DEEPNORM_BETA),
        'w_out': nrm((L, D_MODEL, D_MODEL), D_MODEL ** -0.5 * DEEPNORM_BETA),
        'ln1_g': gain((L, D_MODEL)),
        'ln1_b': nrm((L, D_MODEL), 0.02),
        'router_w': nrm((L, D_MODEL, N_EXPERTS), D_MODEL ** -0.5),
        'router_b': nrm((L, N_EXPERTS), 0.01),
        'moe_w_gate_up': nrm((L, N_EXPERTS, D_MODEL, 2 * D_FF), D_MODEL ** -0.5),
        'moe_b_gate_up': nrm((L, N_EXPERTS, 2 * D_FF), 0.02),
        'moe_w_down': nrm((L, N_EXPERTS, D_FF, D_MODEL), D_FF ** -0.5 * DEEPNORM_BETA),
        'moe_b_down': nrm((L, N_EXPERTS, D_MODEL), 0.02),
        'ln2_g': gain((L, D_MODEL)),
        'ln2_b': nrm((L, D_MODEL), 0.02),
    }


def reference(x, positions, meta, ln_in_g, ln_in_b, w_in, w_gate, gdn_conv_w, gdn_a_log, gdn_dt_bias,
              gdn_norm_g, mla_q_norm_g, mla_kv_norm_g, mla_w_uq, mla_w_ukv, lru_conv_w, lru_conv_b,
              lru_w_a, lru_b_a, lru_w_x, lru_b_x, lru_lambda, diff_lambda, diff_norm_g, w_branch, w_out,
              ln1_g, ln1_b, router_w, router_b, moe_w_gate_up, moe_b_gate_up, moe_w_down, moe_b_down,
              ln2_g, ln2_b):
    bsz = x.shape[0]
    meta_tok = jnp.broadcast_to(meta[None].astype(x.dtype), (bsz, N_META, x.shape[-1]))
    h = jnp.concatenate([meta_tok, x], axis=1)
    t = h.shape[1]
    meta_pos = jnp.broadcast_to(jnp.arange(N_META, dtype=positions.dtype), (bsz, N_META))
    pos = jnp.concatenate([meta_pos, positions + N_META], axis=1)
    h = layer_norm(h, ln_in_g, ln_in_b)
    for l in range(DEPTH):
        proj = h @ w_in[l]
        (g_qkv, g_z, g_a, g_b, m_cq, m_ckv, m_kr, r_x, d_q, d_k, d_v) = split_cols(proj, IN_SPLITS)
        lam_init = 0.8 - 0.6 * math.exp(-0.3 * l)
        branches = (
            gdn_mixer(g_qkv, g_z, g_a, g_b, gdn_conv_w[l], gdn_a_log[l], gdn_dt_bias[l], gdn_norm_g[l]),
            mla_mixer(m_cq, m_ckv, m_kr, pos, mla_q_norm_g[l], mla_kv_norm_g[l], mla_w_uq[l], mla_w_ukv[l]),
            rglru_mixer(r_x, lru_conv_w[l], lru_conv_b[l], lru_w_a[l], lru_b_a[l], lru_w_x[l], lru_b_x[l],
                        lru_lambda[l]),
            diff_mixer(d_q, d_k, d_v, pos, diff_lambda[l], diff_norm_g[l], lam_init),
        )
        merged = jnp.zeros_like(h)
        for i in range(N_BRANCH):
            merged = merged + jax.nn.sigmoid(h @ w_gate[l, i]) * (branches[i] @ w_branch[l, i])
        h = layer_norm(DEEPNORM_ALPHA * h + merged @ w_out[l], ln1_g[l], ln1_b[l])
        y = moe_ffn(h.reshape(-1, h.shape[-1]), router_w[l], router_b[l], moe_w_gate_up[l],
                    moe_b_gate_up[l], moe_w_down[l], moe_b_down[l]).reshape(h.shape)
        h = layer_norm(DEEPNORM_ALPHA * h + y, ln2_g[l], ln2_b[l])
    return h[:, N_META:]
```

```python
import numpy as np
import concourse.bass as bass
import concourse.mybir as mybir
from concourse.bass_utils import run_bass_kernel_spmd
import numpy as np
import concourse.bass as bass
import concourse.mybir as mybir

F32 = mybir.dt.float32
BF16 = mybir.dt.bfloat16
I32 = mybir.dt.int32
AF = mybir.ActivationFunctionType
ALU = mybir.AluOpType
AX = mybir.AxisListType

ENGS = ['pe', 'act', 'dve', 'pool', 'sp']
SEM_EPOCH = 1000000
DMA_RING = 24


class Tok:
    __slots__ = ('w', 'r', 'name')

    def __init__(self, name=''):
        self.w = {}
        self.r = {}
        self.name = name


def toks(n, name=''):
    return [Tok(f'{name}{i}') for i in range(n)]


class Prog:
    def __init__(self, nc, es, same_engine_sync=False):
        self.nc = nc
        self.es = es
        self.same = same_engine_sync
        self.ops = {e: [] for e in ENGS}
        self.cur_sem = {}
        self.cur_cnt = {e: 0 for e in ENGS}
        self.sem_id = {}
        self.waited = {e: {} for e in ENGS}
        self.n_sems = 0
        for e in ['pe', 'act', 'dve', 'pool']:
            self.cur_sem[e] = self._new_sem(f'e_{e}')
        self.rings = {}
        self.ring_i = {}
        for q in ['sp', 'act', 'pool']:
            self.rings[q] = [self._new_sem(f'd_{q}{i}') for i in range(DMA_RING)]
            self.ring_i[q] = 0
        self.bar_sem = self._new_sem('bar')
        self.n_bar = 0
        self.n_ins = 0

    def _new_sem(self, name):
        s = self.es.enter_context(self.nc.semaphore(f'{name}_{self.n_sems}'))
        self.sem_id[id(s)] = self.n_sems
        self.n_sems += 1
        return s

    def _wait(self, eng, sem, val):
        k = id(sem)
        if self.waited[eng].get(k, 0) >= val:
            return
        self.waited[eng][k] = val
        self.ops[eng].append(('wait', sem, val))

    def _collect(self, eng, reads, writes, adds):
        need = {}

        def add(t):
            sem, val, src = t
            if src == eng and (not self.same or eng == 'pe'):
                return
            k = id(sem)
            if k not in need or need[k][1] < val:
                need[k] = (sem, val)
        for b in reads:
            for t in b.w.values():
                add(t)
        for b in writes:
            for t in b.w.values():
                add(t)
            for t in b.r.values():
                add(t)
        for b in adds:
            for t in b.w.values():
                if t[2] != 'dma':
                    add(t)
            for t in b.r.values():
                add(t)
        for sem, val in need.values():
            self._wait(eng, sem, val)

    def _update(self, tok, reads, writes, adds):
        k = id(tok[0])
        for b in reads:
            b.r[k] = tok
        for b in writes:
            b.w = {k: tok}
            b.r = {}
        for b in adds:
            b.w[k] = tok
            b.r = {}

    def op(self, eng, name, *args, reads=(), writes=(), adds=(), **kwargs):
        fn = (name, args, kwargs)
        self._collect(eng, reads, writes, adds)
        if self.cur_cnt[eng] >= SEM_EPOCH:
            self.cur_sem[eng] = self._new_sem(f'e_{eng}')
            self.cur_cnt[eng] = 0
        self.cur_cnt[eng] += 1
        sem = self.cur_sem[eng]
        self.ops[eng].append(('ins', fn, sem, 1))
        self.n_ins += 1
        tok = (sem, self.cur_cnt[eng], eng)
        self._update(tok, reads, writes, adds)
        return tok

    def group(self, eng, fns, reads=(), writes=(), adds=()):
        self._collect(eng, reads, writes, adds)
        for f in fns[:-1]:
            self.ops[eng].append(('ins', f, None, 0))
            self.n_ins += 1
        self.cur_cnt[eng] += 1
        sem = self.cur_sem[eng]
        self.ops[eng].append(('ins', fns[-1], sem, 1))
        self.n_ins += 1
        tok = (sem, self.cur_cnt[eng], eng)
        self._update(tok, reads, writes, adds)
        return tok

    def dma(self, q, out, in_, reads=(), writes=(), adds=(), **kw):
        self._collect(q, reads, writes, adds)
        i = self.ring_i[q]
        self.ring_i[q] += 1
        sem = self.rings[q][i % DMA_RING]
        rnd = i // DMA_RING
        if rnd > 0:
            self._wait(q, sem, 16 * rnd)
        val = 16 * (rnd + 1)
        self.ops[q].append(('ins', ('dma_start', (), dict(out=out, in_=in_, **kw)), sem, 16))
        self.n_ins += 1
        tok = (sem, val, 'dma')
        self._update(tok, reads, writes, adds)
        return tok

    def barrier(self):
        for e in ['pe', 'act', 'dve', 'pool']:
            if self.cur_cnt[e] > 0:
                self._wait('sp', self.cur_sem[e], self.cur_cnt[e])
        for q in self.rings:
            n = self.ring_i[q]
            for j in range(min(n, DMA_RING)):
                last_rnd = (n - 1 - j) // DMA_RING
                self._wait('sp', self.rings[q][j], 16 * (last_rnd + 1))
        self.n_bar += 1
        bs = self.bar_sem
        self.ops['sp'].append(('ins', ('sem_inc', (bs, 1), {}), None, 0))
        for e in ['pe', 'act', 'dve', 'pool']:
            self._wait(e, bs, self.n_bar)

    def emit(self):
        nc = self.nc
        self.barrier()

        def run(eng_name, eng):
            for o in self.ops[eng_name]:
                if o[0] == 'wait':
                    eng.wait_ge(o[1], o[2])
                else:
                    nm, a, k = o[1]
                    ins = getattr(eng, nm)(*a, **k)
                    if o[2] is not None:
                        ins.then_inc(o[2], o[3])
        with nc.Block() as block:
            @block.tensor
            def _(e):
                run('pe', e)

            @block.scalar
            def _(e):
                run('act', e)

            @block.vector
            def _(e):
                run('dve', e)

            @block.gpsimd
            def _(e):
                run('pool', e)

            @block.sync
            def _(e):
                run('sp', e)


class Arena:
    def __init__(self, nc, es, words):
        self.t = es.enter_context(nc.sbuf_tensor('arena', [128, words], F32))
        self.words = words
        self.top = 0
        self.marks = []

    def alloc(self, nwords, dtype=F32, nel=None):
        nw = (nwords + 7) // 8 * 8
        assert self.top + nw <= self.words, f'SBUF arena overflow {self.top}+{nw}>{self.words}'
        ap = self.t[:, self.top:self.top + nw]
        self.top += nw
        if dtype != F32:
            ap = ap.bitcast(dtype)
        return ap[:, 0:nel]

    def f32(self, n):
        return self.alloc(n, F32, n)

    def bf16(self, n):
        return self.alloc((n + 1) // 2, BF16, n)

    def i32(self, n):
        return self.alloc(n, I32, n)

    def push(self):
        self.marks.append(self.top)

    def pop(self):
        self.top = self.marks.pop()
import math
from contextlib import ExitStack

TP = 4224
NT = 33
T = 4112
P0 = 48
D = 1024
L = 2
BLOCKS = [(P0 + 512 * i, 512) for i in range(8)] + [(P0 + 4096, 16)]
KT = [(P0 + 128 * i, 128) for i in range(32)] + [(P0 + 4096, 16)]
ALPHA = (2 * L) ** 0.25
NEGBIG = -30000.0

PARAMS = [('ln_in_g', [1024]), ('ln_in_b', [1024]), ('w_in', [2, 1024, 4560]), ('w_gate', [2, 4, 1024, 1024]),
          ('gdn_conv_w', [2, 5, 1536]), ('gdn_a_log', [2, 2, 4]), ('gdn_dt_bias', [2, 2, 4]), ('gdn_norm_g', [2, 128]),
          ('mla_q_norm_g', [2, 256]), ('mla_kv_norm_g', [2, 128]), ('mla_w_uq', [2, 256, 768]), ('mla_w_ukv', [2, 128, 1024]),
          ('lru_conv_w', [2, 5, 512]), ('lru_conv_b', [2, 512]), ('lru_w_a', [2, 2, 8, 64, 64]), ('lru_b_a', [2, 2, 512]),
          ('lru_w_x', [2, 2, 8, 64, 64]), ('lru_b_x', [2, 2, 512]), ('lru_lambda', [2, 2, 512]), ('diff_lambda', [2, 4, 64]),
          ('diff_norm_g', [2, 128]), ('w_branch', [2, 4, 512, 1024]), ('w_out', [2, 1024, 1024]), ('ln1_g', [2, 1024]),
          ('ln1_b', [2, 1024]), ('router_w', [2, 1024, 32]), ('router_b', [2, 32]), ('moe_w_gate_up', [2, 32, 1024, 2048]),
          ('moe_b_gate_up', [2, 32, 2048]), ('moe_w_down', [2, 32, 1024, 1024]), ('moe_b_down', [2, 32, 1024]),
          ('ln2_g', [2, 1024]), ('ln2_b', [2, 1024])]


def make_consts():
    c = {}
    i = np.arange(128)
    c['ident'] = np.eye(128, dtype=np.float32)
    c['ones'] = np.ones((128, 128), np.float32)
    c['mf'] = (i[:, None] <= i[None, :]).astype(np.float32)
    c['mb'] = (i[:, None] >= i[None, :]).astype(np.float32)
    up_s = np.where(i[None, :] > i[:, None], 0.0, NEGBIG).astype(np.float32)
    lo_s = up_s.T.copy()
    up_i = np.where(i[None, :] >= i[:, None], 0.0, NEGBIG).astype(np.float32)
    lo_i = up_i.T.copy()
    for nm, m in [('up_s', up_s), ('lo_s', lo_s), ('up_i', up_i), ('lo_i', lo_i)]:
        c[nm] = np.tile(m, (1, 4))
    half = 32
    inv = (10000.0 ** (-np.arange(half, dtype=np.float32) / half)).astype(np.float32)
    c['invf'] = np.tile(np.concatenate([inv, inv])[:, None], (2, 1)).astype(np.float32)
    c['mpos'] = np.tile(np.arange(16, dtype=np.float32)[None, :], (128, 1))
    return c


CONST_SHAPES = {k: list(v.shape) for k, v in make_consts().items()}
ALL_SHAPES = {'x': [4096, 1024], 'positions': [4096], 'meta': [16, 1024]}
ALL_SHAPES.update({k: v for k, v in PARAMS})
ALL_SHAPES.update({'c_' + k: v for k, v in CONST_SHAPES.items()})


class LazyIn(dict):
    def __init__(self, kb):
        super().__init__()
        self.kb = kb

    def __missing__(self, name):
        shp = ALL_SHAPES[name]
        dt = I32 if name == 'positions' else F32
        ap = self.kb.nc.dram_tensor(name, shp, dt, kind="ExternalInput").ap()
        self[name] = ap
        return ap


class KB:
    def __init__(self, nc, es, debug=()):
        self.nc = nc
        self.es = es
        self.debug = set(debug)
        self.p = Prog(nc, es, same_engine_sync=True)
        self.ar = Arena(nc, es, 53000)
        self.ps = es.enter_context(nc.psum_tensor("ps", [128, 4096], F32))
        self.pst = toks(8, 'ps')
        self.bank_i = 0
        self.din = LazyIn(self)
        self.dout = {}
        self.dumps = {}
        self.scr = {}
        self.scr_t = {}
        import os
        self.gdn_stop = int(os.environ.get('GDN_STOP', '0'))

    def bank(self):
        b = self.bank_i % 8
        self.bank_i += 1
        return b

    def psb(self, b, n=512, rows=128, off=0):
        return self.ps[0:rows, b * 512 + off: b * 512 + off + n]

    def inp(self, name, shape, dt=F32):
        self.din[name] = self.nc.dram_tensor(name, shape, dt, kind="ExternalInput").ap()
        return self.din[name]

    def scratch(self, name, shape, dt):
        kind = "ExternalOutput" if name in self.debug else "Internal"
        t = self.nc.dram_tensor(name, shape, dt, kind=kind).ap()
        self.scr[name] = t
        self.scr_t[name] = Tok(name)
        return t

    def dump(self, name, ap, tok):
        if name not in self.debug:
            return
        shp = list(ap.shape)
        t = self.nc.dram_tensor(name, shp, ap.dtype, kind="ExternalOutput").ap()
        self.p.dma('sp', t, ap, reads=[tok])
        self.dumps[name] = t

    def mm(self, out, pairs, reads, bank_tok, start=True, stop=True):
        n = len(pairs)
        fns = [('matmul', (out, l, r), dict(start=(start and i == 0), stop=(stop and i == n - 1)))
               for i, (l, r) in enumerate(pairs)]
        return self.p.group('pe', fns, reads=reads, writes=[bank_tok])


def v3(ap, c):
    return ap.rearrange("p (c n) -> p c n", c=c)


def ln_tile(kb, X, xt, H, ht, ST, stt, g_bc, b_bc, cst, eps):
    p = kb.p
    p.op('dve', 'bn_stats', ST[:, 0:6], X[:, 0:512], reads=[xt], writes=[stt])
    p.op('dve', 'bn_stats', ST[:, 6:12], X[:, 512:1024], reads=[xt], writes=[stt])
    p.op('dve', 'bn_aggr', ST[:, 12:14], ST[:, 0:12], reads=[stt], writes=[stt])
    p.op('dve', 'tensor_scalar', ST[:, 14:15], ST[:, 13:14], eps, None, ALU.add, reads=[stt], writes=[stt])
    p.op('act', 'activation', out=ST[:, 14:15], in_=ST[:, 14:15], func=AF.Sqrt, reads=[stt], writes=[stt])
    p.op('dve', 'reciprocal', ST[:, 15:16], ST[:, 14:15], reads=[stt], writes=[stt])
    p.op('dve', 'tensor_scalar', H, X, ST[:, 12:13], ST[:, 15:16], ALU.subtract, ALU.mult,
         reads=[xt, stt], writes=[ht])
    p.op('pool', 'tensor_tensor', H, H, g_bc, ALU.mult, reads=[cst], writes=[ht])
    p.op('pool', 'tensor_tensor', H, H, b_bc, ALU.add, reads=[cst], writes=[ht])


def h_epilogue(kb, i, H, ht, TB, tbt, extra_fp32=None):
    p = kb.p
    p.dma('sp', kb.scr['h_tok'][128 * i:128 * i + 128, :], H, reads=[ht], adds=[kb.htok_t[i]])
    for half in range(2):
        b = kb.bank()
        fns = []
        for j in range(4):
            c = half * 4 + j
            fns.append(('transpose', (kb.psb(b, 128, off=j * 128), H[:, c * 128:(c + 1) * 128], kb.ident), {}))
        p.group('pe', fns, reads=[ht, kb.cst], writes=[kb.pst[b]])
        p.op('act', 'activation', out=TB[:, half * 512:(half + 1) * 512], in_=kb.psb(b), func=AF.Copy,
             reads=[kb.pst[b]], writes=[tbt])
        if extra_fp32 is not None:
            XB, xbt = extra_fp32
            p.op('act', 'activation', out=XB[:, half * 512:(half + 1) * 512], in_=kb.psb(b), func=AF.Copy, reads=[kb.pst[b]], adds=[xbt])
    lo, hi = 0, 128
    if i == 0:
        lo = P0
    if i == NT - 1:
        hi = 64
    hTv = v3(kb.hT, 8)
    TBv = v3(TB, 8)
    p.op('pool', 'tensor_copy', hTv[:, :, 128 * i + lo:128 * i + hi], TBv[:, :, lo:hi], reads=[tbt], writes=[kb.hT_t[i]])


def stage0(kb):
    p, ar = kb.p, kb.ar
    x, meta = kb.din['x'], kb.din['meta']
    ar.push()
    g_bc = ar.f32(1024); b_bc = ar.f32(1024)
    t = Tok('lnp')
    p.dma('sp', g_bc, kb.din['ln_in_g'].rearrange("(o n) -> o n", o=1).broadcast_to([128, 1024]), adds=[t])
    p.dma('sp', b_bc, kb.din['ln_in_b'].rearrange("(o n) -> o n", o=1).broadcast_to([128, 1024]), adds=[t])
    NB = 3
    xb = [ar.f32(1024) for _ in range(NB)]; xb_t = toks(NB, 'xb')
    hb = [ar.f32(1024) for _ in range(NB)]; hb_t = toks(NB, 'hb')
    tb = [ar.bf16(1024) for _ in range(NB)]; tb_t = toks(NB, 'tb')
    st = [ar.f32(16) for _ in range(NB)]; st_t = toks(NB, 'st')
    for i in range(NT):
        k = i % NB
        X = xb[k]
        if i == 0:
            p.op('pool', 'memset', X, 0.0, writes=[xb_t[k]])
            p.dma('sp', X[48:64, :], meta, adds=[xb_t[k]])
            p.dma('sp', X[64:128, :], x[0:64, :], adds=[xb_t[k]])
        elif i == NT - 1:
            p.op('pool', 'memset', X, 0.0, writes=[xb_t[k]])
            p.dma('sp', X[0:64, :], x[4032:4096, :], adds=[xb_t[k]])
        else:
            p.dma('sp', X, x[128 * i - 64:128 * i + 64, :], writes=[xb_t[k]])
        ln_tile(kb, X, xb_t[k], hb[k], hb_t[k], st[k], st_t[k], g_bc, b_bc, t, 1e-5)
        h_epilogue(kb, i, hb[k], hb_t[k], tb[k], tb_t[k])
    p.barrier()
    ar.pop()


def softplus_small(kb, out, x, tmp, tok):
    p = kb.p
    p.op('act', 'activation', out=tmp, in_=x, func=AF.Abs, reads=[tok], writes=[tok])
    p.op('act', 'activation', out=tmp, in_=tmp, func=AF.Exp, scale=-1.0, reads=[tok], writes=[tok])
    p.op('act', 'activation', out=tmp, in_=tmp, func=AF.Ln, bias=1.0, reads=[tok], writes=[tok])
    p.op('dve', 'tensor_scalar', out, x, 0.0, None, ALU.max, reads=[tok], writes=[tok])
    p.op('dve', 'tensor_tensor', out, out, tmp, ALU.add, reads=[tok], writes=[tok])


def load_w_cols(kb, dst3, src2d, c0, c1, tok, nk=8):
    for kc in range(nk):
        kb.p.dma('pool', dst3[:, kc, :], src2d[kc * 128:(kc + 1) * 128, c0:c1], adds=[tok])


def load_cols(kb, dst, src1d, tok):
    kb.p.dma('sp', dst, src1d.rearrange("(c p) -> p c", p=128), adds=[tok], allow_slow_non_contiguous=True)


def bcast_row(kb, dst, src1d, n, tok, rows=128):
    kb.p.dma('sp', dst, src1d.rearrange("(o n) -> o n", o=1).broadcast_to([rows, n]), adds=[tok])


def stage_lru(kb, l):
    p, ar = kb.p, kb.ar
    d = kb.din
    ar.push()
    wt = Tok('lru_w')
    w = v3(ar.bf16(8 * 512), 8)
    load_w_cols(kb, w, d['w_in'][l], 2512, 3024, wt)
    sm = Tok('lru_small')
    convw = v3(ar.f32(20), 4)
    for k in range(5):
        load_cols(kb, convw[:, :, k], d['lru_conv_w'][l, k], sm)
    convb = ar.f32(4)
    load_cols(kb, convb, d['lru_conv_b'][l], sm)
    ba = v3(ar.f32(8), 2); bx = v3(ar.f32(8), 2); lam = v3(ar.f32(8), 2)
    for r in range(2):
        load_cols(kb, ba[:, r, :], d['lru_b_a'][l, r], sm)
        load_cols(kb, bx[:, r, :], d['lru_b_x'][l, r], sm)
        load_cols(kb, lam[:, r, :], d['lru_lambda'][l, r], sm)
    coef = ar.f32(8); tmp8 = ar.f32(8)
    lam2 = lam.rearrange("p r c -> p (r c)")
    p.op('dve', 'tensor_scalar', lam2, lam2, -1.0, None, ALU.mult, reads=[sm], writes=[sm])
    softplus_small(kb, coef, lam2, tmp8, sm)
    p.op('dve', 'tensor_scalar', coef, coef, -8.0, None, ALU.mult, reads=[sm], writes=[sm])
    coef = v3(coef, 2)
    wbd_all = ar.bf16(16 * 128)
    wbd_t = Tok('wbd')
    p.op('pool', 'memset', wbd_all, 0.0, writes=[wbd_t])
    wbd = {}
    idx = 0
    for r in range(2):
        for gi, nm in enumerate(['lru_w_a', 'lru_w_x']):
            for c in range(4):
                m = wbd_all[:, idx * 128:(idx + 1) * 128]
                idx += 1
                p.dma('pool', m[0:64, 0:64], d[nm][l, r, 2 * c], adds=[wbd_t])
                p.dma('pool', m[64:128, 64:128], d[nm][l, r, 2 * c + 1], adds=[wbd_t])
                wbd[(r, gi, c)] = m
    bufs = [ar.f32(TP) for _ in range(6)]
    bt = toks(6, 'lrub')
    u_bf = ar.bf16(TP); ubt = Tok('ubf')
    o_bf = ar.bf16(TP); obt = Tok('obf')
    hT3 = v3(kb.hT, 8)
    V = slice(P0, P0 + T)
    for c in range(4):
        pre, u, ra, ix, a, h0 = bufs
        pre_t, u_t, ra_t, ix_t, a_t, h0_t = bt
        p.op('pool', 'memset', pre, 0.0, writes=[pre_t])
        for (p0, n) in BLOCKS:
            b = kb.bank()
            kb.mm(kb.psb(b, n), [(w[:, kc, c * 128:(c + 1) * 128], hT3[:, kc, p0:p0 + n]) for kc in range(8)],
                  reads=[wt] + kb.hT_all, bank_tok=kb.pst[b])
            p.op('act', 'activation', out=pre[:, p0:p0 + n], in_=kb.psb(b, n), func=AF.Copy, reads=[kb.pst[b]], adds=[pre_t])
        uv = u[:, V]
        p.op('act', 'activation', out=uv, in_=pre[:, P0 - 2:P0 - 2 + T], func=AF.Identity,
             scale=convw[:, c, 0:1], bias=convb[:, c:c + 1], reads=[pre_t, sm], writes=[u_t])
        for k in range(1, 5):
            p.op('dve', 'scalar_tensor_tensor', out=uv, in0=pre[:, P0 - 2 + k:P0 - 2 + k + T], scalar=convw[:, c, k:k + 1],
                 in1=uv, op0=ALU.mult, op1=ALU.add, reads=[pre_t, sm], writes=[u_t])
        p.op('pool', 'tensor_copy', u_bf[:, V], uv, reads=[u_t], writes=[ubt])
        hs = [h0, pre]
        hs_t = [h0_t, pre_t]
        for r in range(2):
            for (p0, n) in BLOCKS:
                for gi, (dst, dt_, bias) in enumerate([(ra, ra_t, ba), (ix, ix_t, bx)]):
                    b = kb.bank()
                    kb.mm(kb.psb(b, n), [(wbd[(r, gi, c)], u_bf[:, p0:p0 + n])], reads=[wbd_t, ubt], bank_tok=kb.pst[b])
                    p.op('act', 'activation', out=dst[:, p0:p0 + n], in_=kb.psb(b, n), func=AF.Sigmoid, bias=bias[:, r, c:c + 1],
                         reads=[kb.pst[b], sm], adds=[dt_])
            rav = ra[:, V]; ixv = ix[:, V]; av = a[:, V]
            p.op('act', 'activation', out=av, in_=rav, func=AF.Exp, scale=coef[:, r, c:c + 1], reads=[ra_t, sm], writes=[a_t])
            p.op('dve', 'tensor_tensor', rav, av, av, ALU.mult, reads=[a_t], writes=[ra_t])
            p.op('dve', 'tensor_scalar', rav, rav, -1.0, 1.0, ALU.mult, ALU.add, reads=[ra_t], writes=[ra_t])
            p.op('act', 'activation', out=rav, in_=rav, func=AF.Sqrt, reads=[ra_t], writes=[ra_t])
            p.op('dve', 'tensor_tensor', ixv, ixv, rav, ALU.mult, reads=[ra_t], writes=[ix_t])
            p.op('pool', 'tensor_tensor', ixv, ixv, uv, ALU.mult, reads=[u_t], writes=[ix_t])
            hv = hs[r][:, V]
            if r == 0:
                p.op('dve', 'tensor_tensor_scan', hv, av, ixv, 0.0, ALU.mult, ALU.add, reads=[a_t, ix_t], writes=[hs_t[r]])
            else:
                p.op('dve', 'tensor_tensor_scan', hv[:, ::-1], av[:, ::-1], ixv[:, ::-1], 0.0, ALU.mult, ALU.add,
                     reads=[a_t, ix_t], writes=[hs_t[r]])
        p.op('pool', 'tensor_tensor', o_bf[:, V], h0[:, V], pre[:, V], ALU.add, reads=[h0_t, pre_t], writes=[obt])
        p.dma('sp', kb.scr['brT2'][c * 128:(c + 1) * 128, V], o_bf[:, V], reads=[obt], adds=[kb.scr_t['brT2']])
    p.barrier()
    ar.pop()


TWO_PI = 6.28318
MAGIC = 12582912.0


def setup_consts(kb):
    p, ar = kb.p, kb.ar
    d = kb.din
    kb.ones = ar.f32(128)
    p.dma('sp', kb.ones, d['c_ones'], adds=[kb.cst])
    kb.ones_bf = ar.bf16(128)
    p.op('dve', 'tensor_copy', kb.ones_bf, kb.ones, reads=[kb.cst], adds=[kb.cst])
    kb.ones_row = ar.bf16(512)
    p.op('pool', 'memset', kb.ones_row, 1.0, adds=[kb.cst])
    kb.scratch('cs', [2, 64, TP], F32)
    ar.push()
    t = Tok('rope')
    posi = ar.i32(4096)[0:64, :]
    p.dma('sp', posi, d['positions'].rearrange("(o n) -> o n", o=1).broadcast_to([64, 4096]), adds=[t])
    x = ar.f32(TP)[0:64, :]
    tmp = ar.f32(TP)[0:64, :]
    o = ar.f32(TP)[0:64, :]
    invf = ar.f32(1)
    p.dma('sp', invf, d['c_invf'], adds=[t])
    p.op('pool', 'memset', x, 0.0, writes=[t])
    p.op('dve', 'tensor_copy', x[:, 64:64 + 4096], posi, reads=[t], writes=[t])
    p.op('dve', 'tensor_scalar', x[:, 64:64 + 4096], x[:, 64:64 + 4096], 16.0, None, ALU.add, reads=[t], writes=[t])
    p.dma('sp', x[:, 48:64], d['c_mpos'][0:64, :], reads=[t], adds=[t])
    p.op('dve', 'tensor_scalar', x, x, invf[0:64, 0:1], 1.0 / (2 * math.pi), ALU.mult, ALU.mult, reads=[t], writes=[t])
    for which, off in ((0, 0.0), (1, 0.25)):
        src = x
        if off != 0.0:
            p.op('dve', 'tensor_scalar', x, x, off, None, ALU.add, reads=[t], writes=[t])
        p.op('dve', 'tensor_scalar', tmp, src, MAGIC, None, ALU.add, reads=[t], writes=[t])
        p.op('dve', 'tensor_scalar', tmp, tmp, MAGIC, None, ALU.subtract, reads=[t], writes=[t])
        p.op('dve', 'tensor_tensor', tmp, src, tmp, ALU.subtract, reads=[t], writes=[t])
        p.op('act', 'activation', out=o, in_=tmp, func=AF.Sin, scale=TWO_PI, reads=[t], writes=[t])
        p.dma('sp', kb.scr['cs'][which], o, reads=[t], adds=[kb.scr_t['cs']])
    p.barrier()
    ar.pop()


def load_cs(kb, rows, tok):
    ar, p = kb.ar, kb.p
    sin = ar.f32(TP); cos = ar.f32(TP)
    for r0 in range(0, rows, 64):
        p.dma('sp', sin[r0:r0 + 64, :], kb.scr['cs'][0], reads=[kb.scr_t['cs']], adds=[tok])
        p.dma('sp', cos[r0:r0 + 64, :], kb.scr['cs'][1], reads=[kb.scr_t['cs']], adds=[tok])
    return sin, cos


def make_rot(kb, dst, src, reads, tok):
    p = kb.p
    nd = len(dst.shape)
    lo = (slice(None),) * (nd - 1) + (slice(0, 32),)
    hi = (slice(None),) * (nd - 1) + (slice(32, 64),)
    p.op('dve', 'tensor_scalar', dst[lo], src[hi], -1.0, None, ALU.mult, reads=reads, adds=[tok])
    p.op('dve', 'tensor_copy', dst[hi], src[lo], reads=reads, adds=[tok])


def rms_from_ss(kb, rstd, ss_ps, inv_n, eps, reads, tok):
    p = kb.p
    p.op('dve', 'tensor_scalar', rstd, ss_ps, inv_n, eps, ALU.mult, ALU.add, reads=reads, writes=[tok])
    p.op('act', 'activation', out=rstd, in_=rstd, func=AF.Sqrt, reads=[tok], writes=[tok])
    p.op('dve', 'reciprocal', rstd, rstd, reads=[tok], writes=[tok])


def attention(kb, nmaps, kparts, qparts, vsrc, scale, epilogue):
    p, ar = kb.p, kb.ar
    ar.push()
    NQB = 2
    qbuf = []
    for _ in range(NQB):
        qb = []
        for m in range(nmaps):
            qb.append([ar.bf16(512) for _ in qparts[m]])
        qbuf.append(qb)
    qt = toks(NQB, 'qb')
    NP = 4
    pts = [ar.bf16(512) for _ in range(NP)]
    ptt = toks(NP, 'pt')
    pi = 0
    si = 0
    for bi, (p0, n) in enumerate(BLOCKS):
        qb = qbuf[bi % NQB]
        for m in range(nmaps):
            for ci, (qd, r0, rows) in enumerate(qparts[m]):
                p.dma('sp', qb[m][ci][r0:r0 + rows, 0:n], qd[:, p0:p0 + n], reads=kb.att_qreads, adds=[qt[bi % NQB]])
        obanks = [4 + m for m in range(nmaps)]
        dbanks = [4 + nmaps + m for m in range(nmaps)]
        for j, (k0, nk) in enumerate(KT):
            for m in range(nmaps):
                sb = si % 4
                si += 1
                pairs = []
                for ci, (kap, r0, rows) in enumerate(kparts[m]):
                    pairs.append((kap[r0:r0 + rows, k0:k0 + nk], qb[m][ci][r0:r0 + rows, 0:n]))
                kb.mm(kb.psb(sb, n, rows=nk), pairs, reads=[qt[bi % NQB]] + kb.att_kreads, bank_tok=kb.pst[sb])
                PT = pts[pi % NP]; ptk = ptt[pi % NP]
                pi += 1
                p.op('act', 'activation', out=PT[0:nk, 0:n], in_=kb.psb(sb, n, rows=nk), func=AF.Exp, scale=scale,
                     reads=[kb.pst[sb]], writes=[ptk])
                p.op('pe', 'matmul', kb.psb(obanks[m], n), vsrc[0:nk, j, :], PT[0:nk, 0:n], start=(j == 0), stop=(j == len(KT) - 1),
                     reads=[ptk] + kb.att_vreads, writes=[kb.pst[obanks[m]]])
                p.op('pe', 'matmul', kb.psb(dbanks[m], n), kb.ones_bf[0:nk, :], PT[0:nk, 0:n], start=(j == 0), stop=(j == len(KT) - 1),
                     reads=[ptk, kb.cst], writes=[kb.pst[dbanks[m]]])
        epilogue(bi, p0, n, obanks, dbanks)
    ar.pop()


def stage_mla(kb, l):
    p, ar = kb.p, kb.ar
    d = kb.din
    for nm, shp, dt in [('mq_n', [512, TP], BF16), ('mq_r', [256, TP], BF16), ('mk_n', [512, TP], BF16),
                        ('mk_r', [64, TP], BF16), ('mv', [NT * 128, 512], BF16)]:
        if nm not in kb.scr:
            kb.scratch(nm, shp, dt)
    ar.push()
    wt = Tok('mla_w')
    w = v3(ar.bf16(8 * 448), 8)
    load_w_cols(kb, w, d['w_in'][l], 2064, 2512, wt)
    wkr_rot = v3(ar.bf16(8 * 64), 8)
    make_rot(kb, wkr_rot, w[:, :, 384:448], [wt], wt)
    wuq = v3(ar.bf16(2 * 768), 2)
    load_w_cols(kb, wuq, d['mla_w_uq'][l], 0, 768, wt, nk=2)
    wuq4 = wuq.rearrange("p c (h x) -> p c h x", h=4)
    wuq_rot = ar.bf16(2 * 4 * 64).rearrange("p (c h x) -> p c h x", c=2, h=4)
    make_rot(kb, wuq_rot, wuq4[:, :, :, 128:192], [wt], wt)
    wukv = ar.bf16(1024)
    p.dma('pool', wukv, d['mla_w_ukv'][l], adds=[wt])
    wukv4 = wukv.rearrange("p (h x) -> p h x", h=4)
    sm = Tok('mla_small')
    qg = ar.f32(2); kvg = ar.f32(1)
    load_cols(kb, qg, d['mla_q_norm_g'][l], sm)
    load_cols(kb, kvg, d['mla_kv_norm_g'][l], sm)
    cst_t = Tok('cs')
    sin, cos = load_cs(kb, 64, cst_t)
    ckvn = ar.bf16(TP); ckvn_t = Tok('ckvn')
    hT3 = v3(kb.hT, 8)
    ar.push()
    NB = 2
    cq_sb = [[ar.f32(512) for _ in range(2)] for _ in range(NB)]
    sq_sb = [[ar.f32(512) for _ in range(2)] for _ in range(NB)]
    ckv_sb = [ar.f32(512) for _ in range(NB)]
    sqk_sb = [ar.f32(512) for _ in range(NB)]
    rstd = [ar.f32(512) for _ in range(NB)]; rstdk = [ar.f32(512) for _ in range(NB)]
    cqn = [[ar.bf16(512) for _ in range(2)] for _ in range(NB)]
    t1 = [ar.f32(512) for _ in range(NB)]; t2 = [ar.f32(512) for _ in range(NB)]
    oqn = [ar.bf16(4 * 512) for _ in range(NB)]; oqr = [ar.bf16(4 * 512) for _ in range(NB)]
    okn = [ar.bf16(4 * 512) for _ in range(NB)]; okr = [ar.bf16(512) for _ in range(NB)]
    bt = [Tok(f'mlab{i}') for i in range(NB)]
    ot = [Tok(f'mlao{i}') for i in range(NB)]
    for bi, (p0, n) in enumerate(BLOCKS):
        k = bi % NB
        T_ = bt[k]
        for c in range(2):
            b = kb.bank()
            kb.mm(kb.psb(b, n), [(w[:, kc, c * 128:(c + 1) * 128], hT3[:, kc, p0:p0 + n]) for kc in range(8)],
                  reads=[wt] + kb.hT_all, bank_tok=kb.pst[b])
            p.op('act', 'activation', out=cq_sb[k][c][:, 0:n], in_=kb.psb(b, n), func=AF.Copy, reads=[kb.pst[b]], adds=[T_])
            p.op('act', 'activation', out=sq_sb[k][c][:, 0:n], in_=kb.psb(b, n), func=AF.Square, reads=[kb.pst[b]], adds=[T_])
        b = kb.bank()
        kb.mm(kb.psb(b, n), [(w[:, kc, 256:384], hT3[:, kc, p0:p0 + n]) for kc in range(8)], reads=[wt] + kb.hT_all, bank_tok=kb.pst[b])
        p.op('act', 'activation', out=ckv_sb[k][:, 0:n], in_=kb.psb(b, n), func=AF.Copy, reads=[kb.pst[b]], adds=[T_])
        p.op('act', 'activation', out=sqk_sb[k][:, 0:n], in_=kb.psb(b, n), func=AF.Square, reads=[kb.pst[b]], adds=[T_])
        b = kb.bank()
        kb.mm(kb.psb(b, n), [(kb.ones, sq_sb[k][0][:, 0:n]), (kb.ones, sq_sb[k][1][:, 0:n])], reads=[T_, kb.cst], bank_tok=kb.pst[b])
        rms_from_ss(kb, rstd[k][:, 0:n], kb.psb(b, n), 1.0 / 256, 1e-6, [kb.pst[b]], T_)
        b = kb.bank()
        kb.mm(kb.psb(b, n), [(kb.ones, sqk_sb[k][:, 0:n])], reads=[T_, kb.cst], bank_tok=kb.pst[b])
        rms_from_ss(kb, rstdk[k][:, 0:n], kb.psb(b, n), 1.0 / 128, 1e-6, [kb.pst[b]], T_)
        for c in range(2):
            p.op('dve', 'scalar_tensor_tensor', out=cqn[k][c][:, 0:n], in0=cq_sb[k][c][:, 0:n], scalar=qg[:, c:c + 1],
                 in1=rstd[k][:, 0:n], op0=ALU.mult, op1=ALU.mult, reads=[T_, sm], writes=[T_])
        p.op('dve', 'scalar_tensor_tensor', out=ckvn[:, p0:p0 + n], in0=ckv_sb[k][:, 0:n], scalar=kvg[:, 0:1],
             in1=rstdk[k][:, 0:n], op0=ALU.mult, op1=ALU.mult, reads=[T_, sm], adds=[ckvn_t])
        O_ = ot[k]
        oqn3 = v3(oqn[k], 4); oqr3 = v3(oqr[k], 4); okn3 = v3(okn[k], 4)
        for h in range(4):
            b = kb.bank()
            kb.mm(kb.psb(b, n), [(wuq[:, kc, h * 192:h * 192 + 128], cqn[k][kc][:, 0:n]) for kc in range(2)], reads=[wt, T_], bank_tok=kb.pst[b])
            p.op('act', 'activation', out=oqn3[:, h, 0:n], in_=kb.psb(b, n), func=AF.Copy, reads=[kb.pst[b]], adds=[O_])
            b1 = kb.bank()
            kb.mm(kb.psb(b1, n, rows=64), [(wuq[:, kc, h * 192 + 128:h * 192 + 192], cqn[k][kc][:, 0:n]) for kc in range(2)],
                  reads=[wt, T_], bank_tok=kb.pst[b1])
            b2 = kb.bank()
            kb.mm(kb.psb(b2, n, rows=64), [(wuq_rot[:, kc, h, :], cqn[k][kc][:, 0:n]) for kc in range(2)], reads=[wt, T_], bank_tok=kb.pst[b2])
            p.op('dve', 'tensor_tensor', t1[k][0:64, 0:n], kb.psb(b1, n, rows=64), cos[0:64, p0:p0 + n], ALU.mult,
                 reads=[kb.pst[b1], cst_t], writes=[T_])
            p.op('dve', 'tensor_tensor', t2[k][0:64, 0:n], kb.psb(b2, n, rows=64), sin[0:64, p0:p0 + n], ALU.mult,
                 reads=[kb.pst[b2], cst_t], writes=[T_])
            p.op('pool', 'tensor_tensor', oqr3[0:64, h, 0:n], t1[k][0:64, 0:n], t2[k][0:64, 0:n], ALU.add, reads=[T_], adds=[O_])
            b = kb.bank()
            kb.mm(kb.psb(b, n), [(wukv4[:, h, 0:128], ckvn[:, p0:p0 + n])], reads=[wt, ckvn_t], bank_tok=kb.pst[b])
            p.op('act', 'activation', out=okn3[:, h, 0:n], in_=kb.psb(b, n), func=AF.Copy, reads=[kb.pst[b]], adds=[O_])
        b1 = kb.bank()
        kb.mm(kb.psb(b1, n, rows=64), [(w[:, kc, 384:448], hT3[:, kc, p0:p0 + n]) for kc in range(8)], reads=[wt] + kb.hT_all, bank_tok=kb.pst[b1])
        b2 = kb.bank()
        kb.mm(kb.psb(b2, n, rows=64), [(wkr_rot[:, kc, :], hT3[:, kc, p0:p0 + n]) for kc in range(8)], reads=[wt] + kb.hT_all, bank_tok=kb.pst[b2])
        p.op('dve', 'tensor_tensor', t1[k][0:64, 0:n], kb.psb(b1, n, rows=64), cos[0:64, p0:p0 + n], ALU.mult, reads=[kb.pst[b1], cst_t], writes=[T_])
        p.op('dve', 'tensor_tensor', t2[k][0:64, 0:n], kb.psb(b2, n, rows=64), sin[0:64, p0:p0 + n], ALU.mult, reads=[kb.pst[b2], cst_t], writes=[T_])
        p.op('pool', 'tensor_tensor', okr[k][0:64, 0:n], t1[k][0:64, 0:n], t2[k][0:64, 0:n], ALU.add, reads=[T_], adds=[O_])
        sc = kb.scr
        p.dma('sp', sc['mq_n'].rearrange("(h p) t -> p h t", p=128)[:, :, p0:p0 + n], oqn3[:, :, 0:n], reads=[O_], adds=[kb.scr_t['mq_n']])
        p.dma('sp', sc['mq_r'].rearrange("(h p) t -> p h t", p=64)[:, :, p0:p0 + n], oqr3[0:64, :, 0:n], reads=[O_], adds=[kb.scr_t['mq_r']])
        p.dma('sp', sc['mk_n'].rearrange("(h p) t -> p h t", p=128)[:, :, p0:p0 + n], okn3[:, :, 0:n], reads=[O_], adds=[kb.scr_t['mk_n']])
        p.dma('sp', sc['mk_r'][:, p0:p0 + n], okr[k][0:64, 0:n], reads=[O_], adds=[kb.scr_t['mk_r']])
    vb = [ar.bf16(512) for _ in range(2)]; vbt = toks(2, 'vb')
    for j, (k0, nk) in enumerate(KT):
        b = kb.bank()
        kb.mm(kb.psb(b, 512, rows=nk), [(ckvn[:, k0:k0 + nk], wukv4[:, :, 128:256])], reads=[wt, ckvn_t], bank_tok=kb.pst[b])
        p.op('act', 'activation', out=vb[j % 2][0:nk, :], in_=kb.psb(b, 512, rows=nk), func=AF.Copy, reads=[kb.pst[b]], writes=[vbt[j % 2]])
        p.dma('sp', kb.scr['mv'][128 * j:128 * j + nk, :], vb[j % 2][0:nk, :], reads=[vbt[j % 2]], adds=[kb.scr_t['mv']])
    ar.pop()
    ar.pop()
    p.barrier()
    ar.push()
    scale = (128 + 64) ** -0.5
    kr = ar.bf16(TP); krt = Tok('kr')
    p.dma('sp', kr[0:64, :], kb.scr['mk_r'], reads=[kb.scr_t['mk_r']], writes=[krt])
    NH = 2
    kn = [ar.bf16(TP) for _ in range(NH)]; knt = toks(NH, 'kn')
    vh = [ar.bf16(NT * 128) for _ in range(NH)]; vht = toks(NH, 'vh')
    ob = [ar.bf16(512) for _ in range(2)]; obt = toks(2, 'ob')
    rc = [ar.f32(512) for _ in range(2)]
    kb.att_qreads = [kb.scr_t['mq_n'], kb.scr_t['mq_r']]
    for h in range(4):
        k = h % NH
        p.dma('sp', kn[k], kb.scr['mk_n'][h * 128:(h + 1) * 128, :], reads=[kb.scr_t['mk_n']], writes=[knt[k]])
        vv = v3(vh[k], NT)
        p.dma('sp', vv, kb.scr['mv'].rearrange("(j p) c -> p j c", p=128)[:, :, h * 128:(h + 1) * 128], reads=[kb.scr_t['mv']], writes=[vht[k]])
        kb.att_kreads = [knt[k], krt]
        kb.att_vreads = [vht[k]]

        def epi(bi, p0, n, obanks, dbanks, h=h):
            q = bi % 2
            p.op('dve', 'reciprocal', rc[q][:, 0:n], kb.psb(dbanks[0], n), reads=[kb.pst[dbanks[0]]], writes=[obt[q]])
            p.op('dve', 'tensor_tensor', ob[q][:, 0:n], kb.psb(obanks[0], n), rc[q][:, 0:n], ALU.mult, reads=[kb.pst[obanks[0]]], writes=[obt[q]])
            p.dma('sp', kb.scr['brT1'][h * 128:(h + 1) * 128, p0:p0 + n], ob[q][:, 0:n], reads=[obt[q]], adds=[kb.scr_t['brT1']])
        attention(kb, 1, [[(kn[k], 0, 128), (kr, 0, 64)]],
                  [[(kb.scr['mq_n'][h * 128:(h + 1) * 128, :], 0, 128), (kb.scr['mq_r'][h * 64:(h + 1) * 64, :], 0, 64)]],
                  vv, scale, epi)
    ar.pop()
    p.barrier()


def stage_diff(kb, l):
    p, ar = kb.p, kb.ar
    d = kb.din
    for nm, shp, dt in [('dq', [512, TP], BF16), ('dk', [512, TP], BF16), ('dv', [NT * 128, 512], BF16)]:
        if nm not in kb.scr:
            kb.scratch(nm, shp, dt)
    lam_init = 0.8 - 0.6 * math.exp(-0.3 * l)
    ar.push()
    wt = Tok('diff_w')
    cst_t = Tok('cs')
    sin, cos = load_cs(kb, 128, cst_t)
    hT3 = v3(kb.hT, 8)
    t1 = [ar.f32(512) for _ in range(2)]; t2 = [ar.f32(512) for _ in range(2)]
    oq = [ar.bf16(512) for _ in range(2)]
    tt = toks(2, 'difft'); ot = toks(2, 'diffo')
    cnt = 0
    for which, c0, dst in (('q', 3024, 'dq'), ('k', 3536, 'dk')):
        ar.push()
        w = v3(ar.bf16(8 * 512), 8)
        w_t = Tok('dw' + which)
        load_w_cols(kb, w, d['w_in'][l], c0, c0 + 512, w_t)
        wr = v3(ar.bf16(8 * 512), 8)
        make_rot(kb, wr.rearrange("p c (m x) -> p c m x", x=64), w.rearrange("p c (m x) -> p c m x", x=64), [w_t], w_t)
        for h in range(4):
            for (p0, n) in BLOCKS:
                k = cnt % 2
                cnt += 1
                b1 = kb.bank()
                kb.mm(kb.psb(b1, n), [(w[:, kc, h * 128:(h + 1) * 128], hT3[:, kc, p0:p0 + n]) for kc in range(8)],
                      reads=[w_t] + kb.hT_all, bank_tok=kb.pst[b1])
                b2 = kb.bank()
                kb.mm(kb.psb(b2, n), [(wr[:, kc, h * 128:(h + 1) * 128], hT3[:, kc, p0:p0 + n]) for kc in range(8)],
                      reads=[w_t] + kb.hT_all, bank_tok=kb.pst[b2])
                p.op('dve', 'tensor_tensor', t1[k][:, 0:n], kb.psb(b1, n), cos[:, p0:p0 + n], ALU.mult, reads=[kb.pst[b1], cst_t], writes=[tt[k]])
                p.op('dve', 'tensor_tensor', t2[k][:, 0:n], kb.psb(b2, n), sin[:, p0:p0 + n], ALU.mult, reads=[kb.pst[b2], cst_t], writes=[tt[k]])
                p.op('pool', 'tensor_tensor', oq[k][:, 0:n], t1[k][:, 0:n], t2[k][:, 0:n], ALU.add, reads=[tt[k]], writes=[ot[k]])
                p.dma('sp', kb.scr[dst][h * 128:(h + 1) * 128, p0:p0 + n], oq[k][:, 0:n], reads=[ot[k]], adds=[kb.scr_t[dst]])
        ar.pop()
        p.barrier()
    ar.push()
    w = v3(ar.bf16(8 * 512), 8); w_t = Tok('dwv')
    load_w_cols(kb, w, d['w_in'][l], 4048, 4560, w_t)
    vb = [ar.bf16(512) for _ in range(2)]; vbt = toks(2, 'dvb')
    for j, (k0, nk) in enumerate(KT):
        b = kb.bank()
        kb.mm(kb.psb(b, 512, rows=nk), [(hT3[:, kc, k0:k0 + nk], w[:, kc, :]) for kc in range(8)], reads=[w_t] + kb.hT_all, bank_tok=kb.pst[b])
        p.op('act', 'activation', out=vb[j % 2][0:nk, :], in_=kb.psb(b, 512, rows=nk), func=AF.Copy, reads=[kb.pst[b]], writes=[vbt[j % 2]])
        p.dma('sp', kb.scr['dv'][128 * j:128 * j + nk, :], vb[j % 2][0:nk, :], reads=[vbt[j % 2]], adds=[kb.scr_t['dv']])
    ar.pop()
    ar.pop()
    p.barrier()
    ar.push()
    sm = Tok('diff_small')
    lv = ar.f32(256)
    bcast_row(kb, lv, d['diff_lambda'][l].rearrange("a b -> (a b)"), 256, sm)
    pr = ar.f32(64); e1 = ar.f32(1); e2 = ar.f32(1); neglam = ar.f32(1); gsc = ar.f32(1)
    p.op('dve', 'tensor_tensor', pr, lv[:, 0:64], lv[:, 64:128], ALU.mult, reads=[sm], writes=[sm])
    p.op('dve', 'reduce_sum', e1, pr, AX.X, reads=[sm], writes=[sm])
    p.op('dve', 'tensor_tensor', pr, lv[:, 128:192], lv[:, 192:256], ALU.mult, reads=[sm], writes=[sm])
    p.op('dve', 'reduce_sum', e2, pr, AX.X, reads=[sm], writes=[sm])
    p.op('act', 'activation', out=e1, in_=e1, func=AF.Exp, reads=[sm], writes=[sm])
    p.op('act', 'activation', out=e2, in_=e2, func=AF.Exp, reads=[sm], writes=[sm])
    p.op('dve', 'tensor_tensor', neglam, e2, e1, ALU.subtract, reads=[sm], writes=[sm])
    p.op('dve', 'tensor_scalar', neglam, neglam, -lam_init, None, ALU.add, reads=[sm], writes=[sm])
    load_cols(kb, gsc, d['diff_norm_g'][l], sm)
    p.op('dve', 'tensor_scalar', gsc, gsc, 1.0 - lam_init, None, ALU.mult, reads=[sm], writes=[sm])
    scale = 64 ** -0.5
    NH = 2
    kh = [ar.bf16(TP) for _ in range(NH)]; kht = toks(NH, 'dkh')
    vh = [ar.bf16(NT * 128) for _ in range(NH)]; vht = toks(NH, 'dvh')
    r1 = ar.f32(512); r2 = ar.f32(512); o1 = ar.f32(512); o2 = ar.f32(512); sq = ar.f32(512); rs = ar.f32(512)
    ob = [ar.bf16(512) for _ in range(2)]; obt = toks(2, 'dob')
    et = Tok('depi')
    kb.att_qreads = [kb.scr_t['dq']]
    for h in range(4):
        k = h % NH
        p.dma('sp', kh[k], kb.scr['dk'][h * 128:(h + 1) * 128, :], reads=[kb.scr_t['dk']], writes=[kht[k]])
        vv = v3(vh[k], NT)
        p.dma('sp', vv, kb.scr['dv'].rearrange("(j p) c -> p j c", p=128)[:, :, h * 128:(h + 1) * 128], reads=[kb.scr_t['dv']], writes=[vht[k]])
        kb.att_kreads = [kht[k]]
        kb.att_vreads = [vht[k]]

        def epi(bi, p0, n, obanks, dbanks, h=h):
            q = bi % 2
            N = slice(0, n)
            p.op('dve', 'reciprocal', r1[:, N], kb.psb(dbanks[0], n), reads=[kb.pst[dbanks[0]]], writes=[et])
            p.op('dve', 'reciprocal', r2[:, N], kb.psb(dbanks[1], n), reads=[kb.pst[dbanks[1]]], writes=[et])
            p.op('dve', 'tensor_tensor', o1[:, N], kb.psb(obanks[0], n), r1[:, N], ALU.mult, reads=[kb.pst[obanks[0]], et], writes=[et])
            p.op('dve', 'tensor_scalar', r2[:, N], r2[:, N], neglam[:, 0:1], None, ALU.mult, reads=[et, sm], writes=[et])
            p.op('dve', 'tensor_tensor', o2[:, N], kb.psb(obanks[1], n), r2[:, N], ALU.mult, reads=[kb.pst[obanks[1]], et], writes=[et])
            p.op('dve', 'tensor_tensor', o1[:, N], o1[:, N], o2[:, N], ALU.add, reads=[et], writes=[et])
            p.op('act', 'activation', out=sq[:, N], in_=o1[:, N], func=AF.Square, reads=[et], writes=[et])
            sb = 0
            kb.mm(kb.psb(sb, n), [(kb.ones, sq[:, N])], reads=[et, kb.cst], bank_tok=kb.pst[sb])
            rms_from_ss(kb, rs[:, N], kb.psb(sb, n), 1.0 / 128, 1e-6, [kb.pst[sb]], et)
            p.op('dve', 'scalar_tensor_tensor', out=ob[q][:, N], in0=o1[:, N], scalar=gsc[:, 0:1], in1=rs[:, N], op0=ALU.mult, op1=ALU.mult,
                 reads=[et, sm], writes=[obt[q]])
            p.dma('sp', kb.scr['brT3'][h * 128:(h + 1) * 128, p0:p0 + n], ob[q][:, N], reads=[obt[q]], adds=[kb.scr_t['brT3']])
        qd = kb.scr['dq'][h * 128:(h + 1) * 128, :]
        attention(kb, 2, [[(kh[k], 0, 64)], [(kh[k], 64, 64)]], [[(qd[0:64, :], 0, 64)], [(qd[64:128, :], 64, 64)]], vv, scale, epi)
    ar.pop()
    p.barrier()


def bc_mid(ap, k):
    return ap.unsqueeze(1).broadcast_to([ap.shape[0], k, ap.shape[1]])


def bc_last(ap, n):
    return ap.unsqueeze(2).broadcast_to([ap.shape[0], ap.shape[1], n])


def stage_gdn(kb, l):
    p, ar = kb.p, kb.ar
    d = kb.din
    for nm in ('gq', 'gk', 'gv'):
        if nm not in kb.scr:
            kb.scratch(nm, [512, TP], F32)
    if 'gof' not in kb.scr:
        kb.scratch('gof', [TP, 512], F32)
    hT3 = v3(kb.hT, 8)
    ar.push()
    gall = v3(ar.f32(NT * 8), NT); lball = v3(ar.f32(NT * 8), NT); betall = v3(ar.f32(NT * 8), NT)
    sc_t = Tok('gdn_sc')
    ar.push()
    wt = Tok('gdn_w')
    w = v3(ar.bf16(8 * 1536), 8)
    load_w_cols(kb, w, d['w_in'][l], 0, 1536, wt)
    wab = v3(ar.bf16(8 * 16), 8)
    load_w_cols(kb, wab, d['w_in'][l], 2048, 2064, wt)
    sm = Tok('gdn_small')
    convw = v3(ar.f32(60), 12)
    for k in range(5):
        load_cols(kb, convw[:, :, k], d['gdn_conv_w'][l, k], sm)
    dtb = ar.f32(8); alog = ar.f32(8)
    bcast_row(kb, dtb, d['gdn_dt_bias'][l].rearrange("a b -> (a b)"), 8, sm)
    bcast_row(kb, alog, d['gdn_a_log'][l].rearrange("a b -> (a b)"), 8, sm)
    p.op('act', 'activation', out=alog, in_=alog, func=AF.Exp, reads=[sm], writes=[sm])
    p.op('dve', 'tensor_scalar', alog, alog, -1.0, None, ALU.mult, reads=[sm], writes=[sm])
    xa = ar.f32(8); tmp8 = ar.f32(8); nb = ar.f32(8); xt_ = Tok('gdn_x')
    for i in range(NT):
        b = kb.bank()
        kb.mm(kb.psb(b, 16), [(hT3[:, kc, 128 * i:128 * i + 128], wab[:, kc, :]) for kc in range(8)], reads=[wt] + kb.hT_all, bank_tok=kb.pst[b])
        p.op('dve', 'tensor_tensor', xa, kb.psb(b, 8), dtb, ALU.add, reads=[kb.pst[b], sm], writes=[xt_])
        p.op('dve', 'tensor_scalar', nb, kb.psb(b, 8, off=8), -1.0, None, ALU.mult, reads=[kb.pst[b]], writes=[xt_])
        softplus_small(kb, gall[:, i, :], xa, tmp8, xt_)
        p.op('dve', 'tensor_tensor', gall[:, i, :], gall[:, i, :], alog, ALU.mult, reads=[xt_, sm], writes=[xt_])
        softplus_small(kb, lball[:, i, :], nb, tmp8, xt_)
        p.op('dve', 'tensor_scalar', lball[:, i, :], lball[:, i, :], -1.0, None, ALU.mult, reads=[xt_], writes=[xt_])
        p.op('act', 'activation', out=betall[:, i, :], in_=lball[:, i, :], func=AF.Exp, reads=[xt_], writes=[xt_])
    for (tile, lo, hi) in ((0, 0, P0), (NT - 1, 64, 128)):
        p.op('pool', 'memset', gall[lo:hi, tile, :], 0.0, reads=[xt_], writes=[xt_])
        p.op('pool', 'memset', betall[lo:hi, tile, :], 0.0, reads=[xt_], writes=[xt_])
        p.op('pool', 'memset', lball[lo:hi, tile, :], -100.0, reads=[xt_], writes=[xt_])
    p.op('pool', 'tensor_copy', gall[:, 0, 0:1], gall[:, 0, 0:1], reads=[xt_], writes=[sc_t])
    V = slice(P0, P0 + T)
    NBUF = 2
    pre = [ar.f32(TP) for _ in range(NBUF)]; pre_t = toks(NBUF, 'gpre')
    cv = [ar.f32(TP) for _ in range(NBUF)]; cv_t = toks(NBUF, 'gcv')
    sq = [ar.f32(512) for _ in range(2)]; rn = [ar.f32(512) for _ in range(2)]; nt_ = toks(2, 'gnrm')
    for k in range(NBUF):
        p.op('pool', 'memset', pre[k], 0.0, writes=[pre_t[k]])
        p.op('pool', 'memset', cv[k], 0.0, writes=[cv_t[k]])
    cnt = 0
    for c in range(12):
        k = c % NBUF
        PR, CV = pre[k], cv[k]
        for (p0, n) in BLOCKS:
            b = kb.bank()
            kb.mm(kb.psb(b, n), [(w[:, kc, c * 128:(c + 1) * 128], hT3[:, kc, p0:p0 + n]) for kc in range(8)],
                  reads=[wt] + kb.hT_all, bank_tok=kb.pst[b])
            p.op('act', 'activation', out=PR[:, p0:p0 + n], in_=kb.psb(b, n), func=AF.Copy, reads=[kb.pst[b]], adds=[pre_t[k]])
        cvv = CV[:, V]
        p.op('act', 'activation', out=cvv, in_=PR[:, P0 - 2:P0 - 2 + T], func=AF.Identity, scale=convw[:, c, 0:1], reads=[pre_t[k], sm], writes=[cv_t[k]])
        for kk in range(1, 5):
            p.op('dve', 'scalar_tensor_tensor', out=cvv, in0=PR[:, P0 - 2 + kk:P0 - 2 + kk + T], scalar=convw[:, c, kk:kk + 1],
                 in1=cvv, op0=ALU.mult, op1=ALU.add, reads=[pre_t[k], sm], writes=[cv_t[k]])
        p.op('act', 'activation', out=cvv, in_=cvv, func=AF.Silu, reads=[cv_t[k]], writes=[cv_t[k]])
        if c < 8:
            qscale = (128 ** -0.5) if c < 4 else 1.0
            for (p0, n) in BLOCKS:
                j = cnt % 2
                cnt += 1
                p.op('act', 'activation', out=sq[j][:, 0:n], in_=CV[:, p0:p0 + n], func=AF.Square, reads=[cv_t[k]], writes=[nt_[j]])
                b = kb.bank()
                kb.mm(kb.psb(b, n), [(kb.ones, sq[j][:, 0:n])], reads=[nt_[j], kb.cst], bank_tok=kb.pst[b])
                rms_from_ss(kb, rn[j][:, 0:n], kb.psb(b, n), 1.0, 1e-6, [kb.pst[b]], nt_[j])
                p.op('dve', 'scalar_tensor_tensor', out=CV[:, p0:p0 + n], in0=CV[:, p0:p0 + n], scalar=qscale, in1=rn[j][:, 0:n],
                     op0=ALU.mult, op1=ALU.mult, reads=[nt_[j]], writes=[cv_t[k]])
        dst = ('gq', 'gk', 'gv')[c // 4]
        hh = c % 4
        p.dma('sp', kb.scr[dst][hh * 128:(hh + 1) * 128, :], CV, reads=[cv_t[k]], adds=[kb.scr_t[dst]])
    ar.pop()
    p.barrier()
    if 'gdnA' in kb.debug:
        ar.pop()
        return
    cc = Tok('gdn_c')
    def ld(nm, n=128):
        a = ar.f32(n)
        p.dma('sp', a, d['c_' + nm], adds=[cc])
        return a
    mf = ld('mf'); mb = ld('mb'); ups = ld('up_s', 512); los = ld('lo_s', 512); upi = ld('up_i', 512); loi = ld('lo_i', 512)
    negones = ar.f32(128); negmf = ar.f32(128); negmb = ar.f32(128); ident4 = ar.f32(512)
    p.op('dve', 'tensor_scalar', negones, kb.ones, -1.0, None, ALU.mult, reads=[kb.cst], adds=[cc])
    p.op('dve', 'tensor_scalar', negmf, mf, -1.0, None, ALU.mult, reads=[cc], adds=[cc])
    p.op('dve', 'tensor_scalar', negmb, mb, -1.0, None, ALU.mult, reads=[cc], adds=[cc])
    p.op('dve', 'tensor_copy', v3(ident4, 4), bc_mid(kb.ident, 4), reads=[kb.cst], adds=[cc])
    wz = v3(ar.bf16(8 * 512), 8); wz_t = Tok('wz')
    load_w_cols(kb, wz, d['w_in'][l], 1536, 2048, wz_t)
    gn4 = ar.f32(512)
    for hh in range(4):
        bcast_row(kb, gn4[:, hh * 128:(hh + 1) * 128], d['gdn_norm_g'][l], 128, wz_t)
    F = lambda: ar.f32(512)
    B = lambda: ar.bf16(512)
    qT, kT, vT = F(), F(), F(); in_t = Tok('g_in')
    qb_, kb_ = B(), B(); inb_t = Tok('g_inb')
    ktok, vtok, bv = F(), F(), F(); tk_t = Tok('g_tok')
    GM, LBI, NG, LBb = F(), F(), F(), F(); bt_ = Tok('g_build')
    Ea, Eb, Ec, EG = F(), F(), F(), F(); e_t = Tok('g_exp')
    Lm, Ltm, X = F(), F(), F(); l_t = Tok('g_L'); x_t = Tok('g_X')
    Pp = [F(), F()]; Qq = [F(), F()]; pq_t = [Tok('g_P0'), Tok('g_P1')]
    attnT, Tt, Rp, vnew, qdT, kdec = B(), B(), B(), B(), B(), B()
    a_t, tt_t, r_t, vn_t, qd_t, kd_t = Tok('g_at'), Tok('g_Tt'), Tok('g_R'), Tok('g_vn'), Tok('g_qd'), Tok('g_kd')
    smalls = ar.f32(32); s_t = Tok('g_small')
    S = F(); Sb = B(); S_t = Tok('g_S'); Sb_t = Tok('g_Sb')
    ot = [F(), F()]; ot_t = toks(2, 'g_o')
    of_ = F(); of_t = Tok('g_of')
    zt = F(); z_t = Tok('g_z')
    ss4 = ar.f32(8); ob4 = B(); ob_t = Tok('g_ob')
    H4 = lambda a: v3(a, 4)
    for dr in range(2):
        M_, negM, NEGs_ji, NEGs_ij, NEGi_ji = (mf, negmf, ups, los, upi) if dr == 0 else (mb, negmb, los, ups, loi)
        last = 127 if dr == 0 else 0
        p.op('pool', 'memset', S, 0.0, writes=[S_t])
        p.op('pool', 'memset', Sb, 0.0, writes=[Sb_t])
        order = range(NT) if dr == 0 else range(NT - 1, -1, -1)
        for step, i in enumerate(order):
            cols = slice(128 * i, 128 * i + 128)
            for nm, dst in (('gq', qT), ('gk', kT), ('gv', vT)):
                p.dma('sp', H4(dst), kb.scr[nm].rearrange("(h p) t -> p h t", p=128)[:, :, cols], reads=[kb.scr_t[nm]], adds=[in_t])
            p.op('pool', 'tensor_copy', qb_, qT, reads=[in_t], writes=[inb_t])
            p.op('pool', 'tensor_copy', kb_, kT, reads=[in_t], adds=[inb_t])
            g4 = gall[:, i, dr * 4:dr * 4 + 4]; lb4 = lball[:, i, dr * 4:dr * 4 + 4]; be4 = betall[:, i, dr * 4:dr * 4 + 4]
            b1 = kb.bank()
            p.group('pe', [('transpose', (kb.psb(b1, 128, off=hh * 128), kT[:, hh * 128:(hh + 1) * 128], kb.ident), {}) for hh in range(4)],
                    reads=[in_t, kb.cst], writes=[kb.pst[b1]])
            b2 = kb.bank()
            p.group('pe', [('transpose', (kb.psb(b2, 128, off=hh * 128), vT[:, hh * 128:(hh + 1) * 128], kb.ident), {}) for hh in range(4)],
                    reads=[in_t, kb.cst], writes=[kb.pst[b2]])
            p.op('act', 'activation', out=ktok, in_=kb.psb(b1), func=AF.Copy, reads=[kb.pst[b1]], writes=[tk_t])
            p.op('dve', 'tensor_tensor', H4(bv), H4(kb.psb(b2)), bc_last(be4, 128), ALU.mult, reads=[kb.pst[b2], sc_t], adds=[tk_t])
            if kb.gdn_stop == 1:
                ar.pop(); p.barrier(); return
            p.op('pool', 'tensor_tensor', H4(GM), bc_mid(M_, 4), bc_last(g4, 128), ALU.mult, reads=[cc, sc_t], writes=[bt_])
            p.op('pool', 'tensor_tensor', H4(LBI), bc_mid(kb.ident, 4), bc_last(lb4, 128), ALU.mult, reads=[kb.cst, sc_t], adds=[bt_])
            p.op('pool', 'tensor_tensor', H4(NG), bc_mid(negones, 4), bc_last(g4, 128), ALU.mult, reads=[cc, sc_t], adds=[bt_])
            p.op('pool', 'tensor_tensor', H4(LBb), bc_mid(kb.ones, 4), bc_last(lb4, 128), ALU.mult, reads=[kb.cst, sc_t], adds=[bt_])
            if kb.gdn_stop == 2:
                ar.pop(); p.barrier(); return
            bg = kb.bank()
            kb.mm(kb.psb(bg, 4), [(M_, g4)], reads=[cc, sc_t], bank_tok=kb.pst[bg])
            p.op('dve', 'tensor_copy', smalls[:, 0:4], kb.psb(bg, 4), reads=[kb.pst[bg]], writes=[s_t])
            p.op('act', 'activation', out=smalls[:, 4:8], in_=smalls[:, 0:4], func=AF.Exp, reads=[s_t], writes=[s_t])
            p.op('dve', 'scalar_tensor_tensor', out=smalls[:, 8:12], in0=smalls[:, 4:8], scalar=-1.0, in1=be4, op0=ALU.mult, op1=ALU.mult,
                 reads=[s_t, sc_t], writes=[s_t])
            if kb.gdn_stop == 3:
                ar.pop(); p.barrier(); return
            ba_ = kb.bank()
            kb.mm(kb.psb(ba_), [(kb.ones, GM), (kb.ones, LBI), (M_, NG), (kb.ident, NEGs_ji)], reads=[bt_, cc, kb.cst], bank_tok=kb.pst[ba_])
            p.op('act', 'activation', out=Ea, in_=kb.psb(ba_), func=AF.Exp, reads=[kb.pst[ba_]], writes=[e_t])
            bb_ = kb.bank()
            kb.mm(kb.psb(bb_), [(negM, NG), (kb.ident, LBb), (negones, GM), (kb.ident, NEGs_ij)], reads=[bt_, cc, kb.cst], bank_tok=kb.pst[bb_])
            p.op('act', 'activation', out=Eb, in_=kb.psb(bb_), func=AF.Exp, reads=[kb.pst[bb_]], adds=[e_t])
            bc_ = kb.bank()
            kb.mm(kb.psb(bc_), [(kb.ones, GM), (M_, NG), (kb.ident, NEGi_ji)], reads=[bt_, cc, kb.cst], bank_tok=kb.pst[bc_])
            p.op('act', 'activation', out=Ec, in_=kb.psb(bc_), func=AF.Exp, reads=[kb.pst[bc_]], adds=[e_t])
            bd_ = kb.bank()
            kb.mm(kb.psb(bd_), [(kb.ones, GM)], reads=[bt_, kb.cst], bank_tok=kb.pst[bd_])
            p.op('act', 'activation', out=EG, in_=kb.psb(bd_), func=AF.Exp, reads=[kb.pst[bd_]], adds=[e_t])
            bgl = kb.bank()
            kb.mm(kb.psb(bgl, 4), [(kb.ones, g4)], reads=[kb.cst, sc_t], bank_tok=kb.pst[bgl])
            p.op('dve', 'tensor_tensor', smalls[:, 12:16], kb.psb(bgl, 4), smalls[:, 0:4], ALU.subtract, reads=[kb.pst[bgl], s_t], writes=[s_t])
            p.op('act', 'activation', out=smalls[:, 16:20], in_=kb.psb(bgl, 4), func=AF.Exp, reads=[kb.pst[bgl]], writes=[s_t])
            p.op('act', 'activation', out=smalls[:, 12:16], in_=smalls[:, 12:16], func=AF.Exp, reads=[s_t], writes=[s_t])
            if kb.gdn_stop == 4:
                ar.pop(); p.barrier(); return
            bk = kb.bank()
            p.group('pe', [('matmul', (kb.psb(bk, 128, off=hh * 128), kb_[:, hh * 128:(hh + 1) * 128], kb_[:, hh * 128:(hh + 1) * 128]), dict(start=True, stop=True))
                           for hh in range(4)], reads=[inb_t], writes=[kb.pst[bk]])
            bq = kb.bank()
            p.group('pe', [('matmul', (kb.psb(bq, 128, off=hh * 128), kb_[:, hh * 128:(hh + 1) * 128], qb_[:, hh * 128:(hh + 1) * 128]), dict(start=True, stop=True))
                           for hh in range(4)], reads=[inb_t], writes=[kb.pst[bq]])
            p.op('dve', 'tensor_tensor', Ltm, kb.psb(bk), Ea, ALU.mult, reads=[kb.pst[bk], e_t], writes=[l_t])
            p.op('dve', 'tensor_tensor', Lm, kb.psb(bk), Eb, ALU.mult, reads=[kb.pst[bk], e_t], adds=[l_t])
            p.op('dve', 'tensor_tensor', attnT, kb.psb(bq), Ec, ALU.mult, reads=[kb.pst[bq], e_t], writes=[a_t])
            p.op('pool', 'tensor_tensor', X, ident4, Ltm, ALU.subtract, reads=[cc, l_t], writes=[x_t])
            if kb.gdn_stop == 5:
                ar.pop(); p.barrier(); return
            Pc, Qc, pqc = Lm, Ltm, l_t
            for lev in range(6):
                Pn, Qn, pqn = Pp[lev % 2], Qq[lev % 2], pq_t[lev % 2]
                b_p = kb.bank()
                p.group('pe', [('matmul', (kb.psb(b_p, 128, off=hh * 128), Qc[:, hh * 128:(hh + 1) * 128], Pc[:, hh * 128:(hh + 1) * 128]), dict(start=True, stop=True))
                               for hh in range(4)], reads=[pqc], writes=[kb.pst[b_p]])
                p.op('act', 'activation', out=Pn, in_=kb.psb(b_p), func=AF.Copy, reads=[kb.pst[b_p]], writes=[pqn])
                if lev < 5:
                    b_q = kb.bank()
                    p.group('pe', [('matmul', (kb.psb(b_q, 128, off=hh * 128), Pc[:, hh * 128:(hh + 1) * 128], Qc[:, hh * 128:(hh + 1) * 128]), dict(start=True, stop=True))
                                   for hh in range(4)], reads=[pqc], writes=[kb.pst[b_q]])
                    p.op('dve', 'tensor_copy', Qn, kb.psb(b_q), reads=[kb.pst[b_q]], adds=[pqn])
                b_x = kb.bank()
                p.group('pe', [('matmul', (kb.psb(b_x, 128, off=hh * 128), Pn[:, hh * 128:(hh + 1) * 128], X[:, hh * 128:(hh + 1) * 128]), dict(start=True, stop=True))
                               for hh in range(4)], reads=[pqn, x_t], writes=[kb.pst[b_x]])
                p.op('dve', 'tensor_tensor', X, X, kb.psb(b_x), ALU.add, reads=[kb.pst[b_x]], writes=[x_t])
                Pc, Qc, pqc = Pn, Qn, pqn
            p.op('act', 'activation', out=Tt, in_=X, func=AF.Copy, reads=[x_t], writes=[tt_t])
            if kb.gdn_stop == 6:
                ar.pop(); p.barrier(); return
            p.op('dve', 'tensor_tensor', qdT, qT, EG, ALU.mult, reads=[in_t, e_t], writes=[qd_t])
            p.op('pool', 'tensor_tensor', H4(kdec), H4(ktok), bc_last(smalls[:, 12:16], 128), ALU.mult, reads=[tk_t, s_t], writes=[kd_t])
            if kb.gdn_stop == 7:
                ar.pop(); p.barrier(); return
            b_ks = kb.bank()
            p.group('pe', [('matmul', (kb.psb(b_ks, 128, off=hh * 128), kb_[:, hh * 128:(hh + 1) * 128], Sb[:, hh * 128:(hh + 1) * 128]), dict(start=True, stop=True))
                           for hh in range(4)], reads=[inb_t, Sb_t], writes=[kb.pst[b_ks]])
            p.op('dve', 'tensor_tensor', H4(Rp), H4(kb.psb(b_ks)), bc_last(smalls[:, 8:12], 128), ALU.mult, reads=[kb.pst[b_ks], s_t], writes=[r_t])
            p.op('pool', 'tensor_tensor', Rp, Rp, bv, ALU.add, reads=[tk_t], writes=[r_t])
            b_vn = kb.bank()
            p.group('pe', [('matmul', (kb.psb(b_vn, 128, off=hh * 128), Tt[:, hh * 128:(hh + 1) * 128], Rp[:, hh * 128:(hh + 1) * 128]), dict(start=True, stop=True))
                           for hh in range(4)], reads=[tt_t, r_t], writes=[kb.pst[b_vn]])
            p.op('act', 'activation', out=vnew, in_=kb.psb(b_vn), func=AF.Copy, reads=[kb.pst[b_vn]], writes=[vn_t])
            b_o = kb.bank()
            fns = []
            for hh in range(4):
                hs = slice(hh * 128, (hh + 1) * 128)
                fns.append(('matmul', (kb.psb(b_o, 128, off=hh * 128), qdT[:, hs], Sb[:, hs]), dict(start=True, stop=False)))
                fns.append(('matmul', (kb.psb(b_o, 128, off=hh * 128), attnT[:, hs], vnew[:, hs]), dict(start=False, stop=True)))
            p.group('pe', fns, reads=[qd_t, Sb_t, a_t, vn_t], writes=[kb.pst[b_o]])
            b_su = kb.bank()
            p.group('pe', [('matmul', (kb.psb(b_su, 128, off=hh * 128), kdec[:, hh * 128:(hh + 1) * 128], vnew[:, hh * 128:(hh + 1) * 128]), dict(start=True, stop=True))
                           for hh in range(4)], reads=[kd_t, vn_t], writes=[kb.pst[b_su]])
            for hh in range(4):
                hs = slice(hh * 128, (hh + 1) * 128)
                p.op('dve', 'scalar_tensor_tensor', out=S[:, hs], in0=S[:, hs], scalar=smalls[:, 16 + hh:17 + hh], in1=kb.psb(b_su, 128, off=hh * 128),
                     op0=ALU.mult, op1=ALU.add, reads=[kb.pst[b_su], s_t], writes=[S_t])
            p.op('act', 'activation', out=Sb, in_=S, func=AF.Copy, reads=[S_t], writes=[Sb_t])
            if kb.gdn_stop == 8:
                ar.pop(); p.barrier(); return
            O_ = ot[step % 2]; O_t = ot_t[step % 2]
            if dr == 0:
                p.op('act', 'activation', out=O_, in_=kb.psb(b_o), func=AF.Copy, reads=[kb.pst[b_o]], writes=[O_t])
                p.dma('sp', kb.scr['gof'][128 * i:128 * i + 128, :], O_, reads=[O_t], adds=[kb.scr_t['gof']])
            else:
                p.dma('sp', of_, kb.scr['gof'][128 * i:128 * i + 128, :], reads=[kb.scr_t['gof']], writes=[of_t])
                p.op('dve', 'tensor_tensor', O_, kb.psb(b_o), of_, ALU.add, reads=[kb.pst[b_o], of_t], writes=[O_t])
                bz = kb.bank()
                kb.mm(kb.psb(bz), [(hT3[:, kc, cols], wz[:, kc, :]) for kc in range(8)], reads=[wz_t] + kb.hT_all, bank_tok=kb.pst[bz])
                p.op('act', 'activation', out=zt, in_=kb.psb(bz), func=AF.Silu, reads=[kb.pst[bz]], writes=[z_t])
                for hh in range(4):
                    hs = slice(hh * 128, (hh + 1) * 128)
                    p.op('act', 'activation', out=of_[:, hs], in_=O_[:, hs], func=AF.Square, accum_out=ss4[:, hh:hh + 1], reads=[O_t, of_t], writes=[of_t])
                p.op('dve', 'tensor_scalar', ss4[:, 4:8], ss4[:, 0:4], 1.0 / 128, 1e-6, ALU.mult, ALU.add, reads=[of_t], writes=[of_t])
                p.op('act', 'activation', out=ss4[:, 4:8], in_=ss4[:, 4:8], func=AF.Sqrt, reads=[of_t], writes=[of_t])
                p.op('dve', 'reciprocal', ss4[:, 4:8], ss4[:, 4:8], reads=[of_t], writes=[of_t])
                p.op('dve', 'tensor_tensor', H4(O_), H4(O_), bc_last(ss4[:, 4:8], 128), ALU.mult, reads=[of_t], writes=[O_t])
                p.op('pool', 'tensor_tensor', O_, O_, gn4, ALU.mult, reads=[wz_t], writes=[O_t])
                p.op('pool', 'tensor_tensor', O_, O_, zt, ALU.mult, reads=[z_t], writes=[O_t])
                bt2 = kb.bank()
                p.group('pe', [('transpose', (kb.psb(bt2, 128, off=hh * 128), O_[:, hh * 128:(hh + 1) * 128], kb.ident), {}) for hh in range(4)],
                        reads=[O_t, kb.cst], writes=[kb.pst[bt2]])
                p.op('act', 'activation', out=ob4, in_=kb.psb(bt2), func=AF.Copy, reads=[kb.pst[bt2]], writes=[ob_t])
                lo, hi = 0, 128
                if i == 0:
                    lo = P0
                if i == NT - 1:
                    hi = 64
                p.dma('sp', kb.scr['brT0'].rearrange("(h p) t -> p h t", p=128)[:, :, 128 * i + lo:128 * i + hi], H4(ob4)[:, :, lo:hi],
                      reads=[ob_t], adds=[kb.scr_t['brT0']])
    ar.pop()
    p.barrier()


MBLOCKS = [(512 * b, 512) for b in range(8)] + [(4096, 128)]


def stage_merge(kb, l):
    import os
    p, ar = kb.p, kb.ar
    d = kb.din
    if 'mT' not in kb.scr:
        kb.scratch('mT', [1024, TP], BF16)
    hT3 = v3(kb.hT, 8)
    ar.push()
    zp = ar.bf16(64); zp_t = Tok('zp')
    p.op('pool', 'memset', zp, 0.0, writes=[zp_t])
    if os.environ.get('MERGE_STOP') != 'nopad':
        for c in range(8):
            p.dma('sp', kb.scr['mT'][c * 128:(c + 1) * 128, 0:P0], zp[:, 0:P0], reads=[zp_t], adds=[kb.scr_t['mT']])
            p.dma('sp', kb.scr['mT'][c * 128:(c + 1) * 128, P0 + T:TP], zp[:, 0:64], reads=[zp_t], adds=[kb.scr_t['mT']])
    wg = ar.bf16(4 * 8 * 512).rearrange("p (b c n) -> p b c n", b=4, c=8)
    wb = ar.bf16(4 * 4 * 512).rearrange("p (b c n) -> p b c n", b=4, c=4)
    wg_t = Tok('wg'); wb_t = Tok('wb')
    NB = 2
    brb = [[v3(ar.bf16(4 * 512), 4) for _ in range(4)] for _ in range(NB)]; brb_t = toks(NB, 'brb')
    sg = [ar.f32(512) for _ in range(2)]; tm = [ar.f32(512) for _ in range(2)]; sg_t = toks(2, 'sg')
    macc = [ar.f32(512) for _ in range(2)]; macc_t = toks(2, 'macc')
    mout = [v3(ar.bf16(4 * 512), 4) for _ in range(NB)]; mout_t = toks(NB, 'mout')
    cnt = 0
    fcnt = 0
    for g in range(2):
        for br in range(4):
            for kc in range(8):
                p.dma('pool', wg[:, br, kc, :], d['w_gate'][l, br, kc * 128:(kc + 1) * 128, g * 512:(g + 1) * 512], adds=[wg_t])
            for kc in range(4):
                p.dma('pool', wb[:, br, kc, :], d['w_branch'][l, br, kc * 128:(kc + 1) * 128, g * 512:(g + 1) * 512], adds=[wb_t])
        import os
        if os.environ.get('MERGE_STOP') == 'a':
            continue
        for bi, (p0, n) in enumerate(BLOCKS):
            k = bi % NB
            if os.environ.get('MERGE_STOP') == 'b' and bi > 0:
                continue
            for br in range(4):
                p.dma('sp', brb[k][br][:, :, 0:n], kb.scr[f'brT{br}'].rearrange("(c p) t -> p c t", p=128)[:, :, p0:p0 + n],
                      reads=[kb.scr_t[f'brT{br}']], adds=[brb_t[k]])
            for fc in range(4):
                fs = slice(fc * 128, (fc + 1) * 128)
                mk = fcnt % 2
                fcnt += 1
                for br in range(4):
                    j = cnt % 2
                    cnt += 1
                    bg = kb.bank()
                    kb.mm(kb.psb(bg, n), [(wg[:, br, kc, fs], hT3[:, kc, p0:p0 + n]) for kc in range(8)], reads=[wg_t] + kb.hT_all, bank_tok=kb.pst[bg])
                    bp = kb.bank()
                    kb.mm(kb.psb(bp, n), [(wb[:, br, kc, fs], brb[k][br][:, kc, 0:n]) for kc in range(4)], reads=[wb_t, brb_t[k]], bank_tok=kb.pst[bp])
                    p.op('act', 'activation', out=sg[j][:, 0:n], in_=kb.psb(bg, n), func=AF.Sigmoid, reads=[kb.pst[bg]], writes=[sg_t[j]])
                    if br == 0:
                        p.op('dve', 'tensor_tensor', macc[mk][:, 0:n], sg[j][:, 0:n], kb.psb(bp, n), ALU.mult, reads=[sg_t[j], kb.pst[bp]], writes=[macc_t[mk]])
                    else:
                        p.op('dve', 'tensor_tensor', tm[j][:, 0:n], sg[j][:, 0:n], kb.psb(bp, n), ALU.mult, reads=[sg_t[j], kb.pst[bp]], writes=[sg_t[j]])
                        p.op('pool', 'tensor_tensor', macc[mk][:, 0:n], macc[mk][:, 0:n], tm[j][:, 0:n], ALU.add, reads=[sg_t[j]], writes=[macc_t[mk]])
                p.op('act', 'activation', out=mout[k][:, fc, 0:n], in_=macc[mk][:, 0:n], func=AF.Copy, reads=[macc_t[mk]], adds=[mout_t[k]])
            p.dma('sp', kb.scr['mT'].rearrange("(c p) t -> p c t", p=128)[:, g * 4:(g + 1) * 4, p0:p0 + n], mout[k][:, :, 0:n],
                  reads=[mout_t[k]], adds=[kb.scr_t['mT']])
    ar.pop()
    p.barrier()
    import os
    if os.environ.get('MERGE_STOP') == '1':
        return
    ar.push()
    wo = v3(ar.bf16(8 * 1024), 8); wo_t = Tok('wo')
    load_w_cols(kb, wo, d['w_out'][l], 0, 1024, wo_t)
    lt = Tok('ln1p')
    g_bc = ar.f32(1024); b_bc = ar.f32(1024)
    bcast_row(kb, g_bc, d['ln1_g'][l], 1024, lt)
    bcast_row(kb, b_bc, d['ln1_b'][l], 1024, lt)
    rw = v3(ar.f32(8 * 32), 8)
    for kc in range(8):
        p.dma('sp', rw[:, kc, :], d['router_w'][l, kc * 128:(kc + 1) * 128, :], adds=[lt])
    rb = ar.f32(32)
    bcast_row(kb, rb, d['router_b'][l], 32, lt)
    NB = 2
    mt = [v3(ar.bf16(8 * 128), 8) for _ in range(NB)]; mt_t = toks(NB, 'mt')
    R = [ar.f32(1024) for _ in range(NB)]; R_t = toks(NB, 'R')
    X = [ar.f32(1024) for _ in range(NB)]; X_t = toks(NB, 'X')
    H = [ar.f32(1024) for _ in range(NB)]; H_t = toks(NB, 'H')
    TB = [ar.bf16(1024) for _ in range(NB)]; TB_t = toks(NB, 'TB')
    XB = [ar.f32(1024) for _ in range(NB)]; XB_t = toks(NB, 'XB')
    ST = [ar.f32(16) for _ in range(NB)]; ST_t = toks(NB, 'ST')
    rs = [ar.f32(64) for _ in range(NB)]; rs_t = toks(NB, 'rs')
    for i in range(NT):
        if os.environ.get('MERGE_STOP') == '3':
            continue
        k = i % NB
        cols = slice(128 * i, 128 * i + 128)
        p.dma('sp', mt[k], kb.scr['mT'].rearrange("(c p) t -> p c t", p=128)[:, :, cols], reads=[kb.scr_t['mT']], writes=[mt_t[k]])
        p.dma('sp', R[k], kb.scr['h_tok'][cols, :], reads=[kb.htok_t[i]], writes=[R_t[k]])
        for half in range(2):
            b = kb.bank()
            hs = slice(half * 512, (half + 1) * 512)
            kb.mm(kb.psb(b), [(mt[k][:, kc, :], wo[:, kc, hs]) for kc in range(8)], reads=[mt_t[k], wo_t], bank_tok=kb.pst[b])
            p.op('dve', 'scalar_tensor_tensor', out=X[k][:, hs], in0=R[k][:, hs], scalar=ALPHA, in1=kb.psb(b), op0=ALU.mult, op1=ALU.add,
                 reads=[R_t[k], kb.pst[b]], adds=[X_t[k]])
        if os.environ.get('MERGE_STOP') == '4':
            continue
        ln_tile(kb, X[k], X_t[k], H[k], H_t[k], ST[k], ST_t[k], g_bc, b_bc, lt, 1e-5)
        if os.environ.get('MERGE_STOP') == '5':
            continue
        h_epilogue(kb, i, H[k], H_t[k], TB[k], TB_t[k], extra_fp32=((XB[k], XB_t[k]) if os.environ.get('MERGE_STOP') != '6' else None))
        if os.environ.get('MERGE_STOP') == '2':
            continue
        b = kb.bank()
        kb.mm(kb.psb(b, 32), [(XB[k][:, c * 128:(c + 1) * 128], rw[:, c, :]) for c in range(8)], reads=[XB_t[k], lt], bank_tok=kb.pst[b])
        lg = rs[k][:, 0:32]; top8 = rs[k][:, 32:40]; s1 = rs[k][:, 40:41]; s2 = rs[k][:, 41:42]
        T_ = rs_t[k]
        p.op('dve', 'tensor_tensor', lg, kb.psb(b, 32), rb, ALU.add, reads=[kb.pst[b], lt], writes=[T_])
        p.op('dve', 'max', top8, lg, reads=[T_], writes=[T_])
        p.op('dve', 'tensor_scalar', s1, top8[:, 0:1], -1.0, None, ALU.mult, reads=[T_], writes=[T_])
        ga = kb.gates[:, i, :]
        p.op('act', 'activation', out=ga, in_=lg, func=AF.Exp, bias=s1, reads=[T_], writes=[kb.gates_t[i]])
        p.op('dve', 'tensor_scalar', lg, lg, top8[:, 3:4], None, ALU.is_ge, reads=[T_], writes=[T_])
        p.op('dve', 'tensor_tensor', ga, ga, lg, ALU.mult, reads=[T_], writes=[kb.gates_t[i]])
        p.op('dve', 'reduce_sum', s2, ga, AX.X, reads=[kb.gates_t[i]], writes=[T_])
        p.op('dve', 'reciprocal', s2, s2, reads=[T_], writes=[T_])
        p.op('dve', 'tensor_scalar', ga, ga, s2, None, ALU.mult, reads=[T_], writes=[kb.gates_t[i]])
    ar.pop()
    p.barrier()


def stage_moe(kb, l, last):
    p, ar = kb.p, kb.ar
    d = kb.din
    if 'macc' not in kb.scr:
        kb.scratch('macc', [TP, 1024], F32)
    kb.macc_t = toks(NT, 'macc')
    hT3 = v3(kb.hT, 8)
    ar.push()
    wgu = [v3(ar.bf16(8 * 2048), 8) for _ in range(2)]; wgu_t = toks(2, 'wgu')
    wdn = [v3(ar.bf16(8 * 1024), 8)]; wdn_t = toks(1, 'wdn')
    brow = [ar.bf16(3072) for _ in range(2)]; brow_t = toks(2, 'brow')
    aT = [v3(ar.bf16(8 * 128), 8) for _ in range(2)]; aT_t = toks(2, 'aT')
    atok = [ar.f32(1024) for _ in range(2)]; atok_t = toks(2, 'atok')
    gW = [ar.f32(512) for _ in range(2)]; sW = [ar.f32(512) for _ in range(2)]; lW = [ar.f32(512) for _ in range(2)]; w_t = toks(2, 'moew')
    A = [ar.f32(1024) for _ in range(2)]; A_t = toks(2, 'A')
    NE = 32

    def load_e(e):
        k = e % 2
        for kc in range(8):
            p.dma('pool', wgu[k][:, kc, :], d['moe_w_gate_up'][l, e, kc * 128:(kc + 1) * 128, :], adds=[wgu_t[k]])
        p.dma('pool', brow[k][0:1, 0:2048], d['moe_b_gate_up'][l, e].rearrange("(o n) -> o n", o=1), adds=[brow_t[k]])
        p.dma('pool', brow[k][0:1, 2048:3072], d['moe_b_down'][l, e].rearrange("(o n) -> o n", o=1), adds=[brow_t[k]])

    def load_dn(e):
        for kc in range(8):
            p.dma('pool', wdn[0][:, kc, :], d['moe_w_down'][l, e, kc * 128:(kc + 1) * 128, :], adds=[wdn_t[0]])

    load_e(0)
    acnt = 0
    wcnt = 0
    tcnt = 0
    for e in range(NE):
        k = e % 2
        if e + 1 < NE:
            load_e(e + 1)
        load_dn(e)
        W = wgu[k]; BR = brow[k]
        for i in range(NT):
            cols = slice(128 * i, 128 * i + 128)
            ak = acnt % 2
            acnt += 1
            for hf in range(2):
                banks = [kb.bank(), kb.bank()]
                fns = []
                for kc in range(8):
                    for cbi in range(2):
                        c0 = 1024 * hf + 512 * cbi
                        fns.append(('matmul', (kb.psb(banks[cbi]), hT3[:, kc, cols], W[:, kc, c0:c0 + 512]), dict(start=(kc == 0), stop=False)))
                for cbi in range(2):
                    c0 = 1024 * hf + 512 * cbi
                    fns.append(('matmul', (kb.psb(banks[cbi]), kb.ones_row[0:1, 0:128], BR[0:1, c0:c0 + 512]), dict(start=False, stop=True)))
                p.group('pe', fns, reads=[wgu_t[k], brow_t[k], kb.cst, kb.hT_t[i]], writes=[kb.pst[banks[0]], kb.pst[banks[1]]])
                for cbi in range(2):
                    j = wcnt % 2
                    wcnt += 1
                    ps2 = kb.psb(banks[cbi]).rearrange("p (n two) -> p n two", two=2)
                    glu = ps2[:, :, 0]; lin = ps2[:, :, 1]
                    N = slice(0, 256)
                    ff0 = 512 * hf + 256 * cbi
                    p.op('dve', 'tensor_scalar', gW[j][:, N], glu, 7.0, None, ALU.min, reads=[kb.pst[banks[cbi]]], writes=[w_t[j]])
                    p.op('act', 'activation', out=sW[j][:, N], in_=gW[j][:, N], func=AF.Sigmoid, scale=1.702, reads=[w_t[j]], writes=[w_t[j]])
                    p.op('dve', 'tensor_scalar', lW[j][:, N], lin, 7.0, -7.0, ALU.min, ALU.max, reads=[kb.pst[banks[cbi]]], writes=[w_t[j]])
                    p.op('dve', 'scalar_tensor_tensor', out=lW[j][:, N], in0=lW[j][:, N], scalar=1.0, in1=gW[j][:, N], op0=ALU.add, op1=ALU.mult,
                         reads=[w_t[j]], writes=[w_t[j]])
                    p.op('pool', 'tensor_tensor', atok[ak][:, ff0:ff0 + 256], lW[j][:, N], sW[j][:, N], ALU.mult, reads=[w_t[j]], adds=[atok_t[ak]])
                bt = kb.bank()
                fns = [('transpose', (kb.psb(bt, 128, off=c * 128), atok[ak][:, 512 * hf + c * 128:512 * hf + (c + 1) * 128], kb.ident), {}) for c in range(4)]
                p.group('pe', fns, reads=[atok_t[ak], kb.cst], writes=[kb.pst[bt]])
                p.op('act', 'activation', out=aT[ak][:, 4 * hf:4 * hf + 4, :], in_=v3(kb.psb(bt), 4), func=AF.Copy, reads=[kb.pst[bt]], adds=[aT_t[ak]])
            tk = tcnt % 2
            tcnt += 1
            if e > 0:
                p.dma('sp', A[tk], kb.scr['macc'][128 * i:128 * i + 128, :], reads=[kb.macc_t[i]], writes=[A_t[tk]])
            bb = [kb.bank(), kb.bank()]
            fns = []
            for kc in range(8):
                for half in range(2):
                    fns.append(('matmul', (kb.psb(bb[half]), aT[ak][:, kc, :], wdn[0][:, kc, half * 512:(half + 1) * 512]), dict(start=(kc == 0), stop=False)))
            for half in range(2):
                fns.append(('matmul', (kb.psb(bb[half]), kb.ones_bf[0:1, 0:128], BR[0:1, 2048 + half * 512:2048 + (half + 1) * 512]), dict(start=False, stop=True)))
            p.group('pe', fns, reads=[aT_t[ak], wdn_t[0], brow_t[k], kb.cst], writes=[kb.pst[bb[0]], kb.pst[bb[1]]])
            for half in range(2):
                hs = slice(half * 512, (half + 1) * 512)
                gcol = kb.gates[:, i, e:e + 1]
                if e == 0:
                    p.op('dve', 'tensor_scalar', A[tk][:, hs], kb.psb(bb[half]), gcol, None, ALU.mult, reads=[kb.pst[bb[half]], kb.gates_t[i]], adds=[A_t[tk]])
                else:
                    p.op('dve', 'scalar_tensor_tensor', out=A[tk][:, hs], in0=kb.psb(bb[half]), scalar=gcol, in1=A[tk][:, hs], op0=ALU.mult, op1=ALU.add,
                         reads=[kb.pst[bb[half]], kb.gates_t[i]], writes=[A_t[tk]])
            p.dma('sp', kb.scr['macc'][128 * i:128 * i + 128, :], A[tk], reads=[A_t[tk]], writes=[kb.macc_t[i]])
    ar.pop()
    p.barrier()
    ar.push()
    lt = Tok('ln2p')
    g_bc = ar.f32(1024); b_bc = ar.f32(1024)
    bcast_row(kb, g_bc, d['ln2_g'][l], 1024, lt)
    bcast_row(kb, b_bc, d['ln2_b'][l], 1024, lt)
    NB = 2
    R = [ar.f32(1024) for _ in range(NB)]; R_t = toks(NB, 'R2')
    Y = [ar.f32(1024) for _ in range(NB)]; Y_t = toks(NB, 'Y2')
    H = [ar.f32(1024) for _ in range(NB)]; H_t = toks(NB, 'H2')
    TB = [ar.bf16(1024) for _ in range(NB)]; TB_t = toks(NB, 'TB2')
    ST = [ar.f32(16) for _ in range(NB)]; ST_t = toks(NB, 'ST2')
    for i in range(NT):
        k = i % NB
        rows = slice(128 * i, 128 * i + 128)
        p.dma('sp', R[k], kb.scr['h_tok'][rows, :], reads=[kb.htok_t[i]], writes=[R_t[k]])
        p.dma('sp', Y[k], kb.scr['macc'][rows, :], reads=[kb.macc_t[i]], writes=[Y_t[k]])
        p.op('dve', 'scalar_tensor_tensor', out=Y[k], in0=R[k], scalar=ALPHA, in1=Y[k], op0=ALU.mult, op1=ALU.add, reads=[R_t[k]], writes=[Y_t[k]])
        ln_tile(kb, Y[k], Y_t[k], H[k], H_t[k], ST[k], ST_t[k], g_bc, b_bc, lt, 1e-5)
        if last:
            if i == 0:
                p.dma('sp', kb.out[0:64, :], H[k][64:128, :], reads=[H_t[k]])
            elif i == NT - 1:
                p.dma('sp', kb.out[4032:4096, :], H[k][0:64, :], reads=[H_t[k]])
            else:
                p.dma('sp', kb.out[128 * i - 64:128 * i + 64, :], H[k], reads=[H_t[k]])
            if 'h_tok' in kb.debug:
                p.dma('sp', kb.scr['h_tok'][rows, :], H[k], reads=[H_t[k]], adds=[kb.htok_t[i]])
        else:
            h_epilogue(kb, i, H[k], H_t[k], TB[k], TB_t[k])
    ar.pop()
    p.barrier()


ALL_STAGES = ('lru', 'mla', 'diff', 'gdn', 'merge', 'moe')


def build_program(debug=(), stages=ALL_STAGES, layers=(0, 1)):
    nc = bass.Bass("TRN2", target_bir_lowering=False)
    es = ExitStack()
    kb = KB(nc, es, debug)
    out = nc.dram_tensor('out', [4096, 1024], F32, kind="ExternalOutput").ap()
    kb.out = out
    kb.scratch('h_tok', [TP, 1024], F32)
    for i in range(4):
        kb.scratch(f'brT{i}', [512, TP], BF16)
    p, ar = kb.p, kb.ar
    kb.htok_t = toks(NT, 'htok')
    kb.hT = ar.bf16(8 * TP)
    kb.hT_t = toks(NT, 'hT')
    kb.hT_all = kb.hT_t
    kb.cst = Tok('cst')
    kb.ident = ar.f32(128)
    p.dma('sp', kb.ident, kb.din['c_ident'], adds=[kb.cst])
    p.op('pool', 'memset', kb.hT, 0.0, writes=kb.hT_t)
    kb.gates = v3(ar.f32(NT * 32), NT)
    kb.gates_t = toks(NT, 'gates')
    setup_consts(kb)
    stage0(kb)
    for l in layers:
        if 'lru' in stages:
            stage_lru(kb, l)
        if 'mla' in stages:
            stage_mla(kb, l)
        if 'diff' in stages:
            stage_diff(kb, l)
        if 'gdn' in stages:
            stage_gdn(kb, l)
        if 'merge' in stages:
            stage_merge(kb, l)
        if 'moe' in stages:
            stage_moe(kb, l, last=(l == layers[-1]))
    if 'moe' not in stages:
        z = ar.f32(1024); zt = Tok('z')
        p.op('pool', 'memset', z, 0.0, writes=[zt])
        p.dma('sp', out[0:128, :], z, reads=[zt])
    p.emit()
    print('n_ins', p.n_ins, 'cnt', p.cur_cnt, 'dma', p.ring_i, 'sbuf top', ar.top)
    kb.used_inputs = list(kb.din.keys())
    return nc, es, kb


_CACHE = {}


def kernel(**inputs):
    if 'prog' not in _CACHE:
        _CACHE['prog'] = build_program(stages=ALL_STAGES, layers=(0, 1))
    nc, es, kb = _CACHE['prog']
    consts = make_consts()
    shared = {}
    for nm in kb.used_inputs:
        if nm in ('x', 'positions'):
            continue
        if nm.startswith('c_'):
            shared[nm] = consts[nm[2:]]
        else:
            shared[nm] = np.ascontiguousarray(np.asarray(inputs[nm], dtype=np.float32))
    in_maps = []
    for b in range(8):
        m = dict(shared)
        m['x'] = np.ascontiguousarray(np.asarray(inputs['x'][b], dtype=np.float32))
        m['positions'] = np.ascontiguousarray(np.asarray(inputs['positions'][b]).astype(np.int32))
        in_maps.append(m)
    res = run_bass_kernel_spmd(nc, in_maps, core_ids=list(range(8)))
    return np.stack([np.asarray(r['out']) for r in res.results], axis=0).astype(np.float32)
```

```python
import numpy as np
import concourse.bass as bass
import concourse.mybir as mybir
from concourse.bass_utils import run_bass_kernel_spmd
import numpy as np
import concourse.bass as bass
import concourse.mybir as mybir

F32 = mybir.dt.float32
BF16 = mybir.dt.bfloat16
I32 = mybir.dt.int32
AF = mybir.ActivationFunctionType
ALU = mybir.AluOpType
AX = mybir.AxisListType

ENGS = ['pe', 'act', 'dve', 'pool', 'sp']
SEM_EPOCH = 1000000
DMA_RING = 24


class Tok:
    __slots__ = ('w', 'r', 'name')

    def __init__(self, name=''):
        self.w = {}
        self.r = {}
        self.name = name


def toks(n, name=''):
    return [Tok(f'{name}{i}') for i in range(n)]


class Prog:
    def __init__(self, nc, es, same_engine_sync=False):
        self.nc = nc
        self.es = es
        self.same = same_engine_sync
        self.ops = {e: [] for e in ENGS}
        self.cur_sem = {}
        self.cur_cnt = {e: 0 for e in ENGS}
        self.sem_id = {}
        self.waited = {e: {} for e in ENGS}
        self.n_sems = 0
        for e in ['pe', 'act', 'dve', 'pool']:
            self.cur_sem[e] = self._new_sem(f'e_{e}')
        self.rings = {}
        self.ring_i = {}
        for q in ['sp', 'act', 'pool']:
            self.rings[q] = [self._new_sem(f'd_{q}{i}') for i in range(DMA_RING)]
            self.ring_i[q] = 0
        self.bar_sem = self._new_sem('bar')
        self.n_bar = 0
        self.n_ins = 0

    def _new_sem(self, name):
        s = self.es.enter_context(self.nc.semaphore(f'{name}_{self.n_sems}'))
        self.sem_id[id(s)] = self.n_sems
        self.n_sems += 1
        return s

    def _wait(self, eng, sem, val):
        k = id(sem)
        if self.waited[eng].get(k, 0) >= val:
            return
        self.waited[eng][k] = val
        self.ops[eng].append(('wait', sem, val))

    def _collect(self, eng, reads, writes, adds):
        need = {}

        def add(t):
            sem, val, src = t
            if src == eng and (not self.same or eng == 'pe'):
                return
            k = id(sem)
            if k not in need or need[k][1] < val:
                need[k] = (sem, val)
        for b in reads:
            for t in b.w.values():
                add(t)
        for b in writes:
            for t in b.w.values():
                add(t)
            for t in b.r.values():
                add(t)
        for b in adds:
            for t in b.w.values():
                if t[2] != 'dma':
                    add(t)
            for t in b.r.values():
                add(t)
        for sem, val in need.values():
            self._wait(eng, sem, val)

    def _update(self, tok, reads, writes, adds):
        k = id(tok[0])
        for b in reads:
            b.r[k] = tok
        for b in writes:
            b.w = {k: tok}
            b.r = {}
        for b in adds:
            b.w[k] = tok
            b.r = {}

    def op(self, eng, name, *args, reads=(), writes=(), adds=(), **kwargs):
        fn = (name, args, kwargs)
        self._collect(eng, reads, writes, adds)
        if self.cur_cnt[eng] >= SEM_EPOCH:
            self.cur_sem[eng] = self._new_sem(f'e_{eng}')
            self.cur_cnt[eng] = 0
        self.cur_cnt[eng] += 1
        sem = self.cur_sem[eng]
        self.ops[eng].append(('ins', fn, sem, 1))
        self.n_ins += 1
        tok = (sem, self.cur_cnt[eng], eng)
        self._update(tok, reads, writes, adds)
        return tok

    def group(self, eng, fns, reads=(), writes=(), adds=()):
        self._collect(eng, reads, writes, adds)
        for f in fns[:-1]:
            self.ops[eng].append(('ins', f, None, 0))
            self.n_ins += 1
        self.cur_cnt[eng] += 1
        sem = self.cur_sem[eng]
        self.ops[eng].append(('ins', fns[-1], sem, 1))
        self.n_ins += 1
        tok = (sem, self.cur_cnt[eng], eng)
        self._update(tok, reads, writes, adds)
        return tok

    def dma(self, q, out, in_, reads=(), writes=(), adds=(), **kw):
        self._collect(q, reads, writes, adds)
        i = self.ring_i[q]
        self.ring_i[q] += 1
        sem = self.rings[q][i % DMA_RING]
        rnd = i // DMA_RING
        if rnd > 0:
            self._wait(q, sem, 16 * rnd)
        val = 16 * (rnd + 1)
        self.ops[q].append(('ins', ('dma_start', (), dict(out=out, in_=in_, **kw)), sem, 16))
        self.n_ins += 1
        tok = (sem, val, 'dma')
        self._update(tok, reads, writes, adds)
        return tok

    def barrier(self):
        for e in ['pe', 'act', 'dve', 'pool']:
            if self.cur_cnt[e] > 0:
                self._wait('sp', self.cur_sem[e], self.cur_cnt[e])
        for q in self.rings:
            n = self.ring_i[q]
            for j in range(min(n, DMA_RING)):
                last_rnd = (n - 1 - j) // DMA_RING
                self._wait('sp', self.rings[q][j], 16 * (last_rnd + 1))
        self.n_bar += 1
        bs = self.bar_sem
        self.ops['sp'].append(('ins', ('sem_inc', (bs, 1), {}), None, 0))
        for e in ['pe', 'act', 'dve', 'pool']:
            self._wait(e, bs, self.n_bar)

    def emit(self):
        nc = self.nc
        self.barrier()

        def run(eng_name, eng):
            for o in self.ops[eng_name]:
                if o[0] == 'wait':
                    eng.wait_ge(o[1], o[2])
                else:
                    nm, a, k = o[1]
                    ins = getattr(eng, nm)(*a, **k)
                    if o[2] is not None:
                        ins.then_inc(o[2], o[3])
        with nc.Block() as block:
            @block.tensor
            def _(e):
                run('pe', e)

            @block.scalar
            def _(e):
                run('act', e)

            @block.vector
            def _(e):
                run('dve', e)

            @block.gpsimd
            def _(e):
                run('pool', e)

            @block.sync
            def _(e):
                run('sp', e)


class Arena:
    def __init__(self, nc, es, words):
        self.t = es.enter_context(nc.sbuf_tensor('arena', [128, words], F32))
        self.words = words
        self.top = 0
        self.marks = []

    def alloc(self, nwords, dtype=F32, nel=None):
        nw = (nwords + 7) // 8 * 8
        assert self.top + nw <= self.words, f'SBUF arena overflow {self.top}+{nw}>{self.words}'
        ap = self.t[:, self.top:self.top + nw]
        self.top += nw
        if dtype != F32:
            ap = ap.bitcast(dtype)
        return ap[:, 0:nel]

    def f32(self, n):
        return self.alloc(n, F32, n)

    def bf16(self, n):
        return self.alloc((n + 1) // 2, BF16, n)

    def i32(self, n):
        return self.alloc(n, I32, n)

    def push(self):
        self.marks.append(self.top)

    def pop(self):
        self.top = self.marks.pop()
import math
from contextlib import ExitStack

TP = 4224
NT = 33
T = 4112
P0 = 48
D = 1024
L = 2
BLOCKS = [(P0 + 512 * i, 512) for i in range(8)] + [(P0 + 4096, 16)]
KT = [(P0 + 128 * i, 128) for i in range(32)] + [(P0 + 4096, 16)]
ALPHA = (2 * L) ** 0.25
NEGBIG = -30000.0

PARAMS = [('ln_in_g', [1024]), ('ln_in_b', [1024]), ('w_in', [2, 1024, 4560]), ('w_gate', [2, 4, 1024, 1024]),
          ('gdn_conv_w', [2, 5, 1536]), ('gdn_a_log', [2, 2, 4]), ('gdn_dt_bias', [2, 2, 4]), ('gdn_norm_g', [2, 128]),
          ('mla_q_norm_g', [2, 256]), ('mla_kv_norm_g', [2, 128]), ('mla_w_uq', [2, 256, 768]), ('mla_w_ukv', [2, 128, 1024]),
          ('lru_conv_w', [2, 5, 512]), ('lru_conv_b', [2, 512]), ('lru_w_a', [2, 2, 8, 64, 64]), ('lru_b_a', [2, 2, 512]),
          ('lru_w_x', [2, 2, 8, 64, 64]), ('lru_b_x', [2, 2, 512]), ('lru_lambda', [2, 2, 512]), ('diff_lambda', [2, 4, 64]),
          ('diff_norm_g', [2, 128]), ('w_branch', [2, 4, 512, 1024]), ('w_out', [2, 1024, 1024]), ('ln1_g', [2, 1024]),
          ('ln1_b', [2, 1024]), ('router_w', [2, 1024, 32]), ('router_b', [2, 32]), ('moe_w_gate_up', [2, 32, 1024, 2048]),
          ('moe_b_gate_up', [2, 32, 2048]), ('moe_w_down', [2, 32, 1024, 1024]), ('moe_b_down', [2, 32, 1024]),
          ('ln2_g', [2, 1024]), ('ln2_b', [2, 1024])]


def make_consts():
    c = {}
    i = np.arange(128)
    c['ident'] = np.eye(128, dtype=np.float32)
    c['ones'] = np.ones((128, 128), np.float32)
    c['mf'] = (i[:, None] <= i[None, :]).astype(np.float32)
    c['mb'] = (i[:, None] >= i[None, :]).astype(np.float32)
    up_s = np.where(i[None, :] > i[:, None], 0.0, NEGBIG).astype(np.float32)
    lo_s = up_s.T.copy()
    up_i = np.where(i[None, :] >= i[:, None], 0.0, NEGBIG).astype(np.float32)
    lo_i = up_i.T.copy()
    for nm, m in [('up_s', up_s), ('lo_s', lo_s), ('up_i', up_i), ('lo_i', lo_i)]:
        c[nm] = np.tile(m, (1, 4))
    half = 32
    inv = (10000.0 ** (-np.arange(half, dtype=np.float32) / half)).astype(np.float32)
    c['invf'] = np.tile(np.concatenate([inv, inv])[:, None], (2, 1)).astype(np.float32)
    c['mpos'] = np.tile(np.arange(16, dtype=np.float32)[None, :], (128, 1))
    return c


CONST_SHAPES = {k: list(v.shape) for k, v in make_consts().items()}
ALL_SHAPES = {'x': [4096, 1024], 'positions': [4096], 'meta': [16, 1024]}
ALL_SHAPES.update({k: v for k, v in PARAMS})
ALL_SHAPES.update({'c_' + k: v for k, v in CONST_SHAPES.items()})


class LazyIn(dict):
    def __init__(self, kb):
        super().__init__()
        self.kb = kb

    def __missing__(self, name):
        shp = ALL_SHAPES[name]
        dt = I32 if name == 'positions' else F32
        ap = self.kb.nc.dram_tensor(name, shp, dt, kind="ExternalInput").ap()
        self[name] = ap
        return ap


class KB:
    def __init__(self, nc, es, debug=()):
        self.nc = nc
        self.es = es
        self.debug = set(debug)
        self.p = Prog(nc, es, same_engine_sync=True)
        self.ar = Arena(nc, es, 53000)
        self.ps = es.enter_context(nc.psum_tensor("ps", [128, 4096], F32))
        self.pst = toks(8, 'ps')
        self.bank_i = 0
        self.din = LazyIn(self)
        self.dout = {}
        self.dumps = {}
        self.scr = {}
        self.scr_t = {}
        import os
        self.gdn_stop = int(os.environ.get('GDN_STOP', '0'))

    def bank(self):
        b = self.bank_i % 8
        self.bank_i += 1
        return b

    def psb(self, b, n=512, rows=128, off=0):
        return self.ps[0:rows, b * 512 + off: b * 512 + off + n]

    def inp(self, name, shape, dt=F32):
        self.din[name] = self.nc.dram_tensor(name, shape, dt, kind="ExternalInput").ap()
        return self.din[name]

    def scratch(self, name, shape, dt):
        kind = "ExternalOutput" if name in self.debug else "Internal"
        t = self.nc.dram_tensor(name, shape, dt, kind=kind).ap()
        self.scr[name] = t
        self.scr_t[name] = Tok(name)
        return t

    def dump(self, name, ap, tok):
        if name not in self.debug:
            return
        shp = list(ap.shape)
        t = self.nc.dram_tensor(name, shp, ap.dtype, kind="ExternalOutput").ap()
        self.p.dma('sp', t, ap, reads=[tok])
        self.dumps[name] = t

    def mm(self, out, pairs, reads, bank_tok, start=True, stop=True):
        n = len(pairs)
        fns = [('matmul', (out, l, r), dict(start=(start and i == 0), stop=(stop and i == n - 1)))
               for i, (l, r) in enumerate(pairs)]
        return self.p.group('pe', fns, reads=reads, writes=[bank_tok])


def v3(ap, c):
    return ap.rearrange("p (c n) -> p c n", c=c)


def ln_tile(kb, X, xt, H, ht, ST, stt, g_bc, b_bc, cst, eps):
    p = kb.p
    p.op('dve', 'bn_stats', ST[:, 0:6], X[:, 0:512], reads=[xt], writes=[stt])
    p.op('dve', 'bn_stats', ST[:, 6:12], X[:, 512:1024], reads=[xt], writes=[stt])
    p.op('dve', 'bn_aggr', ST[:, 12:14], ST[:, 0:12], reads=[stt], writes=[stt])
    p.op('dve', 'tensor_scalar', ST[:, 14:15], ST[:, 13:14], eps, None, ALU.add, reads=[stt], writes=[stt])
    p.op('act', 'activation', out=ST[:, 14:15], in_=ST[:, 14:15], func=AF.Sqrt, reads=[stt], writes=[stt])
    p.op('dve', 'reciprocal', ST[:, 15:16], ST[:, 14:15], reads=[stt], writes=[stt])
    p.op('dve', 'tensor_scalar', H, X, ST[:, 12:13], ST[:, 15:16], ALU.subtract, ALU.mult,
         reads=[xt, stt], writes=[ht])
    p.op('pool', 'tensor_tensor', H, H, g_bc, ALU.mult, reads=[cst], writes=[ht])
    p.op('pool', 'tensor_tensor', H, H, b_bc, ALU.add, reads=[cst], writes=[ht])


def h_epilogue(kb, i, H, ht, TB, tbt, extra_fp32=None):
    p = kb.p
    p.dma('sp', kb.scr['h_tok'][128 * i:128 * i + 128, :], H, reads=[ht], adds=[kb.htok_t[i]])
    for half in range(2):
        b = kb.bank()
        fns = []
        for j in range(4):
            c = half * 4 + j
            fns.append(('transpose', (kb.psb(b, 128, off=j * 128), H[:, c * 128:(c + 1) * 128], kb.ident), {}))
        p.group('pe', fns, reads=[ht, kb.cst], writes=[kb.pst[b]])
        p.op('act', 'activation', out=TB[:, half * 512:(half + 1) * 512], in_=kb.psb(b), func=AF.Copy,
             reads=[kb.pst[b]], writes=[tbt])
        if extra_fp32 is not None:
            XB, xbt = extra_fp32
            p.op('act', 'activation', out=XB[:, half * 512:(half + 1) * 512], in_=kb.psb(b), func=AF.Copy, reads=[kb.pst[b]], adds=[xbt])
    lo, hi = 0, 128
    if i == 0:
        lo = P0
    if i == NT - 1:
        hi = 64
    hTv = v3(kb.hT, 8)
    TBv = v3(TB, 8)
    p.op('pool', 'tensor_copy', hTv[:, :, 128 * i + lo:128 * i + hi], TBv[:, :, lo:hi], reads=[tbt], writes=[kb.hT_t[i]])


def stage0(kb):
    p, ar = kb.p, kb.ar
    x, meta = kb.din['x'], kb.din['meta']
    ar.push()
    g_bc = ar.f32(1024); b_bc = ar.f32(1024)
    t = Tok('lnp')
    p.dma('sp', g_bc, kb.din['ln_in_g'].rearrange("(o n) -> o n", o=1).broadcast_to([128, 1024]), adds=[t])
    p.dma('sp', b_bc, kb.din['ln_in_b'].rearrange("(o n) -> o n", o=1).broadcast_to([128, 1024]), adds=[t])
    NB = 3
    xb = [ar.f32(1024) for _ in range(NB)]; xb_t = toks(NB, 'xb')
    hb = [ar.f32(1024) for _ in range(NB)]; hb_t = toks(NB, 'hb')
    tb = [ar.bf16(1024) for _ in range(NB)]; tb_t = toks(NB, 'tb')
    st = [ar.f32(16) for _ in range(NB)]; st_t = toks(NB, 'st')
    for i in range(NT):
        k = i % NB
        X = xb[k]
        if i == 0:
            p.op('pool', 'memset', X, 0.0, writes=[xb_t[k]])
            p.dma('sp', X[48:64, :], meta, adds=[xb_t[k]])
            p.dma('sp', X[64:128, :], x[0:64, :], adds=[xb_t[k]])
        elif i == NT - 1:
            p.op('pool', 'memset', X, 0.0, writes=[xb_t[k]])
            p.dma('sp', X[0:64, :], x[4032:4096, :], adds=[xb_t[k]])
        else:
            p.dma('sp', X, x[128 * i - 64:128 * i + 64, :], writes=[xb_t[k]])
        ln_tile(kb, X, xb_t[k], hb[k], hb_t[k], st[k], st_t[k], g_bc, b_bc, t, 1e-5)
        h_epilogue(kb, i, hb[k], hb_t[k], tb[k], tb_t[k])
    p.barrier()
    ar.pop()


def softplus_small(kb, out, x, tmp, tok):
    p = kb.p
    p.op('act', 'activation', out=tmp, in_=x, func=AF.Abs, reads=[tok], writes=[tok])
    p.op('act', 'activation', out=tmp, in_=tmp, func=AF.Exp, scale=-1.0, reads=[tok], writes=[tok])
    p.op('act', 'activation', out=tmp, in_=tmp, func=AF.Ln, bias=1.0, reads=[tok], writes=[tok])
    p.op('dve', 'tensor_scalar', out, x, 0.0, None, ALU.max, reads=[tok], writes=[tok])
    p.op('dve', 'tensor_tensor', out, out, tmp, ALU.add, reads=[tok], writes=[tok])


def load_w_cols(kb, dst3, src2d, c0, c1, tok, nk=8):
    for kc in range(nk):
        kb.p.dma('pool', dst3[:, kc, :], src2d[kc * 128:(kc + 1) * 128, c0:c1], adds=[tok])


def load_cols(kb, dst, src1d, tok):
    kb.p.dma('sp', dst, src1d.rearrange("(c p) -> p c", p=128), adds=[tok], allow_slow_non_contiguous=True)


def bcast_row(kb, dst, src1d, n, tok, rows=128):
    kb.p.dma('sp', dst, src1d.rearrange("(o n) -> o n", o=1).broadcast_to([rows, n]), adds=[tok])


def stage_lru(kb, l):
    p, ar = kb.p, kb.ar
    d = kb.din
    ar.push()
    wt = Tok('lru_w')
    w = v3(ar.bf16(8 * 512), 8)
    load_w_cols(kb, w, d['w_in'][l], 2512, 3024, wt)
    sm = Tok('lru_small')
    convw = v3(ar.f32(20), 4)
    for k in range(5):
        load_cols(kb, convw[:, :, k], d['lru_conv_w'][l, k], sm)
    convb = ar.f32(4)
    load_cols(kb, convb, d['lru_conv_b'][l], sm)
    ba = v3(ar.f32(8), 2); bx = v3(ar.f32(8), 2); lam = v3(ar.f32(8), 2)
    for r in range(2):
        load_cols(kb, ba[:, r, :], d['lru_b_a'][l, r], sm)
        load_cols(kb, bx[:, r, :], d['lru_b_x'][l, r], sm)
        load_cols(kb, lam[:, r, :], d['lru_lambda'][l, r], sm)
    coef = ar.f32(8); tmp8 = ar.f32(8)
    lam2 = lam.rearrange("p r c -> p (r c)")
    p.op('dve', 'tensor_scalar', lam2, lam2, -1.0, None, ALU.mult, reads=[sm], writes=[sm])
    softplus_small(kb, coef, lam2, tmp8, sm)
    p.op('dve', 'tensor_scalar', coef, coef, -8.0, None, ALU.mult, reads=[sm], writes=[sm])
    coef = v3(coef, 2)
    wbd_all = ar.bf16(16 * 128)
    wbd_t = Tok('wbd')
    p.op('pool', 'memset', wbd_all, 0.0, writes=[wbd_t])
    wbd = {}
    idx = 0
    for r in range(2):
        for gi, nm in enumerate(['lru_w_a', 'lru_w_x']):
            for c in range(4):
                m = wbd_all[:, idx * 128:(idx + 1) * 128]
                idx += 1
                p.dma('pool', m[0:64, 0:64], d[nm][l, r, 2 * c], adds=[wbd_t])
                p.dma('pool', m[64:128, 64:128], d[nm][l, r, 2 * c + 1], adds=[wbd_t])
                wbd[(r, gi, c)] = m
    bufs = [ar.f32(TP) for _ in range(6)]
    bt = toks(6, 'lrub')
    u_bf = ar.bf16(TP); ubt = Tok('ubf')
    o_bf = ar.bf16(TP); obt = Tok('obf')
    hT3 = v3(kb.hT, 8)
    V = slice(P0, P0 + T)
    for c in range(4):
        pre, u, ra, ix, a, h0 = bufs
        pre_t, u_t, ra_t, ix_t, a_t, h0_t = bt
        p.op('pool', 'memset', pre, 0.0, writes=[pre_t])
        for (p0, n) in BLOCKS:
            b = kb.bank()
            kb.mm(kb.psb(b, n), [(w[:, kc, c * 128:(c + 1) * 128], hT3[:, kc, p0:p0 + n]) for kc in range(8)],
                  reads=[wt] + kb.hT_all, bank_tok=kb.pst[b])
            p.op('act', 'activation', out=pre[:, p0:p0 + n], in_=kb.psb(b, n), func=AF.Copy, reads=[kb.pst[b]], adds=[pre_t])
        uv = u[:, V]
        p.op('act', 'activation', out=uv, in_=pre[:, P0 - 2:P0 - 2 + T], func=AF.Identity,
             scale=convw[:, c, 0:1], bias=convb[:, c:c + 1], reads=[pre_t, sm], writes=[u_t])
        for k in range(1, 5):
            p.op('dve', 'scalar_tensor_tensor', out=uv, in0=pre[:, P0 - 2 + k:P0 - 2 + k + T], scalar=convw[:, c, k:k + 1],
                 in1=uv, op0=ALU.mult, op1=ALU.add, reads=[pre_t, sm], writes=[u_t])
        p.op('pool', 'tensor_copy', u_bf[:, V], uv, reads=[u_t], writes=[ubt])
        hs = [h0, pre]
        hs_t = [h0_t, pre_t]
        for r in range(2):
            for (p0, n) in BLOCKS:
                for gi, (dst, dt_, bias) in enumerate([(ra, ra_t, ba), (ix, ix_t, bx)]):
                    b = kb.bank()
                    kb.mm(kb.psb(b, n), [(wbd[(r, gi, c)], u_bf[:, p0:p0 + n])], reads=[wbd_t, ubt], bank_tok=kb.pst[b])
                    p.op('act', 'activation', out=dst[:, p0:p0 + n], in_=kb.psb(b, n), func=AF.Sigmoid, bias=bias[:, r, c:c + 1],
                         reads=[kb.pst[b], sm], adds=[dt_])
            rav = ra[:, V]; ixv = ix[:, V]; av = a[:, V]
            p.op('act', 'activation', out=av, in_=rav, func=AF.Exp, scale=coef[:, r, c:c + 1], reads=[ra_t, sm], writes=[a_t])
            p.op('dve', 'tensor_tensor', rav, av, av, ALU.mult, reads=[a_t], writes=[ra_t])
            p.op('dve', 'tensor_scalar', rav, rav, -1.0, 1.0, ALU.mult, ALU.add, reads=[ra_t], writes=[ra_t])
            p.op('act', 'activation', out=rav, in_=rav, func=AF.Sqrt, reads=[ra_t], writes=[ra_t])
            p.op('dve', 'tensor_tensor', ixv, ixv, rav, ALU.mult, reads=[ra_t], writes=[ix_t])
            p.op('pool', 'tensor_tensor', ixv, ixv, uv, ALU.mult, reads=[u_t], writes=[ix_t])
            hv = hs[r][:, V]
            if r == 0:
                p.op('dve', 'tensor_tensor_scan', hv, av, ixv, 0.0, ALU.mult, ALU.add, reads=[a_t, ix_t], writes=[hs_t[r]])
            else:
                p.op('dve', 'tensor_tensor_scan', hv[:, ::-1], av[:, ::-1], ixv[:, ::-1], 0.0, ALU.mult, ALU.add,
                     reads=[a_t, ix_t], writes=[hs_t[r]])
        p.op('pool', 'tensor_tensor', o_bf[:, V], h0[:, V], pre[:, V], ALU.add, reads=[h0_t, pre_t], writes=[obt])
        p.dma('sp', kb.scr['brT2'][c * 128:(c + 1) * 128, V], o_bf[:, V], reads=[obt], adds=[kb.scr_t['brT2']])
    p.barrier()
    ar.pop()


TWO_PI = 6.28318
MAGIC = 12582912.0


def setup_consts(kb):
    p, ar = kb.p, kb.ar
    d = kb.din
    kb.ones = ar.f32(128)
    p.dma('sp', kb.ones, d['c_ones'], adds=[kb.cst])
    kb.ones_bf = ar.bf16(128)
    p.op('dve', 'tensor_copy', kb.ones_bf, kb.ones, reads=[kb.cst], adds=[kb.cst])
    kb.ones_row = ar.bf16(512)
    p.op('pool', 'memset', kb.ones_row, 1.0, adds=[kb.cst])
    kb.scratch('cs', [2, 64, TP], F32)
    ar.push()
    t = Tok('rope')
    posi = ar.i32(4096)[0:64, :]
    p.dma('sp', posi, d['positions'].rearrange("(o n) -> o n", o=1).broadcast_to([64, 4096]), adds=[t])
    x = ar.f32(TP)[0:64, :]
    tmp = ar.f32(TP)[0:64, :]
    o = ar.f32(TP)[0:64, :]
    invf = ar.f32(1)
    p.dma('sp', invf, d['c_invf'], adds=[t])
    p.op('pool', 'memset', x, 0.0, writes=[t])
    p.op('dve', 'tensor_copy', x[:, 64:64 + 4096], posi, reads=[t], writes=[t])
    p.op('dve', 'tensor_scalar', x[:, 64:64 + 4096], x[:, 64:64 + 4096], 16.0, None, ALU.add, reads=[t], writes=[t])
    p.dma('sp', x[:, 48:64], d['c_mpos'][0:64, :], reads=[t], adds=[t])
    p.op('dve', 'tensor_scalar', x, x, invf[0:64, 0:1], 1.0 / (2 * math.pi), ALU.mult, ALU.mult, reads=[t], writes=[t])
    for which, off in ((0, 0.0), (1, 0.25)):
        src = x
        if off != 0.0:
            p.op('dve', 'tensor_scalar', x, x, off, None, ALU.add, reads=[t], writes=[t])
        p.op('dve', 'tensor_scalar', tmp, src, MAGIC, None, ALU.add, reads=[t], writes=[t])
        p.op('dve', 'tensor_scalar', tmp, tmp, MAGIC, None, ALU.subtract, reads=[t], writes=[t])
        p.op('dve', 'tensor_tensor', tmp, src, tmp, ALU.subtract, reads=[t], writes=[t])
        p.op('act', 'activation', out=o, in_=tmp, func=AF.Sin, scale=TWO_PI, reads=[t], writes=[t])
        p.dma('sp', kb.scr['cs'][which], o, reads=[t], adds=[kb.scr_t['cs']])
    p.barrier()
    ar.pop()


def load_cs(kb, rows, tok):
    ar, p = kb.ar, kb.p
    sin = ar.f32(TP); cos = ar.f32(TP)
    for r0 in range(0, rows, 64):
        p.dma('sp', sin[r0:r0 + 64, :], kb.scr['cs'][0], reads=[kb.scr_t['cs']], adds=[tok])
        p.dma('sp', cos[r0:r0 + 64, :], kb.scr['cs'][1], reads=[kb.scr_t['cs']], adds=[tok])
    return sin, cos


def make_rot(kb, dst, src, reads, tok):
    p = kb.p
    nd = len(dst.shape)
    lo = (slice(None),) * (nd - 1) + (slice(0, 32),)
    hi = (slice(None),) * (nd - 1) + (slice(32, 64),)
    p.op('dve', 'tensor_scalar', dst[lo], src[hi], -1.0, None, ALU.mult, reads=reads, adds=[tok])
    p.op('dve', 'tensor_copy', dst[hi], src[lo], reads=reads, adds=[tok])


def rms_from_ss(kb, rstd, ss_ps, inv_n, eps, reads, tok):
    p = kb.p
    p.op('dve', 'tensor_scalar', rstd, ss_ps, inv_n, eps, ALU.mult, ALU.add, reads=reads, writes=[tok])
    p.op('act', 'activation', out=rstd, in_=rstd, func=AF.Sqrt, reads=[tok], writes=[tok])
    p.op('dve', 'reciprocal', rstd, rstd, reads=[tok], writes=[tok])


def attention(kb, nmaps, kparts, qparts, vsrc, scale, epilogue):
    p, ar = kb.p, kb.ar
    ar.push()
    NQB = 2
    qbuf = []
    for _ in range(NQB):
        qb = []
        for m in range(nmaps):
            qb.append([ar.bf16(512) for _ in qparts[m]])
        qbuf.append(qb)
    qt = toks(NQB, 'qb')
    NP = 4
    pts = [ar.bf16(512) for _ in range(NP)]
    ptt = toks(NP, 'pt')
    pi = 0
    si = 0
    for bi, (p0, n) in enumerate(BLOCKS):
        qb = qbuf[bi % NQB]
        for m in range(nmaps):
            for ci, (qd, r0, rows) in enumerate(qparts[m]):
                p.dma('sp', qb[m][ci][r0:r0 + rows, 0:n], qd[:, p0:p0 + n], reads=kb.att_qreads, adds=[qt[bi % NQB]])
        obanks = [4 + m for m in range(nmaps)]
        dbanks = [4 + nmaps + m for m in range(nmaps)]
        for j, (k0, nk) in enumerate(KT):
            for m in range(nmaps):
                sb = si % 4
                si += 1
                pairs = []
                for ci, (kap, r0, rows) in enumerate(kparts[m]):
                    pairs.append((kap[r0:r0 + rows, k0:k0 + nk], qb[m][ci][r0:r0 + rows, 0:n]))
                kb.mm(kb.psb(sb, n, rows=nk), pairs, reads=[qt[bi % NQB]] + kb.att_kreads, bank_tok=kb.pst[sb])
                PT = pts[pi % NP]; ptk = ptt[pi % NP]
                pi += 1
                p.op('act', 'activation', out=PT[0:nk, 0:n], in_=kb.psb(sb, n, rows=nk), func=AF.Exp, scale=scale,
                     reads=[kb.pst[sb]], writes=[ptk])
                p.op('pe', 'matmul', kb.psb(obanks[m], n), vsrc[0:nk, j, :], PT[0:nk, 0:n], start=(j == 0), stop=(j == len(KT) - 1),
                     reads=[ptk] + kb.att_vreads, writes=[kb.pst[obanks[m]]])
                p.op('pe', 'matmul', kb.psb(dbanks[m], n), kb.ones_bf[0:nk, :], PT[0:nk, 0:n], start=(j == 0), stop=(j == len(KT) - 1),
                     reads=[ptk, kb.cst], writes=[kb.pst[dbanks[m]]])
        epilogue(bi, p0, n, obanks, dbanks)
    ar.pop()


def stage_mla(kb, l):
    p, ar = kb.p, kb.ar
    d = kb.din
    for nm, shp, dt in [('mq_n', [512, TP], BF16), ('mq_r', [256, TP], BF16), ('mk_n', [512, TP], BF16),
                        ('mk_r', [64, TP], BF16), ('mv', [NT * 128, 512], BF16)]:
        if nm not in kb.scr:
            kb.scratch(nm, shp, dt)
    ar.push()
    wt = Tok('mla_w')
    w = v3(ar.bf16(8 * 448), 8)
    load_w_cols(kb, w, d['w_in'][l], 2064, 2512, wt)
    wkr_rot = v3(ar.bf16(8 * 64), 8)
    make_rot(kb, wkr_rot, w[:, :, 384:448], [wt], wt)
    wuq = v3(ar.bf16(2 * 768), 2)
    load_w_cols(kb, wuq, d['mla_w_uq'][l], 0, 768, wt, nk=2)
    wuq4 = wuq.rearrange("p c (h x) -> p c h x", h=4)
    wuq_rot = ar.bf16(2 * 4 * 64).rearrange("p (c h x) -> p c h x", c=2, h=4)
    make_rot(kb, wuq_rot, wuq4[:, :, :, 128:192], [wt], wt)
    wukv = ar.bf16(1024)
    p.dma('pool', wukv, d['mla_w_ukv'][l], adds=[wt])
    wukv4 = wukv.rearrange("p (h x) -> p h x", h=4)
    sm = Tok('mla_small')
    qg = ar.f32(2); kvg = ar.f32(1)
    load_cols(kb, qg, d['mla_q_norm_g'][l], sm)
    load_cols(kb, kvg, d['mla_kv_norm_g'][l], sm)
    cst_t = Tok('cs')
    sin, cos = load_cs(kb, 64, cst_t)
    ckvn = ar.bf16(TP); ckvn_t = Tok('ckvn')
    hT3 = v3(kb.hT, 8)
    ar.push()
    NB = 2
    cq_sb = [[ar.f32(512) for _ in range(2)] for _ in range(NB)]
    sq_sb = [[ar.f32(512) for _ in range(2)] for _ in range(NB)]
    ckv_sb = [ar.f32(512) for _ in range(NB)]
    sqk_sb = [ar.f32(512) for _ in range(NB)]
    rstd = [ar.f32(512) for _ in range(NB)]; rstdk = [ar.f32(512) for _ in range(NB)]
    cqn = [[ar.bf16(512) for _ in range(2)] for _ in range(NB)]
    t1 = [ar.f32(512) for _ in range(NB)]; t2 = [ar.f32(512) for _ in range(NB)]
    oqn = [ar.bf16(4 * 512) for _ in range(NB)]; oqr = [ar.bf16(4 * 512) for _ in range(NB)]
    okn = [ar.bf16(4 * 512) for _ in range(NB)]; okr = [ar.bf16(512) for _ in range(NB)]
    bt = [Tok(f'mlab{i}') for i in range(NB)]
    ot = [Tok(f'mlao{i}') for i in range(NB)]
    for bi, (p0, n) in enumerate(BLOCKS):
        k = bi % NB
        T_ = bt[k]
        for c in range(2):
            b = kb.bank()
            kb.mm(kb.psb(b, n), [(w[:, kc, c * 128:(c + 1) * 128], hT3[:, kc, p0:p0 + n]) for kc in range(8)],
                  reads=[wt] + kb.hT_all, bank_tok=kb.pst[b])
            p.op('act', 'activation', out=cq_sb[k][c][:, 0:n], in_=kb.psb(b, n), func=AF.Copy, reads=[kb.pst[b]], adds=[T_])
            p.op('act', 'activation', out=sq_sb[k][c][:, 0:n], in_=kb.psb(b, n), func=AF.Square, reads=[kb.pst[b]], adds=[T_])
        b = kb.bank()
        kb.mm(kb.psb(b, n), [(w[:, kc, 256:384], hT3[:, kc, p0:p0 + n]) for kc in range(8)], reads=[wt] + kb.hT_all, bank_tok=kb.pst[b])
        p.op('act', 'activation', out=ckv_sb[k][:, 0:n], in_=kb.psb(b, n), func=AF.Copy, reads=[kb.pst[b]], adds=[T_])
        p.op('act', 'activation', out=sqk_sb[k][:, 0:n], in_=kb.psb(b, n), func=AF.Square, reads=[kb.pst[b]], adds=[T_])
        b = kb.bank()
        kb.mm(kb.psb(b, n), [(kb.ones, sq_sb[k][0][:, 0:n]), (kb.ones, sq_sb[k][1][:, 0:n])], reads=[T_, kb.cst], bank_tok=kb.pst[b])
        rms_from_ss(kb, rstd[k][:, 0:n], kb.psb(b, n), 1.0 / 256, 1e-6, [kb.pst[b]], T_)
        b = kb.bank()
        kb.mm(kb.psb(b, n), [(kb.ones, sqk_sb[k][:, 0:n])], reads=[T_, kb.cst], bank_tok=kb.pst[b])
        rms_from_ss(kb, rstdk[k][:, 0:n], kb.psb(b, n), 1.0 / 128, 1e-6, [kb.pst[b]], T_)
        for c in range(2):
            p.op('dve', 'scalar_tensor_tensor', out=cqn[k][c][:, 0:n], in0=cq_sb[k][c][:, 0:n], scalar=qg[:, c:c + 1],
                 in1=rstd[k][:, 0:n], op0=ALU.mult, op1=ALU.mult, reads=[T_, sm], writes=[T_])
        p.op('dve', 'scalar_tensor_tensor', out=ckvn[:, p0:p0 + n], in0=ckv_sb[k][:, 0:n], scalar=kvg[:, 0:1],
             in1=rstdk[k][:, 0:n], op0=ALU.mult, op1=ALU.mult, reads=[T_, sm], adds=[ckvn_t])
        O_ = ot[k]
        oqn3 = v3(oqn[k], 4); oqr3 = v3(oqr[k], 4); okn3 = v3(okn[k], 4)
        for h in range(4):
            b = kb.bank()
            kb.mm(kb.psb(b, n), [(wuq[:, kc, h * 192:h * 192 + 128], cqn[k][kc][:, 0:n]) for kc in range(2)], reads=[wt, T_], bank_tok=kb.pst[b])
            p.op('act', 'activation', out=oqn3[:, h, 0:n], in_=kb.psb(b, n), func=AF.Copy, reads=[kb.pst[b]], adds=[O_])
            b1 = kb.bank()
            kb.mm(kb.psb(b1, n, rows=64), [(wuq[:, kc, h * 192 + 128:h * 192 + 192], cqn[k][kc][:, 0:n]) for kc in range(2)],
                  reads=[wt, T_], bank_tok=kb.pst[b1])
            b2 = kb.bank()
            kb.mm(kb.psb(b2, n, rows=64), [(wuq_rot[:, kc, h, :], cqn[k][kc][:, 0:n]) for kc in range(2)], reads=[wt, T_], bank_tok=kb.pst[b2])
            p.op('dve', 'tensor_tensor', t1[k][0:64, 0:n], kb.psb(b1, n, rows=64), cos[0:64, p0:p0 + n], ALU.mult,
                 reads=[kb.pst[b1], cst_t], writes=[T_])
            p.op('dve', 'tensor_tensor', t2[k][0:64, 0:n], kb.psb(b2, n, rows=64), sin[0:64, p0:p0 + n], ALU.mult,
                 reads=[kb.pst[b2], cst_t], writes=[T_])
            p.op('pool', 'tensor_tensor', oqr3[0:64, h, 0:n], t1[k][0:64, 0:n], t2[k][0:64, 0:n], ALU.add, reads=[T_], adds=[O_])
            b = kb.bank()
            kb.mm(kb.psb(b, n), [(wukv4[:, h, 0:128], ckvn[:, p0:p0 + n])], reads=[wt, ckvn_t], bank_tok=kb.pst[b])
            p.op('act', 'activation', out=okn3[:, h, 0:n], in_=kb.psb(b, n), func=AF.Copy, reads=[kb.pst[b]], adds=[O_])
        b1 = kb.bank()
        kb.mm(kb.psb(b1, n, rows=64), [(w[:, kc, 384:448], hT3[:, kc, p0:p0 + n]) for kc in range(8)], reads=[wt] + kb.hT_all, bank_tok=kb.pst[b1])
        b2 = kb.bank()
        kb.mm(kb.psb(b2, n, rows=64), [(wkr_rot[:, kc, :], hT3[:, kc, p0:p0 + n]) for kc in range(8)], reads=[wt] + kb.hT_all, bank_tok=kb.pst[b2])
        p.op('dve', 'tensor_tensor', t1[k][0:64, 0:n], kb.psb(b1, n, rows=64), cos[0:64, p0:p0 + n], ALU.mult, reads=[kb.pst[b1], cst_t], writes=[T_])
        p.op('dve', 'tensor_tensor', t2[k][0:64, 0:n], kb.psb(b2, n, rows=64), sin[0:64, p0:p0 + n], ALU.mult, reads=[kb.pst[b2], cst_t], writes=[T_])
        p.op('pool', 'tensor_tensor', okr[k][0:64, 0:n], t1[k][0:64, 0:n], t2[k][0:64, 0:n], ALU.add, reads=[T_], adds=[O_])
        sc = kb.scr
        p.dma('sp', sc['mq_n'].rearrange("(h p) t -> p h t", p=128)[:, :, p0:p0 + n], oqn3[:, :, 0:n], reads=[O_], adds=[kb.scr_t['mq_n']])
        p.dma('sp', sc['mq_r'].rearrange("(h p) t -> p h t", p=64)[:, :, p0:p0 + n], oqr3[0:64, :, 0:n], reads=[O_], adds=[kb.scr_t['mq_r']])
        p.dma('sp', sc['mk_n'].rearrange("(h p) t -> p h t", p=128)[:, :, p0:p0 + n], okn3[:, :, 0:n], reads=[O_], adds=[kb.scr_t['mk_n']])
        p.dma('sp', sc['mk_r'][:, p0:p0 + n], okr[k][0:64, 0:n], reads=[O_], adds=[kb.scr_t['mk_r']])
    vb = [ar.bf16(512) for _ in range(2)]; vbt = toks(2, 'vb')
    for j, (k0, nk) in enumerate(KT):
        b = kb.bank()
        kb.mm(kb.psb(b, 512, rows=nk), [(ckvn[:, k0:k0 + nk], wukv4[:, :, 128:256])], reads=[wt, ckvn_t], bank_tok=kb.pst[b])
        p.op('act', 'activation', out=vb[j % 2][0:nk, :], in_=kb.psb(b, 512, rows=nk), func=AF.Copy, reads=[kb.pst[b]], writes=[vbt[j % 2]])
        p.dma('sp', kb.scr['mv'][128 * j:128 * j + nk, :], vb[j % 2][0:nk, :], reads=[vbt[j % 2]], adds=[kb.scr_t['mv']])
    ar.pop()
    ar.pop()
    p.barrier()
    ar.push()
    scale = (128 + 64) ** -0.5
    kr = ar.bf16(TP); krt = Tok('kr')
    p.dma('sp', kr[0:64, P0:P0 + T], kb.scr['mk_r'][:, P0:P0 + T], reads=[kb.scr_t['mk_r']], writes=[krt])
    NH = 2
    kn = [ar.bf16(TP) for _ in range(NH)]; knt = toks(NH, 'kn')
    vh = [ar.bf16(NT * 128) for _ in range(NH)]; vht = toks(NH, 'vh')
    ob = [ar.bf16(512) for _ in range(2)]; obt = toks(2, 'ob')
    rc = [ar.f32(512) for _ in range(2)]
    kb.att_qreads = [kb.scr_t['mq_n'], kb.scr_t['mq_r']]
    for h in range(4):
        k = h % NH
        p.dma('sp', kn[k][:, P0:P0 + T], kb.scr['mk_n'][h * 128:(h + 1) * 128, P0:P0 + T], reads=[kb.scr_t['mk_n']], writes=[knt[k]])
        vv = v3(vh[k], NT)
        p.dma('sp', vv[:, 0:NT - 1, :], kb.scr['mv'].rearrange("(j p) c -> p j c", p=128)[:, 0:NT - 1, h * 128:(h + 1) * 128], reads=[kb.scr_t['mv']], writes=[vht[k]])
        p.dma('sp', vv[0:16, NT - 1, :], kb.scr['mv'][128 * (NT - 1):128 * (NT - 1) + 16, h * 128:(h + 1) * 128], reads=[kb.scr_t['mv']], adds=[vht[k]])
        kb.att_kreads = [knt[k], krt]
        kb.att_vreads = [vht[k]]

        def epi(bi, p0, n, obanks, dbanks, h=h):
            q = bi % 2
            p.op('dve', 'reciprocal', rc[q][:, 0:n], kb.psb(dbanks[0], n), reads=[kb.pst[dbanks[0]]], writes=[obt[q]])
            p.op('dve', 'tensor_tensor', ob[q][:, 0:n], kb.psb(obanks[0], n), rc[q][:, 0:n], ALU.mult, reads=[kb.pst[obanks[0]]], writes=[obt[q]])
            p.dma('sp', kb.scr['brT1'][h * 128:(h + 1) * 128, p0:p0 + n], ob[q][:, 0:n], reads=[obt[q]], adds=[kb.scr_t['brT1']])
        attention(kb, 1, [[(kn[k], 0, 128), (kr, 0, 64)]],
                  [[(kb.scr['mq_n'][h * 128:(h + 1) * 128, :], 0, 128), (kb.scr['mq_r'][h * 64:(h + 1) * 64, :], 0, 64)]],
                  vv, scale, epi)
    ar.pop()
    p.barrier()


def stage_diff(kb, l):
    p, ar = kb.p, kb.ar
    d = kb.din
    for nm, shp, dt in [('dq', [512, TP], BF16), ('dk', [512, TP], BF16), ('dv', [NT * 128, 512], BF16)]:
        if nm not in kb.scr:
            kb.scratch(nm, shp, dt)
    lam_init = 0.8 - 0.6 * math.exp(-0.3 * l)
    ar.push()
    wt = Tok('diff_w')
    cst_t = Tok('cs')
    sin, cos = load_cs(kb, 128, cst_t)
    hT3 = v3(kb.hT, 8)
    t1 = [ar.f32(512) for _ in range(2)]; t2 = [ar.f32(512) for _ in range(2)]
    oq = [ar.bf16(512) for _ in range(2)]
    tt = toks(2, 'difft'); ot = toks(2, 'diffo')
    cnt = 0
    for which, c0, dst in (('q', 3024, 'dq'), ('k', 3536, 'dk')):
        ar.push()
        w = v3(ar.bf16(8 * 512), 8)
        w_t = Tok('dw' + which)
        load_w_cols(kb, w, d['w_in'][l], c0, c0 + 512, w_t)
        wr = v3(ar.bf16(8 * 512), 8)
        make_rot(kb, wr.rearrange("p c (m x) -> p c m x", x=64), w.rearrange("p c (m x) -> p c m x", x=64), [w_t], w_t)
        for h in range(4):
            for (p0, n) in BLOCKS:
                k = cnt % 2
                cnt += 1
                b1 = kb.bank()
                kb.mm(kb.psb(b1, n), [(w[:, kc, h * 128:(h + 1) * 128], hT3[:, kc, p0:p0 + n]) for kc in range(8)],
                      reads=[w_t] + kb.hT_all, bank_tok=kb.pst[b1])
                b2 = kb.bank()
                kb.mm(kb.psb(b2, n), [(wr[:, kc, h * 128:(h + 1) * 128], hT3[:, kc, p0:p0 + n]) for kc in range(8)],
                      reads=[w_t] + kb.hT_all, bank_tok=kb.pst[b2])
                p.op('dve', 'tensor_tensor', t1[k][:, 0:n], kb.psb(b1, n), cos[:, p0:p0 + n], ALU.mult, reads=[kb.pst[b1], cst_t], writes=[tt[k]])
                p.op('dve', 'tensor_tensor', t2[k][:, 0:n], kb.psb(b2, n), sin[:, p0:p0 + n], ALU.mult, reads=[kb.pst[b2], cst_t], writes=[tt[k]])
                p.op('pool', 'tensor_tensor', oq[k][:, 0:n], t1[k][:, 0:n], t2[k][:, 0:n], ALU.add, reads=[tt[k]], writes=[ot[k]])
                p.dma('sp', kb.scr[dst][h * 128:(h + 1) * 128, p0:p0 + n], oq[k][:, 0:n], reads=[ot[k]], adds=[kb.scr_t[dst]])
        ar.pop()
        p.barrier()
    ar.push()
    w = v3(ar.bf16(8 * 512), 8); w_t = Tok('dwv')
    load_w_cols(kb, w, d['w_in'][l], 4048, 4560, w_t)
    vb = [ar.bf16(512) for _ in range(2)]; vbt = toks(2, 'dvb')
    for j, (k0, nk) in enumerate(KT):
        b = kb.bank()
        kb.mm(kb.psb(b, 512, rows=nk), [(hT3[:, kc, k0:k0 + nk], w[:, kc, :]) for kc in range(8)], reads=[w_t] + kb.hT_all, bank_tok=kb.pst[b])
        p.op('act', 'activation', out=vb[j % 2][0:nk, :], in_=kb.psb(b, 512, rows=nk), func=AF.Copy, reads=[kb.pst[b]], writes=[vbt[j % 2]])
        p.dma('sp', kb.scr['dv'][128 * j:128 * j + nk, :], vb[j % 2][0:nk, :], reads=[vbt[j % 2]], adds=[kb.scr_t['dv']])
    ar.pop()
    ar.pop()
    p.barrier()
    ar.push()
    sm = Tok('diff_small')
    lv = ar.f32(256)
    bcast_row(kb, lv, d['diff_lambda'][l].rearrange("a b -> (a b)"), 256, sm)
    pr = ar.f32(64); e1 = ar.f32(1); e2 = ar.f32(1); neglam = ar.f32(1); gsc = ar.f32(1)
    p.op('dve', 'tensor_tensor', pr, lv[:, 0:64], lv[:, 64:128], ALU.mult, reads=[sm], writes=[sm])
    p.op('dve', 'reduce_sum', e1, pr, AX.X, reads=[sm], writes=[sm])
    p.op('dve', 'tensor_tensor', pr, lv[:, 128:192], lv[:, 192:256], ALU.mult, reads=[sm], writes=[sm])
    p.op('dve', 'reduce_sum', e2, pr, AX.X, reads=[sm], writes=[sm])
    p.op('act', 'activation', out=e1, in_=e1, func=AF.Exp, reads=[sm], writes=[sm])
    p.op('act', 'activation', out=e2, in_=e2, func=AF.Exp, reads=[sm], writes=[sm])
    p.op('dve', 'tensor_tensor', neglam, e2, e1, ALU.subtract, reads=[sm], writes=[sm])
    p.op('dve', 'tensor_scalar', neglam, neglam, -lam_init, None, ALU.add, reads=[sm], writes=[sm])
    load_cols(kb, gsc, d['diff_norm_g'][l], sm)
    p.op('dve', 'tensor_scalar', gsc, gsc, 1.0 - lam_init, None, ALU.mult, reads=[sm], writes=[sm])
    scale = 64 ** -0.5
    NH = 2
    kh = [ar.bf16(TP) for _ in range(NH)]; kht = toks(NH, 'dkh')
    vh = [ar.bf16(NT * 128) for _ in range(NH)]; vht = toks(NH, 'dvh')
    r1 = ar.f32(512); r2 = ar.f32(512); o1 = ar.f32(512); o2 = ar.f32(512); sq = ar.f32(512); rs = ar.f32(512)
    ob = [ar.bf16(512) for _ in range(2)]; obt = toks(2, 'dob')
    et = Tok('depi')
    kb.att_qreads = [kb.scr_t['dq']]
    for h in range(4):
        k = h % NH
        p.dma('sp', kh[k][:, P0:P0 + T], kb.scr['dk'][h * 128:(h + 1) * 128, P0:P0 + T], reads=[kb.scr_t['dk']], writes=[kht[k]])
        vv = v3(vh[k], NT)
        p.dma('sp', vv[:, 0:NT - 1, :], kb.scr['dv'].rearrange("(j p) c -> p j c", p=128)[:, 0:NT - 1, h * 128:(h + 1) * 128], reads=[kb.scr_t['dv']], writes=[vht[k]])
        p.dma('sp', vv[0:16, NT - 1, :], kb.scr['dv'][128 * (NT - 1):128 * (NT - 1) + 16, h * 128:(h + 1) * 128], reads=[kb.scr_t['dv']], adds=[vht[k]])
        kb.att_kreads = [kht[k]]
        kb.att_vreads = [vht[k]]

        def epi(bi, p0, n, obanks, dbanks, h=h):
            q = bi % 2
            N = slice(0, n)
            p.op('dve', 'reciprocal', r1[:, N], kb.psb(dbanks[0], n), reads=[kb.pst[dbanks[0]]], writes=[et])
            p.op('dve', 'reciprocal', r2[:, N], kb.psb(dbanks[1], n), reads=[kb.pst[dbanks[1]]], writes=[et])
            p.op('dve', 'tensor_tensor', o1[:, N], kb.psb(obanks[0], n), r1[:, N], ALU.mult, reads=[kb.pst[obanks[0]], et], writes=[et])
            p.op('dve', 'tensor_scalar', r2[:, N], r2[:, N], neglam[:, 0:1], None, ALU.mult, reads=[et, sm], writes=[et])
            p.op('dve', 'tensor_tensor', o2[:, N], kb.psb(obanks[1], n), r2[:, N], ALU.mult, reads=[kb.pst[obanks[1]], et], writes=[et])
            p.op('dve', 'tensor_tensor', o1[:, N], o1[:, N], o2[:, N], ALU.add, reads=[et], writes=[et])
            p.op('act', 'activation', out=sq[:, N], in_=o1[:, N], func=AF.Square, reads=[et], writes=[et])
            sb = 0
            kb.mm(kb.psb(sb, n), [(kb.ones, sq[:, N])], reads=[et, kb.cst], bank_tok=kb.pst[sb])
            rms_from_ss(kb, rs[:, N], kb.psb(sb, n), 1.0 / 128, 1e-6, [kb.pst[sb]], et)
            p.op('dve', 'scalar_tensor_tensor', out=ob[q][:, N], in0=o1[:, N], scalar=gsc[:, 0:1], in1=rs[:, N], op0=ALU.mult, op1=ALU.mult,
                 reads=[et, sm], writes=[obt[q]])
            p.dma('sp', kb.scr['brT3'][h * 128:(h + 1) * 128, p0:p0 + n], ob[q][:, N], reads=[obt[q]], adds=[kb.scr_t['brT3']])
        qd = kb.scr['dq'][h * 128:(h + 1) * 128, :]
        attention(kb, 2, [[(kh[k], 0, 64)], [(kh[k], 64, 64)]], [[(qd[0:64, :], 0, 64)], [(qd[64:128, :], 64, 64)]], vv, scale, epi)
    ar.pop()
    p.barrier()


def bc_mid(ap, k):
    return ap.unsqueeze(1).broadcast_to([ap.shape[0], k, ap.shape[1]])


def bc_last(ap, n):
    return ap.unsqueeze(2).broadcast_to([ap.shape[0], ap.shape[1], n])


def stage_gdn(kb, l):
    p, ar = kb.p, kb.ar
    d = kb.din
    for nm in ('gq', 'gk', 'gv'):
        if nm not in kb.scr:
            kb.scratch(nm, [512, TP], F32)
    if 'gof' not in kb.scr:
        kb.scratch('gof', [TP, 512], F32)
    hT3 = v3(kb.hT, 8)
    ar.push()
    gall = v3(ar.f32(NT * 8), NT); lball = v3(ar.f32(NT * 8), NT); betall = v3(ar.f32(NT * 8), NT)
    sc_t = Tok('gdn_sc')
    ar.push()
    wt = Tok('gdn_w')
    w = v3(ar.bf16(8 * 1536), 8)
    load_w_cols(kb, w, d['w_in'][l], 0, 1536, wt)
    wab = v3(ar.bf16(8 * 16), 8)
    load_w_cols(kb, wab, d['w_in'][l], 2048, 2064, wt)
    sm = Tok('gdn_small')
    convw = v3(ar.f32(60), 12)
    for k in range(5):
        load_cols(kb, convw[:, :, k], d['gdn_conv_w'][l, k], sm)
    dtb = ar.f32(8); alog = ar.f32(8)
    bcast_row(kb, dtb, d['gdn_dt_bias'][l].rearrange("a b -> (a b)"), 8, sm)
    bcast_row(kb, alog, d['gdn_a_log'][l].rearrange("a b -> (a b)"), 8, sm)
    p.op('act', 'activation', out=alog, in_=alog, func=AF.Exp, reads=[sm], writes=[sm])
    p.op('dve', 'tensor_scalar', alog, alog, -1.0, None, ALU.mult, reads=[sm], writes=[sm])
    xa = ar.f32(8); tmp8 = ar.f32(8); nb = ar.f32(8); xt_ = Tok('gdn_x')
    for i in range(NT):
        b = kb.bank()
        kb.mm(kb.psb(b, 16), [(hT3[:, kc, 128 * i:128 * i + 128], wab[:, kc, :]) for kc in range(8)], reads=[wt] + kb.hT_all, bank_tok=kb.pst[b])
        p.op('dve', 'tensor_tensor', xa, kb.psb(b, 8), dtb, ALU.add, reads=[kb.pst[b], sm], writes=[xt_])
        p.op('dve', 'tensor_scalar', nb, kb.psb(b, 8, off=8), -1.0, None, ALU.mult, reads=[kb.pst[b]], writes=[xt_])
        softplus_small(kb, gall[:, i, :], xa, tmp8, xt_)
        p.op('dve', 'tensor_tensor', gall[:, i, :], gall[:, i, :], alog, ALU.mult, reads=[xt_, sm], writes=[xt_])
        softplus_small(kb, lball[:, i, :], nb, tmp8, xt_)
        p.op('dve', 'tensor_scalar', lball[:, i, :], lball[:, i, :], -1.0, None, ALU.mult, reads=[xt_], writes=[xt_])
        p.op('act', 'activation', out=betall[:, i, :], in_=lball[:, i, :], func=AF.Exp, reads=[xt_], writes=[xt_])
    for (tile, lo, hi) in ((0, 0, P0), (NT - 1, 64, 128)):
        p.op('pool', 'memset', gall[lo:hi, tile, :], 0.0, reads=[xt_], writes=[xt_])
        p.op('pool', 'memset', betall[lo:hi, tile, :], 0.0, reads=[xt_], writes=[xt_])
        p.op('pool', 'memset', lball[lo:hi, tile, :], -100.0, reads=[xt_], writes=[xt_])
    p.op('pool', 'tensor_copy', gall[:, 0, 0:1], gall[:, 0, 0:1], reads=[xt_], writes=[sc_t])
    V = slice(P0, P0 + T)
    NBUF = 2
    pre = [ar.f32(TP) for _ in range(NBUF)]; pre_t = toks(NBUF, 'gpre')
    cv = [ar.f32(TP) for _ in range(NBUF)]; cv_t = toks(NBUF, 'gcv')
    sq = [ar.f32(512) for _ in range(2)]; rn = [ar.f32(512) for _ in range(2)]; nt_ = toks(2, 'gnrm')
    for k in range(NBUF):
        p.op('pool', 'memset', pre[k], 0.0, writes=[pre_t[k]])
        p.op('pool', 'memset', cv[k], 0.0, writes=[cv_t[k]])
    cnt = 0
    for c in range(12):
        k = c % NBUF
        PR, CV = pre[k], cv[k]
        for (p0, n) in BLOCKS:
            b = kb.bank()
            kb.mm(kb.psb(b, n), [(w[:, kc, c * 128:(c + 1) * 128], hT3[:, kc, p0:p0 + n]) for kc in range(8)],
                  reads=[wt] + kb.hT_all, bank_tok=kb.pst[b])
            p.op('act', 'activation', out=PR[:, p0:p0 + n], in_=kb.psb(b, n), func=AF.Copy, reads=[kb.pst[b]], adds=[pre_t[k]])
        cvv = CV[:, V]
        p.op('act', 'activation', out=cvv, in_=PR[:, P0 - 2:P0 - 2 + T], func=AF.Identity, scale=convw[:, c, 0:1], reads=[pre_t[k], sm], writes=[cv_t[k]])
        for kk in range(1, 5):
            p.op('dve', 'scalar_tensor_tensor', out=cvv, in0=PR[:, P0 - 2 + kk:P0 - 2 + kk + T], scalar=convw[:, c, kk:kk + 1],
                 in1=cvv, op0=ALU.mult, op1=ALU.add, reads=[pre_t[k], sm], writes=[cv_t[k]])
        p.op('act', 'activation', out=cvv, in_=cvv, func=AF.Silu, reads=[cv_t[k]], writes=[cv_t[k]])
        if c < 8:
            qscale = (128 ** -0.5) if c < 4 else 1.0
            for (p0, n) in BLOCKS:
                j = cnt % 2
                cnt += 1
                p.op('act', 'activation', out=sq[j][:, 0:n], in_=CV[:, p0:p0 + n], func=AF.Square, reads=[cv_t[k]], writes=[nt_[j]])
                b = kb.bank()
                kb.mm(kb.psb(b, n), [(kb.ones, sq[j][:, 0:n])], reads=[nt_[j], kb.cst], bank_tok=kb.pst[b])
                rms_from_ss(kb, rn[j][:, 0:n], kb.psb(b, n), 1.0, 1e-6, [kb.pst[b]], nt_[j])
                p.op('dve', 'scalar_tensor_tensor', out=CV[:, p0:p0 + n], in0=CV[:, p0:p0 + n], scalar=qscale, in1=rn[j][:, 0:n],
                     op0=ALU.mult, op1=ALU.mult, reads=[nt_[j]], writes=[cv_t[k]])
        dst = ('gq', 'gk', 'gv')[c // 4]
        hh = c % 4
        p.dma('sp', kb.scr[dst][hh * 128:(hh + 1) * 128, :], CV, reads=[cv_t[k]], adds=[kb.scr_t[dst]])
    ar.pop()
    p.barrier()
    if 'gdnA' in kb.debug:
        ar.pop()
        return
    cc = Tok('gdn_c')
    def ld(nm, n=128):
        a = ar.f32(n)
        p.dma('sp', a, d['c_' + nm], adds=[cc])
        return a
    mf = ld('mf'); mb = ld('mb'); ups = ld('up_s', 512); los = ld('lo_s', 512); upi = ld('up_i', 512); loi = ld('lo_i', 512)
    negones = ar.f32(128); negmf = ar.f32(128); negmb = ar.f32(128); ident4 = ar.f32(512)
    p.op('dve', 'tensor_scalar', negones, kb.ones, -1.0, None, ALU.mult, reads=[kb.cst], adds=[cc])
    p.op('dve', 'tensor_scalar', negmf, mf, -1.0, None, ALU.mult, reads=[cc], adds=[cc])
    p.op('dve', 'tensor_scalar', negmb, mb, -1.0, None, ALU.mult, reads=[cc], adds=[cc])
    p.op('dve', 'tensor_copy', v3(ident4, 4), bc_mid(kb.ident, 4), reads=[kb.cst], adds=[cc])
    wz = v3(ar.bf16(8 * 512), 8); wz_t = Tok('wz')
    load_w_cols(kb, wz, d['w_in'][l], 1536, 2048, wz_t)
    gn4 = ar.f32(512)
    for hh in range(4):
        bcast_row(kb, gn4[:, hh * 128:(hh + 1) * 128], d['gdn_norm_g'][l], 128, wz_t)
    F = lambda: ar.f32(512)
    B = lambda: ar.bf16(512)
    qT, kT, vT = F(), F(), F(); in_t = Tok('g_in')
    qb_, kb_ = B(), B(); inb_t = Tok('g_inb')
    ktok, vtok, bv = F(), F(), F(); tk_t = Tok('g_tok')
    GM, LBI, NG, LBb = F(), F(), F(), F(); bt_ = Tok('g_build')
    Ea, Eb, Ec, EG = F(), F(), F(), F(); e_t = Tok('g_exp')
    Lm, Ltm, X = F(), F(), F(); l_t = Tok('g_L'); x_t = Tok('g_X')
    Pp = [F(), F()]; Qq = [F(), F()]; pq_t = [Tok('g_P0'), Tok('g_P1')]
    attnT, Tt, Rp, vnew, qdT, kdec = B(), B(), B(), B(), B(), B()
    a_t, tt_t, r_t, vn_t, qd_t, kd_t = Tok('g_at'), Tok('g_Tt'), Tok('g_R'), Tok('g_vn'), Tok('g_qd'), Tok('g_kd')
    smalls = ar.f32(32); s_t = Tok('g_small')
    S = F(); Sb = B(); S_t = Tok('g_S'); Sb_t = Tok('g_Sb')
    ot = [F(), F()]; ot_t = toks(2, 'g_o')
    of_ = F(); of_t = Tok('g_of')
    zt = F(); z_t = Tok('g_z')
    ss4 = ar.f32(8); ob4 = B(); ob_t = Tok('g_ob')
    H4 = lambda a: v3(a, 4)
    for dr in range(2):
        M_, negM, NEGs_ji, NEGs_ij, NEGi_ji = (mf, negmf, ups, los, upi) if dr == 0 else (mb, negmb, los, ups, loi)
        last = 127 if dr == 0 else 0
        p.op('pool', 'memset', S, 0.0, writes=[S_t])
        p.op('pool', 'memset', Sb, 0.0, writes=[Sb_t])
        order = range(NT) if dr == 0 else range(NT - 1, -1, -1)
        for step, i in enumerate(order):
            cols = slice(128 * i, 128 * i + 128)
            for nm, dst in (('gq', qT), ('gk', kT), ('gv', vT)):
                p.dma('sp', H4(dst), kb.scr[nm].rearrange("(h p) t -> p h t", p=128)[:, :, cols], reads=[kb.scr_t[nm]], adds=[in_t])
            p.op('pool', 'tensor_copy', qb_, qT, reads=[in_t], writes=[inb_t])
            p.op('pool', 'tensor_copy', kb_, kT, reads=[in_t], adds=[inb_t])
            g4 = gall[:, i, dr * 4:dr * 4 + 4]; lb4 = lball[:, i, dr * 4:dr * 4 + 4]; be4 = betall[:, i, dr * 4:dr * 4 + 4]
            b1 = kb.bank()
            p.group('pe', [('transpose', (kb.psb(b1, 128, off=hh * 128), kT[:, hh * 128:(hh + 1) * 128], kb.ident), {}) for hh in range(4)],
                    reads=[in_t, kb.cst], writes=[kb.pst[b1]])
            b2 = kb.bank()
            p.group('pe', [('transpose', (kb.psb(b2, 128, off=hh * 128), vT[:, hh * 128:(hh + 1) * 128], kb.ident), {}) for hh in range(4)],
                    reads=[in_t, kb.cst], writes=[kb.pst[b2]])
            p.op('act', 'activation', out=ktok, in_=kb.psb(b1), func=AF.Copy, reads=[kb.pst[b1]], writes=[tk_t])
            p.op('dve', 'tensor_tensor', H4(bv), H4(kb.psb(b2)), bc_last(be4, 128), ALU.mult, reads=[kb.pst[b2], sc_t], adds=[tk_t])
            if kb.gdn_stop == 1:
                ar.pop(); p.barrier(); return
            p.op('pool', 'tensor_tensor', H4(GM), bc_mid(M_, 4), bc_last(g4, 128), ALU.mult, reads=[cc, sc_t], writes=[bt_])
            p.op('pool', 'tensor_tensor', H4(LBI), bc_mid(kb.ident, 4), bc_last(lb4, 128), ALU.mult, reads=[kb.cst, sc_t], adds=[bt_])
            p.op('pool', 'tensor_tensor', H4(NG), bc_mid(negones, 4), bc_last(g4, 128), ALU.mult, reads=[cc, sc_t], adds=[bt_])
            p.op('pool', 'tensor_tensor', H4(LBb), bc_mid(kb.ones, 4), bc_last(lb4, 128), ALU.mult, reads=[kb.cst, sc_t], adds=[bt_])
            if kb.gdn_stop == 2:
                ar.pop(); p.barrier(); return
            bg = kb.bank()
            kb.mm(kb.psb(bg, 4), [(M_, g4)], reads=[cc, sc_t], bank_tok=kb.pst[bg])
            p.op('dve', 'tensor_copy', smalls[:, 0:4], kb.psb(bg, 4), reads=[kb.pst[bg]], writes=[s_t])
            p.op('act', 'activation', out=smalls[:, 4:8], in_=smalls[:, 0:4], func=AF.Exp, reads=[s_t], writes=[s_t])
            p.op('dve', 'scalar_tensor_tensor', out=smalls[:, 8:12], in0=smalls[:, 4:8], scalar=-1.0, in1=be4, op0=ALU.mult, op1=ALU.mult,
                 reads=[s_t, sc_t], writes=[s_t])
            if kb.gdn_stop == 3:
                ar.pop(); p.barrier(); return
            ba_ = kb.bank()
            kb.mm(kb.psb(ba_), [(kb.ones, GM), (kb.ones, LBI), (M_, NG), (kb.ident, NEGs_ji)], reads=[bt_, cc, kb.cst], bank_tok=kb.pst[ba_])
            p.op('act', 'activation', out=Ea, in_=kb.psb(ba_), func=AF.Exp, reads=[kb.pst[ba_]], writes=[e_t])
            bb_ = kb.bank()
            kb.mm(kb.psb(bb_), [(negM, NG), (kb.ident, LBb), (negones, GM), (kb.ident, NEGs_ij)], reads=[bt_, cc, kb.cst], bank_tok=kb.pst[bb_])
            p.op('act', 'activation', out=Eb, in_=kb.psb(bb_), func=AF.Exp, reads=[kb.pst[bb_]], adds=[e_t])
            bc_ = kb.bank()
            kb.mm(kb.psb(bc_), [(kb.ones, GM), (M_, NG), (kb.ident, NEGi_ji)], reads=[bt_, cc, kb.cst], bank_tok=kb.pst[bc_])
            p.op('act', 'activation', out=Ec, in_=kb.psb(bc_), func=AF.Exp, reads=[kb.pst[bc_]], adds=[e_t])
            bd_ = kb.bank()
            kb.mm(kb.psb(bd_), [(kb.ones, GM)], reads=[bt_, kb.cst], bank_tok=kb.pst[bd_])
            p.op('act', 'activation', out=EG, in_=kb.psb(bd_), func=AF.Exp, reads=[kb.pst[bd_]], adds=[e_t])
            bgl = kb.bank()
            kb.mm(kb.psb(bgl, 4), [(kb.ones, g4)], reads=[kb.cst, sc_t], bank_tok=kb.pst[bgl])
            p.op('dve', 'tensor_tensor', smalls[:, 12:16], kb.psb(bgl, 4), smalls[:, 0:4], ALU.subtract, reads=[kb.pst[bgl], s_t], writes=[s_t])
            p.op('act', 'activation', out=smalls[:, 16:20], in_=kb.psb(bgl, 4), func=AF.Exp, reads=[kb.pst[bgl]], writes=[s_t])
            p.op('act', 'activation', out=smalls[:, 12:16], in_=smalls[:, 12:16], func=AF.Exp, reads=[s_t], writes=[s_t])
            if kb.gdn_stop == 4:
                ar.pop(); p.barrier(); return
            bk = kb.bank()
            p.group('pe', [('matmul', (kb.psb(bk, 128, off=hh * 128), kb_[:, hh * 128:(hh + 1) * 128], kb_[:, hh * 128:(hh + 1) * 128]), dict(start=True, stop=True))
                           for hh in range(4)], reads=[inb_t], writes=[kb.pst[bk]])
            bq = kb.bank()
            p.group('pe', [('matmul', (kb.psb(bq, 128, off=hh * 128), kb_[:, hh * 128:(hh + 1) * 128], qb_[:, hh * 128:(hh + 1) * 128]), dict(start=True, stop=True))
                           for hh in range(4)], reads=[inb_t], writes=[kb.pst[bq]])
            p.op('dve', 'tensor_tensor', Ltm, kb.psb(bk), Ea, ALU.mult, reads=[kb.pst[bk], e_t], writes=[l_t])
            p.op('dve', 'tensor_tensor', Lm, kb.psb(bk), Eb, ALU.mult, reads=[kb.pst[bk], e_t], adds=[l_t])
            p.op('dve', 'tensor_tensor', attnT, kb.psb(bq), Ec, ALU.mult, reads=[kb.pst[bq], e_t], writes=[a_t])
            p.op('pool', 'tensor_tensor', X, ident4, Ltm, ALU.subtract, reads=[cc, l_t], writes=[x_t])
            if kb.gdn_stop == 5:
                ar.pop(); p.barrier(); return
            Pc, Qc, pqc = Lm, Ltm, l_t
            for lev in range(6):
                Pn, Qn, pqn = Pp[lev % 2], Qq[lev % 2], pq_t[lev % 2]
                b_p = kb.bank()
                p.group('pe', [('matmul', (kb.psb(b_p, 128, off=hh * 128), Qc[:, hh * 128:(hh + 1) * 128], Pc[:, hh * 128:(hh + 1) * 128]), dict(start=True, stop=True))
                               for hh in range(4)], reads=[pqc], writes=[kb.pst[b_p]])
                p.op('act', 'activation', out=Pn, in_=kb.psb(b_p), func=AF.Copy, reads=[kb.pst[b_p]], writes=[pqn])
                if lev < 5:
                    b_q = kb.bank()
                    p.group('pe', [('matmul', (kb.psb(b_q, 128, off=hh * 128), Pc[:, hh * 128:(hh + 1) * 128], Qc[:, hh * 128:(hh + 1) * 128]), dict(start=True, stop=True))
                                   for hh in range(4)], reads=[pqc], writes=[kb.pst[b_q]])
                    p.op('dve', 'tensor_copy', Qn, kb.psb(b_q), reads=[kb.pst[b_q]], adds=[pqn])
                b_x = kb.bank()
                p.group('pe', [('matmul', (kb.psb(b_x, 128, off=hh * 128), Pn[:, hh * 128:(hh + 1) * 128], X[:, hh * 128:(hh + 1) * 128]), dict(start=True, stop=True))
                               for hh in range(4)], reads=[pqn, x_t], writes=[kb.pst[b_x]])
                p.op('dve', 'tensor_tensor', X, X, kb.psb(b_x), ALU.add, reads=[kb.pst[b_x]], writes=[x_t])
                Pc, Qc, pqc = Pn, Qn, pqn
            p.op('act', 'activation', out=Tt, in_=X, func=AF.Copy, reads=[x_t], writes=[tt_t])
            if kb.gdn_stop == 6:
                ar.pop(); p.barrier(); return
            p.op('dve', 'tensor_tensor', qdT, qT, EG, ALU.mult, reads=[in_t, e_t], writes=[qd_t])
            p.op('pool', 'tensor_tensor', H4(kdec), H4(ktok), bc_last(smalls[:, 12:16], 128), ALU.mult, reads=[tk_t, s_t], writes=[kd_t])
            if kb.gdn_stop == 7:
                ar.pop(); p.barrier(); return
            b_ks = kb.bank()
            p.group('pe', [('matmul', (kb.psb(b_ks, 128, off=hh * 128), kb_[:, hh * 128:(hh + 1) * 128], Sb[:, hh * 128:(hh + 1) * 128]), dict(start=True, stop=True))
                           for hh in range(4)], reads=[inb_t, Sb_t], writes=[kb.pst[b_ks]])
            p.op('dve', 'tensor_tensor', H4(Rp), H4(kb.psb(b_ks)), bc_last(smalls[:, 8:12], 128), ALU.mult, reads=[kb.pst[b_ks], s_t], writes=[r_t])
            p.op('pool', 'tensor_tensor', Rp, Rp, bv, ALU.add, reads=[tk_t], writes=[r_t])
            b_vn = kb.bank()
            p.group('pe', [('matmul', (kb.psb(b_vn, 128, off=hh * 128), Tt[:, hh * 128:(hh + 1) * 128], Rp[:, hh * 128:(hh + 1) * 128]), dict(start=True, stop=True))
                           for hh in range(4)], reads=[tt_t, r_t], writes=[kb.pst[b_vn]])
            p.op('act', 'activation', out=vnew, in_=kb.psb(b_vn), func=AF.Copy, reads=[kb.pst[b_vn]], writes=[vn_t])
            b_o = kb.bank()
            fns = []
            for hh in range(4):
                hs = slice(hh * 128, (hh + 1) * 128)
                fns.append(('matmul', (kb.psb(b_o, 128, off=hh * 128), qdT[:, hs], Sb[:, hs]), dict(start=True, stop=False)))
                fns.append(('matmul', (kb.psb(b_o, 128, off=hh * 128), attnT[:, hs], vnew[:, hs]), dict(start=False, stop=True)))
            p.group('pe', fns, reads=[qd_t, Sb_t, a_t, vn_t], writes=[kb.pst[b_o]])
            b_su = kb.bank()
            p.group('pe', [('matmul', (kb.psb(b_su, 128, off=hh * 128), kdec[:, hh * 128:(hh + 1) * 128], vnew[:, hh * 128:(hh + 1) * 128]), dict(start=True, stop=True))
                           for hh in range(4)], reads=[kd_t, vn_t], writes=[kb.pst[b_su]])
            for hh in range(4):
                hs = slice(hh * 128, (hh + 1) * 128)
                p.op('dve', 'scalar_tensor_tensor', out=S[:, hs], in0=S[:, hs], scalar=smalls[:, 16 + hh:17 + hh], in1=kb.psb(b_su, 128, off=hh * 128),
                     op0=ALU.mult, op1=ALU.add, reads=[kb.pst[b_su], s_t], writes=[S_t])
            p.op('act', 'activation', out=Sb, in_=S, func=AF.Copy, reads=[S_t], writes=[Sb_t])
            if kb.gdn_stop == 8:
                ar.pop(); p.barrier(); return
            O_ = ot[step % 2]; O_t = ot_t[step % 2]
            if dr == 0:
                p.op('act', 'activation', out=O_, in_=kb.psb(b_o), func=AF.Copy, reads=[kb.pst[b_o]], writes=[O_t])
                p.dma('sp', kb.scr['gof'][128 * i:128 * i + 128, :], O_, reads=[O_t], adds=[kb.scr_t['gof']])
            else:
                p.dma('sp', of_, kb.scr['gof'][128 * i:128 * i + 128, :], reads=[kb.scr_t['gof']], writes=[of_t])
                p.op('dve', 'tensor_tensor', O_, kb.psb(b_o), of_, ALU.add, reads=[kb.pst[b_o], of_t], writes=[O_t])
                bz = kb.bank()
                kb.mm(kb.psb(bz), [(hT3[:, kc, cols], wz[:, kc, :]) for kc in range(8)], reads=[wz_t] + kb.hT_all, bank_tok=kb.pst[bz])
                p.op('act', 'activation', out=zt, in_=kb.psb(bz), func=AF.Silu, reads=[kb.pst[bz]], writes=[z_t])
                for hh in range(4):
                    hs = slice(hh * 128, (hh + 1) * 128)
                    p.op('act', 'activation', out=of_[:, hs], in_=O_[:, hs], func=AF.Square, accum_out=ss4[:, hh:hh + 1], reads=[O_t, of_t], writes=[of_t])
                p.op('dve', 'tensor_scalar', ss4[:, 4:8], ss4[:, 0:4], 1.0 / 128, 1e-6, ALU.mult, ALU.add, reads=[of_t], writes=[of_t])
                p.op('act', 'activation', out=ss4[:, 4:8], in_=ss4[:, 4:8], func=AF.Sqrt, reads=[of_t], writes=[of_t])
                p.op('dve', 'reciprocal', ss4[:, 4:8], ss4[:, 4:8], reads=[of_t], writes=[of_t])
                p.op('dve', 'tensor_tensor', H4(O_), H4(O_), bc_last(ss4[:, 4:8], 128), ALU.mult, reads=[of_t], writes=[O_t])
                p.op('pool', 'tensor_tensor', O_, O_, gn4, ALU.mult, reads=[wz_t], writes=[O_t])
                p.op('pool', 'tensor_tensor', O_, O_, zt, ALU.mult, reads=[z_t], writes=[O_t])
                bt2 = kb.bank()
                p.group('pe', [('transpose', (kb.psb(bt2, 128, off=hh * 128), O_[:, hh * 128:(hh + 1) * 128], kb.ident), {}) for hh in range(4)],
                        reads=[O_t, kb.cst], writes=[kb.pst[bt2]])
                p.op('act', 'activation', out=ob4, in_=kb.psb(bt2), func=AF.Copy, reads=[kb.pst[bt2]], writes=[ob_t])
                lo, hi = 0, 128
                if i == 0:
                    lo = P0
                if i == NT - 1:
                    hi = 64
                p.dma('sp', kb.scr['brT0'].rearrange("(h p) t -> p h t", p=128)[:, :, 128 * i + lo:128 * i + hi], H4(ob4)[:, :, lo:hi],
                      reads=[ob_t], adds=[kb.scr_t['brT0']])
    ar.pop()
    p.barrier()


MBLOCKS = [(512 * b, 512) for b in range(8)] + [(4096, 128)]


def stage_merge(kb, l):
    import os
    p, ar = kb.p, kb.ar
    d = kb.din
    if 'mT' not in kb.scr:
        kb.scratch('mT', [1024, TP], BF16)
    hT3 = v3(kb.hT, 8)
    ar.push()
    zp = ar.bf16(64); zp_t = Tok('zp')
    p.op('pool', 'memset', zp, 0.0, writes=[zp_t])
    if os.environ.get('MERGE_STOP') != 'nopad':
        for c in range(8):
            p.dma('sp', kb.scr['mT'][c * 128:(c + 1) * 128, 0:P0], zp[:, 0:P0], reads=[zp_t], adds=[kb.scr_t['mT']])
            p.dma('sp', kb.scr['mT'][c * 128:(c + 1) * 128, P0 + T:TP], zp[:, 0:64], reads=[zp_t], adds=[kb.scr_t['mT']])
    wg = ar.bf16(4 * 8 * 512).rearrange("p (b c n) -> p b c n", b=4, c=8)
    wb = ar.bf16(4 * 4 * 512).rearrange("p (b c n) -> p b c n", b=4, c=4)
    wg_t = Tok('wg'); wb_t = Tok('wb')
    NB = 2
    brb = [[v3(ar.bf16(4 * 512), 4) for _ in range(4)] for _ in range(NB)]; brb_t = toks(NB, 'brb')
    sg = [ar.f32(512) for _ in range(2)]; tm = [ar.f32(512) for _ in range(2)]; sg_t = toks(2, 'sg')
    macc = [ar.f32(512) for _ in range(2)]; macc_t = toks(2, 'macc')
    mout = [v3(ar.bf16(4 * 512), 4) for _ in range(NB)]; mout_t = toks(NB, 'mout')
    cnt = 0
    fcnt = 0
    for g in range(2):
        for br in range(4):
            for kc in range(8):
                p.dma('pool', wg[:, br, kc, :], d['w_gate'][l, br, kc * 128:(kc + 1) * 128, g * 512:(g + 1) * 512], adds=[wg_t])
            for kc in range(4):
                p.dma('pool', wb[:, br, kc, :], d['w_branch'][l, br, kc * 128:(kc + 1) * 128, g * 512:(g + 1) * 512], adds=[wb_t])
        import os
        if os.environ.get('MERGE_STOP') == 'a':
            continue
        for bi, (p0, n) in enumerate(BLOCKS):
            k = bi % NB
            if os.environ.get('MERGE_STOP') == 'b' and bi > 0:
                continue
            for br in range(4):
                p.dma('sp', brb[k][br][:, :, 0:n], kb.scr[f'brT{br}'].rearrange("(c p) t -> p c t", p=128)[:, :, p0:p0 + n],
                      reads=[kb.scr_t[f'brT{br}']], adds=[brb_t[k]])
            for fc in range(4):
                fs = slice(fc * 128, (fc + 1) * 128)
                mk = fcnt % 2
                fcnt += 1
                for br in range(4):
                    j = cnt % 2
                    cnt += 1
                    bg = kb.bank()
                    kb.mm(kb.psb(bg, n), [(wg[:, br, kc, fs], hT3[:, kc, p0:p0 + n]) for kc in range(8)], reads=[wg_t] + kb.hT_all, bank_tok=kb.pst[bg])
                    bp = kb.bank()
                    kb.mm(kb.psb(bp, n), [(wb[:, br, kc, fs], brb[k][br][:, kc, 0:n]) for kc in range(4)], reads=[wb_t, brb_t[k]], bank_tok=kb.pst[bp])
                    p.op('act', 'activation', out=sg[j][:, 0:n], in_=kb.psb(bg, n), func=AF.Sigmoid, reads=[kb.pst[bg]], writes=[sg_t[j]])
                    if br == 0:
                        p.op('dve', 'tensor_tensor', macc[mk][:, 0:n], sg[j][:, 0:n], kb.psb(bp, n), ALU.mult, reads=[sg_t[j], kb.pst[bp]], writes=[macc_t[mk]])
                    else:
                        p.op('dve', 'tensor_tensor', tm[j][:, 0:n], sg[j][:, 0:n], kb.psb(bp, n), ALU.mult, reads=[sg_t[j], kb.pst[bp]], writes=[sg_t[j]])
                        p.op('pool', 'tensor_tensor', macc[mk][:, 0:n], macc[mk][:, 0:n], tm[j][:, 0:n], ALU.add, reads=[sg_t[j]], writes=[macc_t[mk]])
                p.op('act', 'activation', out=mout[k][:, fc, 0:n], in_=macc[mk][:, 0:n], func=AF.Copy, reads=[macc_t[mk]], adds=[mout_t[k]])
            p.dma('sp', kb.scr['mT'].rearrange("(c p) t -> p c t", p=128)[:, g * 4:(g + 1) * 4, p0:p0 + n], mout[k][:, :, 0:n],
                  reads=[mout_t[k]], adds=[kb.scr_t['mT']])
    ar.pop()
    p.barrier()
    import os
    if os.environ.get('MERGE_STOP') == '1':
        return
    ar.push()
    wo = v3(ar.bf16(8 * 1024), 8); wo_t = Tok('wo')
    load_w_cols(kb, wo, d['w_out'][l], 0, 1024, wo_t)
    lt = Tok('ln1p')
    g_bc = ar.f32(1024); b_bc = ar.f32(1024)
    bcast_row(kb, g_bc, d['ln1_g'][l], 1024, lt)
    bcast_row(kb, b_bc, d['ln1_b'][l], 1024, lt)
    rw = v3(ar.f32(8 * 32), 8)
    for kc in range(8):
        p.dma('sp', rw[:, kc, :], d['router_w'][l, kc * 128:(kc + 1) * 128, :], adds=[lt])
    rb = ar.f32(32)
    bcast_row(kb, rb, d['router_b'][l], 32, lt)
    NB = 2
    mt = [v3(ar.bf16(8 * 128), 8) for _ in range(NB)]; mt_t = toks(NB, 'mt')
    R = [ar.f32(1024) for _ in range(NB)]; R_t = toks(NB, 'R')
    X = [ar.f32(1024) for _ in range(NB)]; X_t = toks(NB, 'X')
    H = [ar.f32(1024) for _ in range(NB)]; H_t = toks(NB, 'H')
    TB = [ar.bf16(1024) for _ in range(NB)]; TB_t = toks(NB, 'TB')
    XB = [ar.f32(1024) for _ in range(NB)]; XB_t = toks(NB, 'XB')
    ST = [ar.f32(16) for _ in range(NB)]; ST_t = toks(NB, 'ST')
    rs = [ar.f32(64) for _ in range(NB)]; rs_t = toks(NB, 'rs')
    for i in range(NT):
        if os.environ.get('MERGE_STOP') == '3':
            continue
        k = i % NB
        cols = slice(128 * i, 128 * i + 128)
        p.dma('sp', mt[k], kb.scr['mT'].rearrange("(c p) t -> p c t", p=128)[:, :, cols], reads=[kb.scr_t['mT']], writes=[mt_t[k]])
        p.dma('sp', R[k], kb.scr['h_tok'][cols, :], reads=[kb.htok_t[i]], writes=[R_t[k]])
        for half in range(2):
            b = kb.bank()
            hs = slice(half * 512, (half + 1) * 512)
            kb.mm(kb.psb(b), [(mt[k][:, kc, :], wo[:, kc, hs]) for kc in range(8)], reads=[mt_t[k], wo_t], bank_tok=kb.pst[b])
            p.op('dve', 'scalar_tensor_tensor', out=X[k][:, hs], in0=R[k][:, hs], scalar=ALPHA, in1=kb.psb(b), op0=ALU.mult, op1=ALU.add,
                 reads=[R_t[k], kb.pst[b]], adds=[X_t[k]])
        if os.environ.get('MERGE_STOP') == '4':
            continue
        ln_tile(kb, X[k], X_t[k], H[k], H_t[k], ST[k], ST_t[k], g_bc, b_bc, lt, 1e-5)
        if os.environ.get('MERGE_STOP') == '5':
            continue
        h_epilogue(kb, i, H[k], H_t[k], TB[k], TB_t[k], extra_fp32=((XB[k], XB_t[k]) if os.environ.get('MERGE_STOP') != '6' else None))
        if os.environ.get('MERGE_STOP') == '2':
            continue
        b = kb.bank()
        kb.mm(kb.psb(b, 32), [(XB[k][:, c * 128:(c + 1) * 128], rw[:, c, :]) for c in range(8)], reads=[XB_t[k], lt], bank_tok=kb.pst[b])
        lg = rs[k][:, 0:32]; top8 = rs[k][:, 32:40]; s1 = rs[k][:, 40:41]; s2 = rs[k][:, 41:42]
        T_ = rs_t[k]
        p.op('dve', 'tensor_tensor', lg, kb.psb(b, 32), rb, ALU.add, reads=[kb.pst[b], lt], writes=[T_])
        p.op('dve', 'max', top8, lg, reads=[T_], writes=[T_])
        p.op('dve', 'tensor_scalar', s1, top8[:, 0:1], -1.0, None, ALU.mult, reads=[T_], writes=[T_])
        ga = kb.gates[:, i, :]
        p.op('act', 'activation', out=ga, in_=lg, func=AF.Exp, bias=s1, reads=[T_], writes=[kb.gates_t[i]])
        p.op('dve', 'tensor_scalar', lg, lg, top8[:, 3:4], None, ALU.is_ge, reads=[T_], writes=[T_])
        p.op('dve', 'tensor_tensor', ga, ga, lg, ALU.mult, reads=[T_], writes=[kb.gates_t[i]])
        p.op('dve', 'reduce_sum', s2, ga, AX.X, reads=[kb.gates_t[i]], writes=[T_])
        p.op('dve', 'reciprocal', s2, s2, reads=[T_], writes=[T_])
        p.op('dve', 'tensor_scalar', ga, ga, s2, None, ALU.mult, reads=[T_], writes=[kb.gates_t[i]])
    ar.pop()
    p.barrier()


def stage_moe(kb, l, last):
    p, ar = kb.p, kb.ar
    d = kb.din
    if 'macc' not in kb.scr:
        kb.scratch('macc', [TP, 1024], F32)
    kb.macc_t = toks(NT, 'macc')
    hT3 = v3(kb.hT, 8)
    ar.push()
    wgu = [v3(ar.bf16(8 * 2048), 8) for _ in range(2)]; wgu_t = toks(2, 'wgu')
    wdn = [v3(ar.bf16(8 * 1024), 8)]; wdn_t = toks(1, 'wdn')
    brow = [ar.bf16(3072) for _ in range(2)]; brow_t = toks(2, 'brow')
    aT = [v3(ar.bf16(8 * 512), 8) for _ in range(2)]; aT_t = toks(2, 'aT')
    gW = [ar.f32(512) for _ in range(2)]; sW = [ar.f32(512) for _ in range(2)]; lW = [ar.f32(512) for _ in range(2)]; w_t = toks(2, 'moew')
    A = [ar.f32(1024) for _ in range(2)]; A_t = toks(2, 'A')
    NE = 32

    def load_e(e):
        k = e % 2
        for kc in range(8):
            p.dma('pool', wgu[k][:, kc, :], d['moe_w_gate_up'][l, e, kc * 128:(kc + 1) * 128, :], adds=[wgu_t[k]])
        p.dma('pool', brow[k][0:1, 0:2048], d['moe_b_gate_up'][l, e].rearrange("(o n) -> o n", o=1), adds=[brow_t[k]])
        p.dma('pool', brow[k][0:1, 2048:3072], d['moe_b_down'][l, e].rearrange("(o n) -> o n", o=1), adds=[brow_t[k]])

    def load_dn(e):
        for kc in range(8):
            p.dma('pool', wdn[0][:, kc, :], d['moe_w_down'][l, e, kc * 128:(kc + 1) * 128, :], adds=[wdn_t[0]])

    load_e(0)
    acnt = 0
    wcnt = 0
    tcnt = 0
    for e in range(NE):
        k = e % 2
        if e + 1 < NE:
            load_e(e + 1)
        load_dn(e)
        W = wgu[k]; BR = brow[k]
        for bi, (p0, n) in enumerate(MBLOCKS):
            ak = acnt % 2
            acnt += 1
            for f in range(8):
                j = wcnt % 2
                wcnt += 1
                banks = []
                for par in range(2):
                    b = kb.bank()
                    banks.append(b)
                    cs = slice(256 * f + par, 256 * f + 256, 2)
                    pairs = [(W[:, kc, cs], hT3[:, kc, p0:p0 + n]) for kc in range(8)] + [(BR[0:1, cs], kb.ones_row[0:1, 0:n])]
                    kb.mm(kb.psb(b, n), pairs, reads=[wgu_t[k], brow_t[k], kb.cst] + kb.hT_t[p0 // 128:(p0 + n) // 128], bank_tok=kb.pst[b])
                N = slice(0, n)
                p.op('dve', 'tensor_scalar', gW[j][:, N], kb.psb(banks[0], n), 7.0, None, ALU.min, reads=[kb.pst[banks[0]]], writes=[w_t[j]])
                p.op('act', 'activation', out=sW[j][:, N], in_=gW[j][:, N], func=AF.Sigmoid, scale=1.702, reads=[w_t[j]], writes=[w_t[j]])
                p.op('dve', 'tensor_scalar', lW[j][:, N], kb.psb(banks[1], n), 7.0, -7.0, ALU.min, ALU.max, reads=[kb.pst[banks[1]]], writes=[w_t[j]])
                p.op('dve', 'scalar_tensor_tensor', out=lW[j][:, N], in0=lW[j][:, N], scalar=1.0, in1=gW[j][:, N], op0=ALU.add, op1=ALU.mult,
                     reads=[w_t[j]], writes=[w_t[j]])
                p.op('pool', 'tensor_tensor', aT[ak][:, f, N], lW[j][:, N], sW[j][:, N], ALU.mult, reads=[w_t[j]], adds=[aT_t[ak]])
            for q in range(n // 128):
                i = p0 // 128 + q
                tk = tcnt % 2
                tcnt += 1
                if e > 0:
                    p.dma('sp', A[tk], kb.scr['macc'][128 * i:128 * i + 128, :], reads=[kb.macc_t[i]], writes=[A_t[tk]])
                for half in range(2):
                    hs = slice(half * 512, (half + 1) * 512)
                    b = kb.bank()
                    pairs = [(aT[ak][:, kc, q * 128:(q + 1) * 128], wdn[0][:, kc, hs]) for kc in range(8)] + \
                            [(kb.ones_bf[0:1, 0:128], BR[0:1, 2048 + half * 512:2048 + (half + 1) * 512])]
                    kb.mm(kb.psb(b), pairs, reads=[aT_t[ak], wdn_t[0], brow_t[k], kb.cst], bank_tok=kb.pst[b])
                    gcol = kb.gates[:, i, e:e + 1]
                    if e == 0:
                        p.op('dve', 'tensor_scalar', A[tk][:, hs], kb.psb(b), gcol, None, ALU.mult, reads=[kb.pst[b], kb.gates_t[i]], adds=[A_t[tk]])
                    else:
                        p.op('dve', 'scalar_tensor_tensor', out=A[tk][:, hs], in0=kb.psb(b), scalar=gcol, in1=A[tk][:, hs], op0=ALU.mult, op1=ALU.add,
                             reads=[kb.pst[b], kb.gates_t[i]], writes=[A_t[tk]])
                p.dma('sp', kb.scr['macc'][128 * i:128 * i + 128, :], A[tk], reads=[A_t[tk]], writes=[kb.macc_t[i]])
    ar.pop()
    p.barrier()
    ar.push()
    lt = Tok('ln2p')
    g_bc = ar.f32(1024); b_bc = ar.f32(1024)
    bcast_row(kb, g_bc, d['ln2_g'][l], 1024, lt)
    bcast_row(kb, b_bc, d['ln2_b'][l], 1024, lt)
    NB = 2
    R = [ar.f32(1024) for _ in range(NB)]; R_t = toks(NB, 'R2')
    Y = [ar.f32(1024) for _ in range(NB)]; Y_t = toks(NB, 'Y2')
    H = [ar.f32(1024) for _ in range(NB)]; H_t = toks(NB, 'H2')
    TB = [ar.bf16(1024) for _ in range(NB)]; TB_t = toks(NB, 'TB2')
    ST = [ar.f32(16) for _ in range(NB)]; ST_t = toks(NB, 'ST2')
    for i in range(NT):
        k = i % NB
        rows = slice(128 * i, 128 * i + 128)
        p.dma('sp', R[k], kb.scr['h_tok'][rows, :], reads=[kb.htok_t[i]], writes=[R_t[k]])
        p.dma('sp', Y[k], kb.scr['macc'][rows, :], reads=[kb.macc_t[i]], writes=[Y_t[k]])
        p.op('dve', 'scalar_tensor_tensor', out=Y[k], in0=R[k], scalar=ALPHA, in1=Y[k], op0=ALU.mult, op1=ALU.add, reads=[R_t[k]], writes=[Y_t[k]])
        ln_tile(kb, Y[k], Y_t[k], H[k], H_t[k], ST[k], ST_t[k], g_bc, b_bc, lt, 1e-5)
        if last:
            if i == 0:
                p.dma('sp', kb.out[0:64, :], H[k][64:128, :], reads=[H_t[k]])
            elif i == NT - 1:
                p.dma('sp', kb.out[4032:4096, :], H[k][0:64, :], reads=[H_t[k]])
            else:
                p.dma('sp', kb.out[128 * i - 64:128 * i + 64, :], H[k], reads=[H_t[k]])
            if 'h_tok' in kb.debug:
                p.dma('sp', kb.scr['h_tok'][rows, :], H[k], reads=[H_t[k]], adds=[kb.htok_t[i]])
        else:
            h_epilogue(kb, i, H[k], H_t[k], TB[k], TB_t[k])
    ar.pop()
    p.barrier()


ALL_STAGES = ('lru', 'mla', 'diff', 'gdn', 'merge', 'moe')


def build_program(debug=(), stages=ALL_STAGES, layers=(0, 1)):
    nc = bass.Bass("TRN2", target_bir_lowering=False)
    es = ExitStack()
    kb = KB(nc, es, debug)
    out = nc.dram_tensor('out', [4096, 1024], F32, kind="ExternalOutput").ap()
    kb.out = out
    kb.scratch('h_tok', [TP, 1024], F32)
    for i in range(4):
        kb.scratch(f'brT{i}', [512, TP], BF16)
    p, ar = kb.p, kb.ar
    kb.htok_t = toks(NT, 'htok')
    kb.hT = ar.bf16(8 * TP)
    kb.hT_t = toks(NT, 'hT')
    kb.hT_all = kb.hT_t
    kb.cst = Tok('cst')
    kb.ident = ar.f32(128)
    p.dma('sp', kb.ident, kb.din['c_ident'], adds=[kb.cst])
    p.op('pool', 'memset', kb.hT, 0.0, writes=kb.hT_t)
    kb.gates = v3(ar.f32(NT * 32), NT)
    kb.gates_t = toks(NT, 'gates')
    setup_consts(kb)
    stage0(kb)
    for l in layers:
        if 'lru' in stages:
            stage_lru(kb, l)
        if 'mla' in stages:
            stage_mla(kb, l)
        if 'diff' in stages:
            stage_diff(kb, l)
        if 'gdn' in stages:
            stage_gdn(kb, l)
        if 'merge' in stages:
            stage_merge(kb, l)
        if 'moe' in stages:
            stage_moe(kb, l, last=(l == layers[-1]))
    if 'moe' not in stages:
        z = ar.f32(1024); zt = Tok('z')
        p.op('pool', 'memset', z, 0.0, writes=[zt])
        p.dma('sp', out[0:128, :], z, reads=[zt])
    p.emit()
    print('n_ins', p.n_ins, 'cnt', p.cur_cnt, 'dma', p.ring_i, 'sbuf top', ar.top)
    kb.used_inputs = list(kb.din.keys())
    return nc, es, kb


_CACHE = {}


def kernel(**inputs):
    if 'prog' not in _CACHE:
        _CACHE['prog'] = build_program(stages=ALL_STAGES, layers=(0, 1))
    nc, es, kb = _CACHE['prog']
    consts = make_consts()
    shared = {}
    for nm in kb.used_inputs:
        if nm in ('x', 'positions'):
            continue
        if nm.startswith('c_'):
            shared[nm] = consts[nm[2:]]
        else:
            shared[nm] = np.ascontiguousarray(np.asarray(inputs[nm], dtype=np.float32))
    in_maps = []
    for b in range(8):
        m = dict(shared)
        m['x'] = np.ascontiguousarray(np.asarray(inputs['x'][b], dtype=np.float32))
        m['positions'] = np.ascontiguousarray(np.asarray(inputs['positions'][b]).astype(np.int32))
        in_maps.append(m)
    res = run_bass_kernel_spmd(nc, in_maps, core_ids=list(range(8)))
    return np.stack([np.asarray(r['out']) for r in res.results], axis=0).astype(np.float32)
```
